# Optimizing a Trainium2 kernel written in Bass

```python
import math
import jax, jax.numpy as jnp
from jax import lax
import numpy as np

D_MODEL = 2048
BATCH = 4
SEQ = 8192
DEPTH = 1

CTX_LEN = 256
GRID_W = 64
SSD_EXPAND = 2
D_SSD = SSD_EXPAND * D_MODEL
SSD_HEADDIM = 64
SSD_HEADS = D_SSD // SSD_HEADDIM
SSD_GROUPS = 8
SSD_STATE = 128
SSD_CONV = 5
SSD_CHUNK = 128
GN = SSD_GROUPS * SSD_STATE
D_SC = D_MODEL
SC_CONV = 3
PEER_HEADS = 8
PEER_NKEYS = 128
PEER_EXPERTS = PEER_NKEYS * PEER_NKEYS
PEER_TOPK = 16
PEER_DKEY = 256
PEER_DHALF = PEER_DKEY // 2
PEER_BLOCK = 128
N_ADA = 6
EPS = 1e-6
N_XBC = D_SSD + 2 * GN
OFF_DT = N_XBC
OFF_Z = OFF_DT + 2 * SSD_HEADS
OFF_SC = OFF_Z + D_SSD
OFF_GATE = OFF_SC + 3 * D_SC
N_IN = OFF_GATE + 2 * D_MODEL

kernel_name = "hybrid_ssd_shortconv_peer_dit_block"


def rmsnorm(x, g):
    xf = x.astype(jnp.float32)
    y = xf * lax.rsqrt(jnp.mean(xf * xf, axis=-1, keepdims=True) + EPS)
    return y.astype(x.dtype) * g


def modulate(h, shift, scale):
    return h * (1 + scale) + shift


def rev(t):
    return jnp.flip(t, axis=1)


def ada_chunks(cvec, w, b, n):
    cols = n * D_MODEL
    mod = jax.nn.silu(cvec) @ w[:, :cols] + b[:cols]
    return jnp.split(mod, n, axis=-1)


def dwconv(x, w):
    k_w = w.shape[0]
    pad = k_w // 2
    length = x.shape[-2]
    xp = jnp.pad(x, [(0, 0)] * (x.ndim - 2) + [(pad, pad), (0, 0)])
    acc = w[0] * xp[..., 0:length, :]
    for k in range(1, k_w):
        acc = acc + w[k] * xp[..., k:k + length, :]
    return acc


def ssd_inputs(p_xbc, p_dt, lp):
    b, length, nch = p_xbc.shape
    xbc = jax.nn.silu(dwconv(p_xbc, lp["ssd_conv_w"][:, :nch]) + lp["ssd_conv_b"][:nch])
    xs = xbc[..., :D_SSD].reshape(b, length, SSD_HEADS, SSD_HEADDIM)
    groups = xbc[..., D_SSD:].reshape(b, length, -1, SSD_GROUPS, SSD_STATE)
    dt = jax.nn.softplus(p_dt.astype(jnp.float32) + lp["ssd_dt_bias"].reshape(-1))
    dt = dt.reshape(b, length, 2, SSD_HEADS)
    return xs, groups, dt[:, :, 0], dt[:, :, 1]


def ssd_scan(xs, dt, a_neg, bm, cm, h0):
    b, length, _, _ = xs.shape
    n_chunks = length // SSD_CHUNK
    rg = SSD_HEADS // SSD_GROUPS

    def chunks(t):
        return jnp.moveaxis(t.reshape((b, n_chunks, SSD_CHUNK) + t.shape[2:]), 1, 0)

    xq = chunks(xs.reshape(b, length, SSD_GROUPS, rg, SSD_HEADDIM))
    aq = chunks((dt * a_neg).reshape(b, length, SSD_GROUPS, rg))
    dtq = chunks(dt.reshape(b, length, SSD_GROUPS, rg))
    bq, cq = chunks(bm), chunks(cm)
    tril = jnp.tril(jnp.ones((SSD_CHUNK, SSD_CHUNK), dtype=bool))[:, :, None, None]

    def step(h, inp):
        x_c, a_c, dt_c, b_c, c_c = inp
        acum = jnp.cumsum(a_c, axis=1)
        seg = acum[:, :, None] - acum[:, None, :]
        lmat = jnp.exp(jnp.where(tril, seg, -jnp.inf))
        cb = jnp.einsum("bign,bjgn->bijg", c_c, b_c)
        wts = cb[..., None] * lmat * dt_c[:, None]
        y = jnp.einsum("bijgr,bjgrp->bigrp", wts, x_c)
        y = y + jnp.einsum("bign,bgrpn->bigrp", c_c, h) * jnp.exp(acum)[..., None]
        decay_end = jnp.exp(acum[:, -1:] - acum) * dt_c
        h_new = jnp.exp(acum[:, -1])[..., None, None] * h + jnp.einsum(
            "bjgn,bjgr,bjgrp->bgrpn", b_c, decay_end, x_c)
        return h_new, y

    h_init = h0.reshape(b, SSD_GROUPS, rg, SSD_HEADDIM, SSD_STATE)
    h_fin, y = lax.scan(step, h_init, (xq, aq, dtq, bq, cq))
    y = jnp.moveaxis(y, 0, 1).reshape(b, length, SSD_HEADS, SSD_HEADDIM)
    return y, h_fin.reshape(b, SSD_HEADS, SSD_HEADDIM, SSD_STATE)


def ssd_final_state(xs, dt, a_neg, bm):
    b, length, _, _ = xs.shape
    rg = SSD_HEADS // SSD_GROUPS
    acum = jnp.cumsum(dt * a_neg, axis=1)
    wts = (jnp.exp(acum[:, -1:] - acum) * dt).reshape(b, length, SSD_GROUPS, rg)
    hs = jnp.einsum("blgn,blgr,blgrp->bgrpn", bm, wts,
                    xs.reshape(b, length, SSD_GROUPS, rg, SSD_HEADDIM))
    return hs.reshape(b, SSD_HEADS, SSD_HEADDIM, SSD_STATE)


def token_mixer(h, lp, h0_f, h0_b, on_grid):
    b, length, _ = h.shape
    w_in = lp["w_in"]
    xs, groups, dt_f, dt_b = ssd_inputs(h @ w_in[:, :N_XBC], h @ w_in[:, OFF_DT:OFF_Z], lp)
    bm, cm = groups[:, :, 0], groups[:, :, 1]
    a_neg = lp["A"]
    y_f, h_f = ssd_scan(xs, dt_f, a_neg[0], bm, cm, h0_f)
    y_b, h_b = ssd_scan(rev(xs), rev(dt_b), a_neg[1], rev(bm), rev(cm), h0_b)
    y = y_f + rev(y_b) + lp["ssd_D"][:, None] * xs
    z = h @ w_in[:, OFF_Z:OFF_SC]
    y = rmsnorm(y.reshape(b, length, D_SSD) * jax.nn.silu(z), lp["ssd_norm_g"])
    y_ssd = y @ lp["ssd_w_out"]
    sc_b, sc_c, sc_x = jnp.split(h @ w_in[:, OFF_SC:OFF_GATE], 3, axis=-1)
    u = sc_c * sc_x
    if on_grid:
        rows = length // GRID_W
        u = dwconv(u.reshape(b, rows, GRID_W, D_SC), lp["sc_conv_w"]).reshape(b, length, D_SC)
    else:
        u = dwconv(u, lp["sc_conv_w"])
    y_sc = (sc_b * u) @ lp["sc_w_out"]
    g_ssd, g_sc = jnp.split(jax.nn.sigmoid(h @ w_in[:, OFF_GATE:]), 2, axis=-1)
    out = (g_ssd * y_ssd + g_sc * y_sc) @ lp["w_o"]
    return out, h_f, h_b


def peer_ffn(h, lp):
    b, length, d = h.shape
    blocks = h.reshape(b * length // PEER_BLOCK, PEER_BLOCK, d)

    def one_block(hb):
        q = (hb @ lp["peer_w_q"]).reshape(PEER_BLOCK, PEER_HEADS, 2, PEER_DHALF)
        s = jnp.einsum("thsk,hsnk->thsn", q, lp["peer_keys"])
        sv, si = lax.top_k(s, PEER_TOPK)
        cand_s = (sv[:, :, 0, :, None] + sv[:, :, 1, None, :]).reshape(PEER_BLOCK, PEER_HEADS, -1)
        cand_i = (si[:, :, 0, :, None] * PEER_NKEYS + si[:, :, 1, None, :]).reshape(PEER_BLOCK, PEER_HEADS, -1)
        top_s, pos = lax.top_k(cand_s, PEER_TOPK)
        idx = jnp.take_along_axis(cand_i, pos, axis=-1)
        gate = jax.nn.softmax(top_s.astype(jnp.float32), axis=-1)
        u = lp["peer_u"][idx]
        act = jax.nn.gelu(jnp.einsum("td,thkd->thk", hb, u), approximate=False)
        return jnp.einsum("thk,thkd->td", (gate * act).astype(hb.dtype), lp["peer_v"][idx])

    return lax.map(one_block, blocks).reshape(b, length, d)


def setup_inputs(seed: int = 0) -> dict:
    key = jax.random.key(seed)
    ks = jax.random.split(key, 24)

    def nrm(k, shape, s):
        return jax.random.normal(k, shape, jnp.float32) * s

    dt0 = jnp.exp(jax.random.uniform(ks[10], (DEPTH, 2, SSD_HEADS), jnp.float32,
                                     math.log(1e-3), math.log(1e-1)))
    return {
        "x": nrm(ks[0], (BATCH, SEQ, D_MODEL), 1.0),
        "c": nrm(ks[1], (BATCH, D_MODEL), 1.0),
        "ctx": nrm(ks[2], (BATCH, CTX_LEN, D_MODEL), 1.0),
        "c_ctx": nrm(ks[3], (D_MODEL,), 1.0),
        "w_ada": nrm(ks[4], (DEPTH, D_MODEL, N_ADA * D_MODEL), D_MODEL ** -0.5),
        "b_ada": nrm(ks[5], (DEPTH, N_ADA * D_MODEL), 0.02),
        "norm1_g": 1.0 + nrm(ks[6], (DEPTH, D_MODEL), 0.02),
        "norm2_g": 1.0 + nrm(ks[7], (DEPTH, D_MODEL), 0.02),
        "w_in": nrm(ks[8], (DEPTH, D_MODEL, N_IN), D_MODEL ** -0.5),
        "ssd_conv_w": nrm(ks[9], (DEPTH, SSD_CONV, N_XBC), SSD_CONV ** -0.5),
        "ssd_conv_b": nrm(ks[11], (DEPTH, N_XBC), 0.02),
        "ssd_dt_bias": dt0 + jnp.log(-jnp.expm1(-dt0)),
        "ssd_A_log": jnp.log(jax.random.uniform(ks[12], (DEPTH, 2, SSD_HEADS), jnp.float32, 1.0, 16.0)),
        "ssd_D": 1.0 + nrm(ks[13], (DEPTH, SSD_HEADS), 0.02),
        "ssd_norm_g": 1.0 + nrm(ks[14], (DEPTH, D_SSD), 0.02),
        "ssd_w_out": nrm(ks[15], (DEPTH, D_SSD, D_MODEL), D_SSD ** -0.5),
        "sc_conv_w": nrm(ks[16], (DEPTH, SC_CONV, D_SC), SC_CONV ** -0.5),
        "sc_w_out": nrm(ks[17], (DEPTH, D_SC, D_MODEL), D_SC ** -0.5),
        "w_o": nrm(ks[18], (DEPTH, D_MODEL, D_MODEL), D_MODEL ** -0.5),
        "peer_w_q": nrm(ks[19], (DEPTH, D_MODEL, PEER_HEADS * PEER_DKEY), D_MODEL ** -0.5),
        "peer_keys": nrm(ks[20], (DEPTH, PEER_HEADS, 2, PEER_NKEYS, PEER_DHALF), PEER_DHALF ** -0.5),
        "peer_u": nrm(ks[21], (DEPTH, PEER_EXPERTS, D_MODEL), D_MODEL ** -0.5),
        "peer_v": nrm(ks[22], (DEPTH, PEER_EXPERTS, D_MODEL), PEER_HEADS ** -0.5),
        "final_g": 1.0 + nrm(ks[23], (D_MODEL,), 0.02),
    }


def reference(x, c, ctx, c_ctx, w_ada, b_ada, norm1_g, norm2_g, w_in, ssd_conv_w, ssd_conv_b,
              ssd_dt_bias, ssd_A_log, ssd_D, ssd_norm_g, ssd_w_out, sc_conv_w, sc_w_out, w_o,
              peer_w_q, peer_keys, peer_u, peer_v, final_g):
    a_all = -jnp.exp(ssd_A_log.astype(jnp.float32))
    for i in range(DEPTH):
        lp = {"w_in": w_in[i], "ssd_conv_w": ssd_conv_w[i], "ssd_conv_b": ssd_conv_b[i],
              "ssd_dt_bias": ssd_dt_bias[i], "A": a_all[i], "ssd_D": ssd_D[i],
              "ssd_norm_g": ssd_norm_g[i], "ssd_w_out": ssd_w_out[i], "sc_conv_w": sc_conv_w[i],
              "sc_w_out": sc_w_out[i], "w_o": w_o[i], "peer_w_q": peer_w_q[i],
              "peer_keys": peer_keys[i], "peer_u": peer_u[i], "peer_v": peer_v[i]}
        last = i == DEPTH - 1
        if last:
            sh_c1, sc_c1 = ada_chunks(c_ctx, w_ada[i], b_ada[i], 2)
            hc = modulate(rmsnorm(ctx, norm1_g[i]), sh_c1, sc_c1)
            xs_c, grp_c, dtf_c, dtb_c = ssd_inputs(hc @ w_in[i][:, :D_SSD + GN],
                                                   hc @ w_in[i][:, OFF_DT:OFF_Z], lp)
            b_c = grp_c[:, :, 0]
            hf_c = ssd_final_state(xs_c, dtf_c, lp["A"][0], b_c)
            hb_c = ssd_final_state(rev(xs_c), rev(dtb_c), lp["A"][1], rev(b_c))
        else:
            sh_c1, sc_c1, g_c1, sh_c2, sc_c2, g_c2 = ada_chunks(c_ctx, w_ada[i], b_ada[i], N_ADA)
            hc = modulate(rmsnorm(ctx, norm1_g[i]), sh_c1, sc_c1)
            zero = jnp.zeros((ctx.shape[0], SSD_HEADS, SSD_HEADDIM, SSD_STATE), jnp.float32)
            out_c, hf_c, hb_c = token_mixer(hc, lp, zero, zero, False)
        sh1, sc1, g1, sh2, sc2, g2 = [m[:, None, :] for m in ada_chunks(c, w_ada[i], b_ada[i], N_ADA)]
        h = modulate(rmsnorm(x, norm1_g[i]), sh1, sc1)
        out, _, _ = token_mixer(h, lp, hf_c, hb_c, True)
        x = x + g1 * out
        x = x + g2 * peer_ffn(modulate(rmsnorm(x, norm2_g[i]), sh2, sc2), lp)
        if not last:
            ctx = ctx + g_c1 * out_c
            ctx = ctx + g_c2 * peer_ffn(modulate(rmsnorm(ctx, norm2_g[i]), sh_c2, sc_c2), lp)
    return rmsnorm(x, final_g)
```

```python
import numpy as np
from contextlib import ExitStack
import concourse.bass as bass
import concourse.mybir as mybir
from concourse.bass_utils import run_bass_kernel_spmd

F32 = mybir.dt.float32
BF16 = mybir.dt.bfloat16
U32 = mybir.dt.uint32
I32 = mybir.dt.int32
AF = mybir.ActivationFunctionType
ALU = mybir.AluOpType
AX = mybir.AxisListType

D = 2048
DC = 16
DSSD = 4096
NH = 64
EPS = 1e-6
NG_IN = 161
ENGS = ("pe", "act", "dve", "pool", "sp")
DMA_ENGS = ("sp", "pool")


class Buf:
    __slots__ = ("w", "r")

    def __init__(self):
        self.w = {}
        self.r = {}


class Prog:
    def __init__(self, nc, stack, n_slots=8):
        self.nc = nc
        self.stack = stack
        self.ops = {e: [] for e in ENGS}
        self.cnt = {}
        self.known = {e: {} for e in ENGS}
        self.sems = {}
        self.cur = {}
        self.epoch = 0
        for e in ("pe", "act", "dve", "pool"):
            self._new_compute_sem(e)
        self.slots = {}
        self.slot_next = {}
        for e in DMA_ENGS:
            self.slots[e] = []
            self.slot_next[e] = 0
            for i in range(n_slots):
                k = "d_%s_%d" % (e, i)
                self.sems[k] = stack.enter_context(nc.semaphore(k))
                self.cnt[k] = 0
                self.slots[e].append(k)
        self.bufs = {}
        self.n_ops = 0

    def _new_compute_sem(self, e):
        k = "c_%s_%d" % (e, self.epoch)
        self.sems[k] = self.stack.enter_context(self.nc.semaphore(k))
        self.cnt[k] = 0
        self.cur[e] = k

    def _b(self, name):
        b = self.bufs.get(name)
        if b is None:
            b = Buf()
            self.bufs[name] = b
        return b

    def emit(self, eng, fn, r=(), w=(), pw=(), dma=False):
        deps = {}

        def add(d):
            for k, v in d.items():
                if deps.get(k, 0) < v:
                    deps[k] = v

        for n in r:
            add(self._b(n).w)
        for n in w:
            b = self._b(n)
            add(b.w)
            add(b.r)
        for n in pw:
            add(self._b(n).r)
        if dma:
            sl = self.slots[eng]
            k = sl[self.slot_next[eng] % len(sl)]
            self.slot_next[eng] += 1
            if self.cnt[k] > 0:
                add({k: self.cnt[k]})
            self.cnt[k] += 16
            inc = 16
        else:
            k = self.cur[eng]
            self.cnt[k] += 1
            inc = 1
        sig = (k, self.cnt[k])
        waits = []
        kn = self.known[eng]
        for dk, dv in deps.items():
            if (not dma) and dk == k and eng == "pe":
                continue
            if kn.get(dk, 0) >= dv:
                continue
            kn[dk] = dv
            waits.append((dk, dv))
        self.ops[eng].append((waits, fn, sig[0], inc))
        for n in r:
            b = self._b(n)
            if b.r.get(sig[0], 0) < sig[1]:
                b.r[sig[0]] = sig[1]
        for n in w:
            b = self._b(n)
            b.w = {sig[0]: sig[1]}
            b.r = {}
        for n in pw:
            b = self._b(n)
            b.w[sig[0]] = sig[1]
            b.r = {}
        self.n_ops += 1

    def barrier(self):
        allc = [(k, v) for k, v in self.cnt.items() if v > 0]
        for e in ENGS:
            kn = self.known[e]
            waits = []
            for k, v in allc:
                if kn.get(k, 0) < v:
                    kn[k] = v
                    waits.append((k, v))
            if waits:
                self.ops[e].append((waits, None, None, 0))
        self.bufs = {}
        self.epoch += 1
        for e in ("pe", "act", "dve", "pool"):
            if self.cnt[self.cur[e]] > 20000:
                self._new_compute_sem(e)

    def replay(self):
        nc = self.nc
        with nc.Block() as block:
            def run(e):
                def body(engine):
                    for waits, fn, sk, inc in self.ops[e]:
                        for k, v in waits:
                            engine.wait_ge(self.sems[k], v)
                        if fn is not None:
                            fn(engine).then_inc(self.sems[sk], inc)
                return body
            block.tensor(run("pe"))
            block.scalar(run("act"))
            block.vector(run("dve"))
            block.gpsimd(run("pool"))
            block.sync(run("sp"))


class Alloc:
    def __init__(self, big, nwords):
        self.big = big
        self.cap = nwords
        self.off = 0

    def f32(self, n):
        ap = self.big[:, self.off:self.off + n]
        self.off += n
        assert self.off <= self.cap, ("SBUF overflow", self.off, self.cap)
        return ap

    def bf16(self, n):
        return self.f32((n + 1) // 2).bitcast(BF16)

    def u32(self, n):
        return self.f32(n).bitcast(U32)


def build_nc(Lm, La, Lc, TB=2048, T3=512, CSP=2048, dbg=(), stop_after=None):
    nc = bass.Bass("TRN2", target_bir_lowering=False)
    T3 = min(T3, Lm)
    Ltot = Lm + La
    NM, NA, NC_ = Lm // 128, La // 128, Lc // 128
    NCH = NM + NA + NC_

    def din(name, shape, dt=F32):
        return nc.dram_tensor(name, list(shape), dt, kind="ExternalInput").ap()

    def dscr(name, shape, dt=F32):
        kind = "ExternalOutput" if name in dbg else "Internal"
        return nc.dram_tensor(name, list(shape), dt, kind=kind).ap()

    xm = din("xm", [Lm, D])
    xa = din("xa", [La, D])
    xc = din("xc", [Lc, D])
    cvec = din("cvec", [128, 32])
    w_ada = din("w_ada", [96, 128, 2048])
    b_ada = din("b_ada", [128, 96])
    n1g_d = din("n1g", [128, 16])
    n2g_d = din("n2g", [128, 16])
    w_in = din("w_in", [NG_IN, 128, 2048])
    convw_d = din("convw", [128, 48 * 5])
    convb_d = din("convb", [128, 48])
    dtb_d = din("dtb", [128, 1])
    alog_d = din("alog", [128, 1])
    ssdD_d = din("ssdD", [1, 64])
    gn_d = din("gnorm", [128, 32])
    wssd_d = din("wssd", [16, 128, 4096])
    scw_d = din("scw", [128, 48])
    wsc_d = din("wsc", [16, 128, 2048])
    wo_d = din("wo", [16, 128, 2048])
    wq_d = din("wq", [16, 128, 2048])
    keys_d = din("keysT", [128, 2048])
    pu_d = din("peer_u", [16384, D])
    pv_d = din("peer_v", [16384, D])
    fg_d = din("final_g", [1, D])
    out_d = nc.dram_tensor("out", [Lm, D], F32, kind="ExternalOutput").ap()

    S_a = dscr("S_a", [49 * 128, Ltot + 4])
    S_ac = dscr("S_ac", [49 * 128, Lc + 4])
    S_b = dscr("S_b", [112 * 128, Lm])
    S_xt = dscr("S_xt", [Ltot + Lc, DSSD])
    S_bt = dscr("S_bt", [Ltot + Lc, 1024])
    S_bT = dscr("S_bT", [1024, Lm])
    S_cT = dscr("S_cT", [1024, Lm])
    S_dtt = dscr("S_dtt", [NCH, 128, 128])
    S_act = dscr("S_act", [NCH, 128, 128])
    S_acc = dscr("S_acc", [NCH, 128 * 128])
    S_tot = dscr("S_tot", [NCH, 128])
    S_yb = dscr("S_yb", [Lm, DSSD])
    S_yn = dscr("S_yn", [DSSD, Lm], BF16)
    S_x1 = dscr("S_x1", [Lm, D])
    S_vec = dscr("S_vec", [64, 128])
    S_hst = dscr("S_hst", [2, 128, DSSD])
    S_uv = dscr("S_uv", [16384, 2 * D], BF16)

    with ExitStack() as st:
        NW = 52400
        big = st.enter_context(nc.sbuf_tensor("big", [128, NW], F32))
        PS = st.enter_context(nc.psum_tensor("PS", [128, 4096], F32))
        A = Alloc(big, NW)
        P = Prog(nc, st)
        E = P.emit

        def bank(b, n=512, o=0):
            return PS[:, b * 512 + o:b * 512 + o + n]

        def pb(*bs):
            return ["ps%d" % b for b in bs]

        def ld(name, out_ap, in_ap, r=(), eng="sp"):
            E(eng, lambda e: e.dma_start(out=out_ap, in_=in_ap), r=list(r), w=[name], dma=True)

        def stq(out_name, out_ap, in_name, in_ap, eng="pool"):
            E(eng, lambda e: e.dma_start(out=out_ap, in_=in_ap), r=[in_name], pw=[out_name], dma=True)

        iot = A.f32(128)
        ident = A.f32(128)
        maskU = A.f32(128)
        maskL = A.f32(128)
        io256 = A.f32(256)
        ones = A.f32(128)
        zt = A.f32(128)
        a1 = A.f32(16)
        sh1 = A.f32(16)
        a1c = A.f32(16)
        sh1c = A.f32(16)
        convw = A.f32(240)
        convb = A.f32(48)
        scw = A.f32(48)
        dtb = A.f32(1)
        acol_A = A.f32(1)
        gncol = A.f32(32)
        Dbc = A.f32(64)
        E("pool", lambda e: e.iota(iot, pattern=[[1, 128]], base=0, channel_multiplier=-1,
                                   allow_small_or_imprecise_dtypes=True), w=["iot"])
        E("pool", lambda e: e.iota(io256, pattern=[[1, 256]], base=0, channel_multiplier=0,
                                   allow_small_or_imprecise_dtypes=True), w=["io256"])
        E("dve", lambda e: e.tensor_single_scalar(out=ident, in_=iot, scalar=0.0, op=ALU.is_equal), r=["iot"], w=["ident"])
        E("dve", lambda e: e.tensor_single_scalar(out=maskU, in_=iot, scalar=0.0, op=ALU.is_ge), r=["iot"], w=["maskU"])
        E("dve", lambda e: e.tensor_single_scalar(out=maskL, in_=iot, scalar=0.0, op=ALU.is_le), r=["iot"], w=["maskL"])
        E("pool", lambda e: e.memset(ones, 1.0), w=["ones"])
        E("pool", lambda e: e.memset(zt, 0.0), w=["zt"])
        ld("convw", convw, convw_d)
        ld("convb", convb, convb_d)
        ld("scw", scw, scw_d)
        ld("dtb", dtb, dtb_d)
        ld("gncol", gncol, gn_d)
        ld("Dbc", Dbc, ssdD_d.partition_broadcast(128))
        alog = A.f32(1)
        ld("alog", alog, alog_d)
        E("act", lambda e: e.activation(out=acol_A, in_=alog, func=AF.Exp), r=["alog"], w=["acolA"])
        E("dve", lambda e: e.tensor_single_scalar(out=acol_A, in_=acol_A, scalar=-1.0, op=ALU.mult), r=["acolA"], w=["acolA"])
        E("sp", lambda e: e.dma_start(out=S_a.rearrange("(g p) c -> p g c", p=128)[:, :, 0:2],
                                      in_=zt[:, 0:98].rearrange("p (g c) -> p g c", c=2)), r=["zt"], pw=["S_a"], dma=True)
        E("sp", lambda e: e.dma_start(out=S_a.rearrange("(g p) c -> p g c", p=128)[:, :, Ltot + 2:Ltot + 4],
                                      in_=zt[:, 0:98].rearrange("p (g c) -> p g c", c=2)), r=["zt"], pw=["S_a"], dma=True)
        E("sp", lambda e: e.dma_start(out=S_ac.rearrange("(g p) c -> p g c", p=128)[:, :, 0:2],
                                      in_=zt[:, 0:98].rearrange("p (g c) -> p g c", c=2)), r=["zt"], pw=["S_ac"], dma=True)
        E("sp", lambda e: e.dma_start(out=S_ac.rearrange("(g p) c -> p g c", p=128)[:, :, Lc + 2:Lc + 4],
                                      in_=zt[:, 0:98].rearrange("p (g c) -> p g c", c=2)), r=["zt"], pw=["S_ac"], dma=True)
        base_off = A.off

        cv = A.f32(32)
        scv = A.f32(32)
        wts = [A.f32(2048), A.f32(2048)]
        modv = A.f32(192)
        bl = A.f32(96)
        n1g = A.f32(16)
        n2g = A.f32(16)
        V4 = A.f32(64)
        V4T = A.f32(128)
        ld("cv", cv, cvec)
        ld("bl", bl, b_ada)
        ld("n1g", n1g, n1g_d)
        ld("n2g", n2g, n2g_d)
        E("act", lambda e: e.activation(out=scv, in_=cv, func=AF.Silu), r=["cv"], w=["scv"])
        for j in range(96):
            wt = wts[j % 2]
            wn = "wt%d" % (j % 2)
            ld(wn, wt, w_ada[j])
            for kc in range(16):
                E("pe", lambda e, wt=wt, kc=kc, j=j: e.matmul(bank(0, 2, 2 * j), lhsT=wt[:, kc * 128:(kc + 1) * 128],
                                                             rhs=scv[:, 2 * kc:2 * kc + 2], start=(kc == 0), stop=(kc == 15)),
                  r=[wn, "scv"], w=pb(0))
        m3 = modv.rearrange("p (j n) -> p j n", n=2)
        E("dve", lambda e: e.tensor_tensor(out=m3, in0=bank(0, 192).rearrange("p (j n) -> p j n", n=2),
                                           in1=bl.unsqueeze(2).to_broadcast([128, 96, 2]), op=ALU.add),
          r=pb(0) + ["bl"], w=["modv"])

        def mcol(ch, n):
            return m3[:, ch * 16:(ch + 1) * 16, n]

        E("dve", lambda e: e.scalar_tensor_tensor(out=a1, in0=mcol(1, 0), scalar=1.0, in1=n1g, op0=ALU.add, op1=ALU.mult),
          r=["modv", "n1g"], w=["a1"])
        E("dve", lambda e: e.scalar_tensor_tensor(out=a1c, in0=mcol(1, 1), scalar=1.0, in1=n1g, op0=ALU.add, op1=ALU.mult),
          r=["modv", "n1g"], w=["a1c"])
        E("dve", lambda e: e.tensor_copy(out=sh1, in_=mcol(0, 0)), r=["modv"], w=["sh1"])
        E("dve", lambda e: e.tensor_copy(out=sh1c, in_=mcol(0, 1)), r=["modv"], w=["sh1c"])
        E("dve", lambda e: e.scalar_tensor_tensor(out=V4[:, 0:16], in0=mcol(4, 0), scalar=1.0, in1=n2g, op0=ALU.add, op1=ALU.mult),
          r=["modv", "n2g"], w=["V4"])
        E("dve", lambda e: e.tensor_copy(out=V4[:, 16:32], in_=mcol(3, 0)), r=["modv"], w=["V4"])
        E("dve", lambda e: e.tensor_copy(out=V4[:, 32:48], in_=mcol(2, 0)), r=["modv"], w=["V4"])
        E("dve", lambda e: e.tensor_copy(out=V4[:, 48:64], in_=mcol(5, 0)), r=["modv"], w=["V4"])
        E("pe", lambda e: e.transpose(out=bank(1, 128)[0:64, :], in_=V4, identity=ident), r=["V4", "ident"], w=pb(1))
        E("dve", lambda e: e.tensor_copy(out=V4T[0:64, :], in_=bank(1, 128)[0:64, :]), r=pb(1), w=["V4T"])
        E("sp", lambda e: e.dma_start(out=S_vec, in_=V4T[0:64, :]), r=["V4T"], w=["S_vec"], dma=True)
        P.barrier()
        A.off = base_off
        if stop_after == "P0":
            return _finish(nc, P, out_d, E, A, st)

        def inproj(tag, xd, L, acol, shcol, groups, dest, precast=False):
            T = min(L, TB)
            mark = A.off
            xts = [A.f32(2048), A.f32(2048)]
            junk = A.bf16(2048)
            xs = A.f32(2048)
            hT = A.bf16(16 * T).rearrange("p (k t) -> p k t", k=16)
            w32s = [A.f32(2048), A.f32(2048)]
            wbs = [A.bf16(2048), A.bf16(2048)]
            stg = [A.f32(T), A.f32(T)]
            ss = A.f32(4)
            NS = min(512, T)
            if precast:
                pc32 = [A.f32(2048), A.f32(2048)]
                pcb = [A.bf16(2048), A.bf16(2048)]
            pci = [0]

            def precast_step():
                i = pci[0]
                if (not precast) or i >= 256:
                    return
                pci[0] += 1
                tbl, dst, dn = (pu_d, S_uv[:, 0:D], "S_uv") if i < 128 else (pv_d, S_uv[:, D:2 * D], "S_uv")
                r0 = (i % 128) * 128
                a32, ab = pc32[i % 2], pcb[i % 2]
                n32_, nb_ = "pc32_%d" % (i % 2), "pcb_%d" % (i % 2)
                ld(n32_, a32, tbl[r0:r0 + 128, :])
                E("pool", lambda e: e.tensor_copy(out=ab, in_=a32), r=[n32_], w=[nb_])
                stq(dn, dst[r0:r0 + 128, :], nb_, ab)
            for blk in range(L // T):
                t0 = blk * T
                for tt in range(T // 128):
                    xt = xts[tt % 2]
                    xn = "xt%d" % (tt % 2)
                    ld(xn, xt, xd[t0 + tt * 128:t0 + (tt + 1) * 128, :])
                    E("act", lambda e, xt=xt: e.activation(out=junk, in_=xt, func=AF.Square, accum_out=ss[:, 0:1]),
                      r=[xn], w=["junk", "ss"])
                    E("dve", lambda e: e.tensor_scalar(out=ss[:, 1:2], in0=ss[:, 0:1], scalar1=1.0 / D, scalar2=EPS,
                                                       op0=ALU.mult, op1=ALU.add), r=["ss"], w=["ss1"])
                    E("act", lambda e: e.activation(out=ss[:, 2:3], in_=ss[:, 1:2], func=AF.Sqrt), r=["ss1"], w=["ss2"])
                    E("dve", lambda e: e.reciprocal(out=ss[:, 3:4], in_=ss[:, 2:3]), r=["ss2"], w=["ss3"])
                    E("act", lambda e, xt=xt: e.activation(out=xs, in_=xt, func=AF.Copy, scale=ss[:, 3:4]),
                      r=[xn, "ss3"], w=["xs"])
                    for dc in range(16):
                        E("pe", lambda e, dc=dc: e.transpose(out=bank(dc // 4, 128, (dc % 4) * 128),
                                                             in_=xs[:, dc * 128:(dc + 1) * 128], identity=ident),
                          r=["xs", "ident"], w=pb(dc // 4))
                    for dc in range(16):
                        E("dve", lambda e, dc=dc, tt=tt: e.tensor_scalar(
                            out=hT[:, dc, tt * 128:(tt + 1) * 128], in0=bank(dc // 4, 128, (dc % 4) * 128),
                            scalar1=acol[:, dc:dc + 1], scalar2=shcol[:, dc:dc + 1], op0=ALU.mult, op1=ALU.add),
                          r=pb(dc // 4) + [tag + "a", tag + "s"], w=["hT"])
                for gi, g in enumerate(groups):
                    w32 = w32s[gi % 2]
                    wb = wbs[gi % 2]
                    sg = stg[gi % 2]
                    n32, nb, nsg = "w32_%d" % (gi % 2), "wb_%d" % (gi % 2), "stg_%d" % (gi % 2)
                    ld(n32, w32, w_in[g])
                    E("act", lambda e, wb=wb, w32=w32: e.activation(out=wb, in_=w32, func=AF.Copy), r=[n32], w=[nb])
                    pbase = 4 * (gi % 2)
                    nsl = T // NS
                    for ns in range(nsl):
                        for kc in range(16):
                            E("pe", lambda e, wb=wb, ns=ns, kc=kc, pbase=pbase: e.matmul(
                                bank(pbase + ns, NS), lhsT=wb[:, kc * 128:(kc + 1) * 128],
                                rhs=hT[:, kc, ns * NS:(ns + 1) * NS], start=(kc == 0), stop=(kc == 15)),
                              r=[nb, "hT"], w=pb(pbase + ns))
                    E("dve", lambda e, sg=sg, pbase=pbase: e.tensor_copy(out=sg, in_=PS[:, pbase * 512:pbase * 512 + T]),
                      r=pb(*range(pbase, pbase + nsl)), w=[nsg])
                    dname, dap = dest(g, t0, T)
                    stq(dname, dap, nsg, sg)
                    precast_step()
            while precast and pci[0] < 256:
                precast_step()
            P.barrier()
            A.off = mark

        def dest_lat(off):
            def f(g, t0, T):
                if g < 49:
                    return "S_a", S_a[g * 128:(g + 1) * 128, 2 + off + t0:2 + off + t0 + T]
                return "S_b", S_b[(g - 49) * 128:(g - 48) * 128, t0:t0 + T]
            return f

        def dest_ctx(g, t0, T):
            return "S_ac", S_ac[g * 128:(g + 1) * 128, 2 + t0:2 + t0 + T]

        for nm, src in (("ca", "a1c"), ("cs", "sh1c"), ("la", "a1"), ("ls", "sh1")):
            pass
        inproj("c", xc, Lc, a1c, sh1c, list(range(40)) + [48], dest_ctx)
        inproj("l", xa, La, a1, sh1, list(range(49)), dest_lat(Lm))
        inproj("l", xm, Lm, a1, sh1, list(range(NG_IN)), dest_lat(0), precast=True)
        P.barrier()
        if stop_after == "P1":
            return _finish(nc, P, out_d, E, A, st)

        def conv_set(src, base, L, row0, ccs, feat):
            mark = A.off
            SPN = min(CSP, L)
            pres = [A.f32(SPN + 4), A.f32(SPN + 4)]
            acc = A.f32(SPN)
            ress = [A.f32(SPN), A.f32(SPN)]
            tms = [A.f32(SPN), A.f32(SPN)]
            it = 0
            for cc in ccs:
                for s0 in range(0, L, SPN):
                    n = SPN
                    pre, res, tm = pres[it % 2], ress[it % 2], tms[it % 2]
                    npre, nres, ntm = "pre%d" % (it % 2), "res%d" % (it % 2), "tm%d" % (it % 2)
                    it += 1
                    ld(npre, pre, src[cc * 128:(cc + 1) * 128, base + s0:base + s0 + n + 4])
                    E("dve", lambda e, pre=pre, cc=cc: e.tensor_scalar(out=acc, in0=pre[:, 0:n], scalar1=convw[:, cc * 5:cc * 5 + 1],
                                                                       scalar2=None, op0=ALU.mult), r=[npre, "convw"], w=["acc"])
                    for k in range(1, 5):
                        E("dve", lambda e, pre=pre, cc=cc, k=k: e.scalar_tensor_tensor(
                            out=acc, in0=pre[:, k:k + n], scalar=convw[:, cc * 5 + k:cc * 5 + k + 1], in1=acc,
                            op0=ALU.mult, op1=ALU.add), r=[npre, "convw", "acc"], w=["acc"])
                    E("act", lambda e, res=res, cc=cc: e.activation(out=res, in_=acc, func=AF.Silu, bias=convb[:, cc:cc + 1]),
                      r=["acc", "convb"], w=[nres])
                    if cc < 40:
                        J = n // 128
                        for j in range(J):
                            E("pe", lambda e, res=res, j=j: e.transpose(out=PS[:, j * 128:(j + 1) * 128],
                                                                        in_=res[:, j * 128:(j + 1) * 128], identity=ident),
                              r=[nres, "ident"], w=pb(j // 4))
                        E("act", lambda e, tm=tm: e.activation(out=tm, in_=PS[:, 0:n], func=AF.Copy),
                          r=pb(*range((J + 3) // 4)), w=[ntm])
                        if cc < 32:
                            dst = S_xt[row0 + s0:row0 + s0 + n, cc * 128:(cc + 1) * 128]
                            dn = "S_xt"
                        else:
                            dst = S_bt[row0 + s0:row0 + s0 + n, (cc - 32) * 128:(cc - 31) * 128]
                            dn = "S_bt"
                        stq(dn, dst.rearrange("(j p) c -> p j c", p=128), ntm, tm.rearrange("p (j c) -> p j c", c=128))
                    if feat and cc >= 32:
                        if cc < 40:
                            stq("S_bT", S_bT[(cc - 32) * 128:(cc - 31) * 128, s0:s0 + n], nres, res)
                        else:
                            stq("S_cT", S_cT[(cc - 40) * 128:(cc - 39) * 128, s0:s0 + n], nres, res)
            P.barrier()
            A.off = mark

        conv_set(S_ac, 0, Lc, Ltot, list(range(40)), False)
        conv_set(S_a, Lm, La, Lm, list(range(40)), False)
        conv_set(S_a, 0, Lm, 0, list(range(48)), True)

        def dt_set(src, base, L, cid0):
            mark = A.off
            SPN = min(2048, L)
            pd = A.f32(SPN)
            xb = A.f32(SPN)
            t1 = A.f32(SPN)
            dtv = A.f32(SPN)
            av = A.f32(SPN)
            ac = A.f32(SPN)
            tmpb = A.f32(128)
            totc = A.f32(16)
            tmA = A.f32(SPN)
            tmD = A.f32(SPN)
            totT = A.f32(128)
            for s0 in range(0, L, SPN):
                n = SPN
                ncn = n // 128
                ld("pd", pd, src[48 * 128:49 * 128, 2 + base + s0:2 + base + s0 + n])
                E("act", lambda e: e.activation(out=xb, in_=pd, func=AF.Identity, bias=dtb[:, 0:1]), r=["pd", "dtb"], w=["xb"])
                E("dve", lambda e: e.scalar_tensor_tensor(out=t1, in0=xb, scalar=-1.0, in1=xb, op0=ALU.mult, op1=ALU.max), r=["xb"], w=["t1"])
                E("act", lambda e: e.activation(out=t1, in_=t1, func=AF.Exp, scale=-1.0), r=["t1"], w=["t1"])
                E("act", lambda e: e.activation(out=t1, in_=t1, func=AF.Ln, bias=1.0), r=["t1"], w=["t1"])
                E("dve", lambda e: e.scalar_tensor_tensor(out=dtv, in0=xb, scalar=0.0, in1=t1, op0=ALU.max, op1=ALU.add),
                  r=["xb", "t1"], w=["dtv"])
                E("dve", lambda e: e.tensor_scalar(out=av, in0=dtv, scalar1=acol_A[:, 0:1], scalar2=None, op0=ALU.mult),
                  r=["dtv", "acolA"], w=["av"])
                E("dve", lambda e: e.tensor_reduce(out=totc[:, 0:ncn], in_=av.rearrange("p (c t) -> p c t", t=128),
                                                   axis=AX.X, op=ALU.add), r=["av"], w=["totc"])
                for c in range(ncn):
                    sl = slice(c * 128, (c + 1) * 128)
                    E("dve", lambda e, sl=sl: e.tensor_tensor_scan(out=ac[:, sl], data0=ones, data1=av[:, sl], initial=0.0,
                                                                   op0=ALU.mult, op1=ALU.add), r=["ones", "av", "ac"], w=["ac"])
                    E("dve", lambda e, sl=sl, c=c: e.tensor_scalar(out=tmpb[64:128, :], in0=ac[64:128, sl], scalar1=-1.0,
                                                                   scalar2=totc[64:128, c:c + 1], op0=ALU.mult, op1=ALU.add),
                      r=["ac", "totc"], w=["tmpb"])
                    E("dve", lambda e, sl=sl: e.tensor_tensor(out=ac[64:128, sl], in0=tmpb[64:128, :], in1=av[64:128, sl], op=ALU.add),
                      r=["tmpb", "av", "ac"], w=["ac"])
                stq("S_acc", S_acc[cid0 + s0 // 128:cid0 + s0 // 128 + ncn, :].rearrange("c (p t) -> p c t", p=128),
                    "ac", ac.rearrange("p (c t) -> p c t", t=128), eng="sp")
                for c in range(ncn):
                    E("pe", lambda e, c=c: e.transpose(out=PS[:, c * 128:(c + 1) * 128], in_=ac[:, c * 128:(c + 1) * 128], identity=ident),
                      r=["ac", "ident"], w=pb(c // 4))
                    E("pe", lambda e, c=c: e.transpose(out=PS[:, 2048 + c * 128:2048 + (c + 1) * 128], in_=dtv[:, c * 128:(c + 1) * 128],
                                                       identity=ident), r=["dtv", "ident"], w=pb(4 + c // 4))
                nb4 = (ncn + 3) // 4
                E("act", lambda e: e.activation(out=tmA, in_=PS[:, 0:n], func=AF.Copy), r=pb(*range(nb4)), w=["tmA"])
                E("act", lambda e: e.activation(out=tmD, in_=PS[:, 2048:2048 + n], func=AF.Copy), r=pb(*range(4, 4 + nb4)), w=["tmD"])
                c0 = cid0 + s0 // 128
                stq("S_act", S_act[c0:c0 + ncn].rearrange("c p k -> p c k"), "tmA", tmA.rearrange("p (c k) -> p c k", k=128), eng="sp")
                stq("S_dtt", S_dtt[c0:c0 + ncn].rearrange("c p k -> p c k"), "tmD", tmD.rearrange("p (c k) -> p c k", k=128), eng="sp")
                E("pe", lambda e: e.transpose(out=bank(3, 128)[0:ncn, :], in_=totc[:, 0:ncn], identity=ident),
                  r=["totc", "ident"], w=pb(3))
                E("dve", lambda e: e.tensor_copy(out=totT[0:ncn, :], in_=bank(3, 128)[0:ncn, :]), r=pb(3), w=["totT"])
                stq("S_tot", S_tot[c0:c0 + ncn, :], "totT", totT[0:ncn, :], eng="sp")
            P.barrier()
            A.off = mark

        dt_set(S_a, 0, Ltot, 0)
        dt_set(S_ac, 0, Lc, NM + NA)
        P.barrier()
        if stop_after == "P1b":
            return _finish(nc, P, out_d, E, A, st)

        mark2 = A.off
        hTst = [A.f32(DSSD), A.f32(DSSD)]
        xtoks = [A.f32(DSSD), A.f32(DSSD)]
        btoks = [A.f32(1024), A.f32(1024)]
        btokb = A.bf16(1024)
        bTfs = [A.f32(1024), A.f32(1024)]
        cTfs = [A.f32(1024), A.f32(1024)]
        bTb = A.bf16(1024).rearrange("p (g t) -> p g t", g=8)
        cTb = A.bf16(1024).rearrange("p (g t) -> p g t", g=8)
        dtcs = [A.f32(64), A.f32(64)]
        acls = [A.f32(64), A.f32(64)]
        totbs = [A.f32(128), A.f32(128)]
        chunk_ctr = [0]
        decend = A.f32(64)
        wcol = A.f32(64)
        ein = A.f32(64)
        declast = A.f32(64)
        xdt = A.bf16(DSSD)
        xdd = A.bf16(DSSD)
        bcgs = [A.f32(1024), A.f32(1024)]
        segs = [A.f32(1024), A.f32(1024)]
        Egs = [A.f32(1024), A.f32(1024)]
        wTs = [A.bf16(1024).rearrange("p (r t) -> p r t", r=8), A.bf16(1024).rearrange("p (r t) -> p r t", r=8)]
        cbms = [A.f32(128), A.f32(128)]
        hTbf = A.bf16(DSSD)
        yacc = A.f32(DSSD)
        tmps = [A.f32(512), A.f32(512)]
        zTs = [A.f32(1024), A.f32(1024)]
        sz = A.f32(1024)
        ynT = A.bf16(DSSD).rearrange("p (f t) -> p f t", f=32)
        ssq = A.f32(8)

        E("dve", lambda e: e.memset(hTst[0], 0.0), w=["hT0"])
        E("dve", lambda e: e.memset(hTst[1], 0.0), w=["hT1"])

        def ssd_chunk(cid, row0, dr, need_y, fcol0, final):
            hT = hTst[dr]
            hn = "hT%d" % dr
            mask = maskU if dr == 0 else maskL
            mname = "maskU" if dr == 0 else "maskL"
            cp = chunk_ctr[0] % 2
            chunk_ctr[0] += 1
            xtok, btok, bTf, cTf, dtc, acl, totb = xtoks[cp], btoks[cp], bTfs[cp], cTfs[cp], dtcs[cp], acls[cp], totbs[cp]
            nxt, nbtk, nbTf, ncTf, ndtc, nacl, ntotb = ["%s%d" % (n_, cp) for n_ in ("xtok", "btok", "bTf", "cTf", "dtc", "acl", "totb")]
            ld(nxt, xtok, S_xt[row0:row0 + 128, :])
            ld(nbtk, btok, S_bt[row0:row0 + 128, :])
            ld(ndtc, dtc, S_dtt[cid][:, dr * 64:(dr + 1) * 64])
            ld(nacl, acl, S_act[cid][:, dr * 64:(dr + 1) * 64])
            ld(ntotb, totb, S_tot[cid:cid + 1, :].partition_broadcast(128))
            if need_y:
                ld(nbTf, bTf.rearrange("p (g t) -> p g t", g=8),
                   S_bT[:, fcol0:fcol0 + 128].rearrange("(g p) t -> p g t", p=128))
                ld(ncTf, cTf.rearrange("p (g t) -> p g t", g=8),
                   S_cT[:, fcol0:fcol0 + 128].rearrange("(g p) t -> p g t", p=128))
            td = totb[:, dr * 64:(dr + 1) * 64]
            E("dve", lambda e: e.tensor_tensor(out=decend, in0=td, in1=acl, op=ALU.subtract), r=[ntotb, nacl], w=["decend"])
            E("act", lambda e: e.activation(out=decend, in_=decend, func=AF.Exp), r=["decend"], w=["decend"])
            E("dve", lambda e: e.tensor_tensor(out=wcol, in0=dtc, in1=decend, op=ALU.mult), r=[ndtc, "decend"], w=["wcol"])
            E("act", lambda e: e.activation(out=declast, in_=td, func=AF.Exp), r=[ntotb], w=["declast"])
            x3 = xtok.rearrange("p (h d) -> p h d", d=64)
            E("dve", lambda e: e.tensor_tensor(out=xdd.rearrange("p (h d) -> p h d", d=64), in0=x3,
                                               in1=wcol.unsqueeze(2).to_broadcast([128, 64, 64]), op=ALU.mult),
              r=[nxt, "wcol"], w=["xdd"])
            E("act", lambda e: e.activation(out=btokb, in_=btok, func=AF.Copy), r=[nbtk], w=["btokb"])
            if need_y:
                E("act", lambda e: e.activation(out=bTb, in_=bTf.rearrange("p (g t) -> p g t", g=8), func=AF.Copy), r=[nbTf], w=["bTb"])
                E("act", lambda e: e.activation(out=cTb, in_=cTf.rearrange("p (g t) -> p g t", g=8), func=AF.Copy), r=[ncTf], w=["cTb"])
                E("act", lambda e: e.activation(out=ein, in_=acl, func=AF.Exp), r=[nacl], w=["ein"])
                E("pool", lambda e: e.tensor_tensor(out=xdt.rearrange("p (h d) -> p h d", d=64), in0=x3,
                                                    in1=dtc.unsqueeze(2).to_broadcast([128, 64, 64]), op=ALU.mult),
                  r=[nxt, ndtc], w=["xdt"])
                E("act", lambda e: e.activation(out=hTbf, in_=hT, func=AF.Copy), r=[hn], w=["hTbf"])
                if final:
                    ld("yacc", yacc, S_yb[row0:row0 + 128, :], r=["S_yb"])
                else:
                    E("dve", lambda e: e.tensor_tensor(out=yacc.rearrange("p (h d) -> p h d", d=64), in0=x3,
                                                       in1=Dbc.unsqueeze(2).to_broadcast([128, 64, 64]), op=ALU.mult),
                      r=[nxt, "Dbc"], w=["yacc"])

            def stage_a(g):
                gp = g % 2
                pb0 = 4 * gp
                seg, Eg, cbm, bcg = segs[gp], Egs[gp], cbms[gp], bcgs[gp]
                sgn, egn, cbn, bn = "seg%d" % gp, "Eg%d" % gp, "cbm%d" % gp, "bcg%d" % gp
                o0 = (dr * 64 + g * 8) * 128
                ld(bn, bcg, S_acc[cid:cid + 1, o0:o0 + 1024].partition_broadcast(128))
                E("pe", lambda e: e.matmul(bank(pb0, 128), lhsT=bTb[:, g, :], rhs=cTb[:, g, :], start=True, stop=True),
                  r=["bTb", "cTb"], w=pb(pb0))
                E("dve", lambda e: e.tensor_tensor(out=cbm, in0=bank(pb0, 128), in1=mask, op=ALU.mult), r=pb(pb0) + [mname], w=[cbn])
                for r_ in range(8):
                    hh = g * 8 + r_
                    E("dve", lambda e, r_=r_, hh=hh: e.tensor_scalar(
                        out=seg[:, r_ * 128:(r_ + 1) * 128], in0=bcg[:, r_ * 128:(r_ + 1) * 128],
                        scalar1=acl[:, hh:hh + 1], scalar2=0.0, op0=ALU.subtract, op1=ALU.min),
                      r=[bn, nacl], w=[sgn])
                E("act", lambda e: e.activation(out=Eg, in_=seg, func=AF.Exp), r=[sgn], w=[egn])

            def stage_b(g):
                gp = g % 2
                pb0 = 4 * gp
                gs = slice(g * 512, (g + 1) * 512)
                Eg, cbm, wT, tmp = Egs[gp], cbms[gp], wTs[gp], tmps[gp]
                egn, cbn, wtn, tn = "Eg%d" % gp, "cbm%d" % gp, "wT%d" % gp, "tmp%d" % gp
                if need_y:
                    E("dve", lambda e: e.tensor_tensor(out=wT, in0=Eg.rearrange("p (r t) -> p r t", r=8),
                                                       in1=cbm.unsqueeze(1).to_broadcast([128, 8, 128]), op=ALU.mult),
                      r=[egn, cbn], w=[wtn])
                    for r_ in range(8):
                        hh = g * 8 + r_
                        E("pe", lambda e, r_=r_, hh=hh: e.matmul(bank(pb0 + 1, 64, r_ * 64), lhsT=wT[:, r_, :],
                                                                 rhs=xdt[:, hh * 64:(hh + 1) * 64], start=True, stop=True),
                          r=[wtn, "xdt"], w=pb(pb0 + 1))
                    E("pe", lambda e: e.matmul(bank(pb0 + 2), lhsT=cTb[:, g, :], rhs=hTbf[:, gs], start=True, stop=True),
                      r=["cTb", "hTbf"], w=pb(pb0 + 2))
                    E("dve", lambda e: e.tensor_tensor(
                        out=tmp.rearrange("p (r d) -> p r d", d=64), in0=bank(pb0 + 2).rearrange("p (r d) -> p r d", d=64),
                        in1=ein[:, g * 8:(g + 1) * 8].unsqueeze(2).to_broadcast([128, 8, 64]), op=ALU.mult),
                      r=pb(pb0 + 2) + ["ein"], w=[tn])
                    E("pool", lambda e: e.tensor_tensor(out=yacc[:, gs], in0=yacc[:, gs], in1=tmp, op=ALU.add),
                      r=[tn, "yacc"], w=["yacc"])
                    E("dve", lambda e: e.tensor_tensor(out=yacc[:, gs], in0=yacc[:, gs], in1=bank(pb0 + 1), op=ALU.add),
                      r=pb(pb0 + 1) + ["yacc"], w=["yacc"])
                E("pe", lambda e: e.matmul(bank(pb0 + 3), lhsT=btokb[:, g * 128:(g + 1) * 128], rhs=xdd[:, gs], start=True, stop=True),
                  r=["btokb", "xdd"], w=pb(pb0 + 3))
                E("pool", lambda e: e.tensor_tensor(
                    out=hT[:, gs].rearrange("p (r d) -> p r d", d=64), in0=hT[:, gs].rearrange("p (r d) -> p r d", d=64),
                    in1=declast[:, g * 8:(g + 1) * 8].unsqueeze(2).to_broadcast([128, 8, 64]), op=ALU.mult),
                  r=[hn, "declast", "hTbf"], w=[hn])
                E("dve", lambda e: e.tensor_tensor(out=hT[:, gs], in0=hT[:, gs], in1=bank(pb0 + 3), op=ALU.add),
                  r=pb(pb0 + 3) + [hn], w=[hn])

            if need_y:
                stage_a(0)
            for g in range(8):
                if need_y and g + 1 < 8:
                    stage_a(g + 1)
                stage_b(g)
            if need_y and not final:
                stq("S_yb", S_yb[row0:row0 + 128, :], "yacc", yacc, eng="sp")
            if final:
                for q in range(4):
                    zT = zTs[q % 2]
                    zn = "zT%d" % (q % 2)
                    ld(zn, zT.rearrange("p (f t) -> p f t", f=8),
                       S_b[q * 1024:(q + 1) * 1024, fcol0:fcol0 + 128].rearrange("(f p) t -> p f t", p=128))
                    for f in range(8):
                        E("pe", lambda e, f=f, zT=zT: e.transpose(out=PS[:, 2048 + f * 128:2048 + (f + 1) * 128],
                                                                  in_=zT[:, f * 128:(f + 1) * 128], identity=ident),
                          r=[zn, "ident"], w=pb(4 + f // 4))
                    E("act", lambda e: e.activation(out=sz, in_=PS[:, 2048:3072], func=AF.Silu), r=pb(4, 5), w=["sz"])
                    qs = slice(q * 1024, (q + 1) * 1024)
                    E("dve", lambda e, qs=qs: e.tensor_tensor(out=yacc[:, qs], in0=yacc[:, qs], in1=sz, op=ALU.mult),
                      r=["sz", "yacc"], w=["yacc"])
                    E("act", lambda e, qs=qs, q=q: e.activation(out=sz, in_=yacc[:, qs], func=AF.Square, accum_out=ssq[:, q:q + 1]),
                      r=["yacc", "sz"], w=["sz", "ssq"])
                E("dve", lambda e: e.tensor_reduce(out=ssq[:, 4:5], in_=ssq[:, 0:4], axis=AX.X, op=ALU.add), r=["ssq"], w=["ssq"])
                E("dve", lambda e: e.tensor_scalar(out=ssq[:, 5:6], in0=ssq[:, 4:5], scalar1=1.0 / DSSD, scalar2=EPS,
                                                   op0=ALU.mult, op1=ALU.add), r=["ssq"], w=["ssq"])
                E("act", lambda e: e.activation(out=ssq[:, 6:7], in_=ssq[:, 5:6], func=AF.Sqrt), r=["ssq"], w=["ssq"])
                E("dve", lambda e: e.reciprocal(out=ssq[:, 7:8], in_=ssq[:, 6:7]), r=["ssq"], w=["ssq"])
                E("act", lambda e: e.activation(out=yacc, in_=yacc, func=AF.Copy, scale=ssq[:, 7:8]), r=["yacc", "ssq"], w=["yacc"])
                for half in range(2):
                    for f in range(16):
                        ff = half * 16 + f
                        E("pe", lambda e, f=f, ff=ff: e.transpose(out=PS[:, 2048 + f * 128:2048 + (f + 1) * 128],
                                                                  in_=yacc[:, ff * 128:(ff + 1) * 128], identity=ident),
                          r=["yacc", "ident"], w=pb(4 + f // 4))
                    E("act", lambda e, half=half: e.activation(out=ynT[:, half * 16:(half + 1) * 16, :],
                                                               in_=PS[:, 2048:4096].rearrange("p (f t) -> p f t", f=16), func=AF.Copy),
                      r=pb(4, 5, 6, 7), w=["ynT"])
                stq("S_yn", S_yn[:, fcol0:fcol0 + 128].rearrange("(f p) t -> p f t", p=128), "ynT", ynT, eng="sp")

        for c in range(NC_):
            ssd_chunk(NM + NA + c, Ltot + c * 128, 0, False, 0, False)
        for c in reversed(range(NC_)):
            ssd_chunk(NM + NA + c, Ltot + c * 128, 1, False, 0, False)
        for c in reversed(range(NA)):
            ssd_chunk(NM + c, Lm + c * 128, 1, False, 0, False)
        for c in reversed(range(NM)):
            ssd_chunk(c, c * 128, 1, True, c * 128, False)
        for c in range(NM):
            ssd_chunk(c, c * 128, 0, True, c * 128, True)
        if "S_hst" in dbg:
            stq("S_hst", S_hst[0], "hT0", hTst[0], eng="sp")
            stq("S_hst", S_hst[1], "hT1", hTst[1], eng="sp")
        P.barrier()
        A.off = mark2
        if stop_after == "P2":
            return _finish(nc, P, out_d, E, A, st)

        mark3 = A.off
        vT = A.bf16(16 * T3).rearrange("p (k t) -> p k t", k=16)
        ynB = A.bf16(32 * T3).rearrange("p (k t) -> p k t", k=32)
        mrgT = A.bf16(16 * T3).rearrange("p (k t) -> p k t", k=16)
        oTs = A.f32(16 * T3).rearrange("p (k t) -> p k t", k=16)
        wA32_1 = A.f32(4096)
        wA32 = [wA32_1, wA32_1]
        wAb = [A.bf16(4096), A.bf16(4096)]
        wB32 = [A.f32(2048), A.f32(2048)]
        wBb = [A.bf16(2048), A.bf16(2048)]
        tb_ = [A.f32(T3)] * 2
        tc_ = [A.f32(T3)] * 2
        tx_ = [A.f32(T3)] * 2
        uu = A.f32(T3)
        cu = A.f32(T3)
        gsl = [A.f32(T3), A.f32(T3)]
        gcl = [A.f32(T3), A.f32(T3)]
        m1 = A.f32(T3)
        m2 = A.f32(T3)
        xt3 = A.f32(2048)
        tmp3 = A.f32(2048)
        g1bc = A.f32(2048)
        ld("g1bc", g1bc, S_vec[32:48, :].rearrange("(o a) b -> o (a b)", o=1).partition_broadcast(128))
        R = T3 // 64
        for blk in range(Lm // T3):
            t0 = blk * T3
            ld("ynB", ynB, S_yn[:, t0:t0 + T3].rearrange("(f p) t -> p f t", p=128))
            for dc in range(16):
                tb, tc, tx = tb_[dc % 2], tc_[dc % 2], tx_[dc % 2]
                nbn, ncn2, nxn = "tb0", "tc0", "tx0"
                ld(nbn, tb, S_b[(32 + dc) * 128:(33 + dc) * 128, t0:t0 + T3])
                ld(ncn2, tc, S_b[(48 + dc) * 128:(49 + dc) * 128, t0:t0 + T3])
                ld(nxn, tx, S_b[(64 + dc) * 128:(65 + dc) * 128, t0:t0 + T3])
                E("dve", lambda e, tc=tc, tx=tx: e.tensor_tensor(out=uu, in0=tc, in1=tx, op=ALU.mult), r=[ncn2, nxn], w=["uu"])
                E("dve", lambda e, dc=dc: e.tensor_scalar(out=cu, in0=uu, scalar1=scw[:, dc * 3 + 1:dc * 3 + 2], scalar2=None, op0=ALU.mult),
                  r=["uu", "scw"], w=["cu"])
                u3 = uu.rearrange("p (r w) -> p r w", w=64)
                c3 = cu.rearrange("p (r w) -> p r w", w=64)
                E("dve", lambda e, dc=dc: e.scalar_tensor_tensor(out=c3[:, :, 1:64], in0=u3[:, :, 0:63], scalar=scw[:, dc * 3:dc * 3 + 1],
                                                                 in1=c3[:, :, 1:64], op0=ALU.mult, op1=ALU.add),
                  r=["uu", "scw", "cu"], w=["cu"])
                E("dve", lambda e, dc=dc: e.scalar_tensor_tensor(out=c3[:, :, 0:63], in0=u3[:, :, 1:64], scalar=scw[:, dc * 3 + 2:dc * 3 + 3],
                                                                 in1=c3[:, :, 0:63], op0=ALU.mult, op1=ALU.add),
                  r=["uu", "scw", "cu"], w=["cu"])
                E("dve", lambda e, dc=dc, tb=tb: e.tensor_tensor(out=vT[:, dc, :], in0=tb, in1=cu, op=ALU.mult), r=[nbn, "cu"], w=["vT"])
            for oc in range(16):
                wa, wab, wbt, wbb = wA32[oc % 2], wAb[oc % 2], wB32[oc % 2], wBb[oc % 2]
                na, nab, nbt, nbb = "wA0", "wAb%d" % (oc % 2), "wB%d" % (oc % 2), "wBb%d" % (oc % 2)
                ld(na, wa, wssd_d[oc])
                ld(nbt, wbt, wsc_d[oc])
                E("dve", lambda e, wa=wa, wab=wab: e.tensor_tensor(out=wab.rearrange("p (k m) -> p k m", k=32),
                                                                   in0=wa.rearrange("p (k m) -> p k m", k=32),
                                                                   in1=gncol.unsqueeze(2).to_broadcast([128, 32, 128]), op=ALU.mult),
                  r=[na, "gncol"], w=[nab])
                E("act", lambda e, wbt=wbt, wbb=wbb: e.activation(out=wbb, in_=wbt, func=AF.Copy), r=[nbt], w=[nbb])
                for kc in range(32):
                    E("pe", lambda e, wab=wab, kc=kc: e.matmul(bank(0, T3), lhsT=wab[:, kc * 128:(kc + 1) * 128], rhs=ynB[:, kc, :],
                                                               start=(kc == 0), stop=(kc == 31)), r=[nab, "ynB"], w=pb(0))
                for kc in range(16):
                    E("pe", lambda e, wbb=wbb, kc=kc: e.matmul(bank(1, T3), lhsT=wbb[:, kc * 128:(kc + 1) * 128], rhs=vT[:, kc, :],
                                                               start=(kc == 0), stop=(kc == 15)), r=[nbb, "vT"], w=pb(1))
                gsv, gcv = gsl[oc % 2], gcl[oc % 2]
                ngs, ngc = "gs%d" % (oc % 2), "gc%d" % (oc % 2)
                ld(ngs, gsv, S_b[(80 + oc) * 128:(81 + oc) * 128, t0:t0 + T3])
                ld(ngc, gcv, S_b[(96 + oc) * 128:(97 + oc) * 128, t0:t0 + T3])
                E("act", lambda e, gsv=gsv: e.activation(out=gsv, in_=gsv, func=AF.Sigmoid), r=[ngs], w=[ngs])
                E("act", lambda e, gcv=gcv: e.activation(out=gcv, in_=gcv, func=AF.Sigmoid), r=[ngc], w=[ngc])
                E("dve", lambda e, gsv=gsv: e.tensor_tensor(out=m1, in0=bank(0, T3), in1=gsv, op=ALU.mult), r=pb(0) + [ngs], w=["m1"])
                E("dve", lambda e, gcv=gcv: e.tensor_tensor(out=m2, in0=bank(1, T3), in1=gcv, op=ALU.mult), r=pb(1) + [ngc], w=["m2"])
                E("pool", lambda e, oc=oc: e.tensor_tensor(out=mrgT[:, oc, :], in0=m1, in1=m2, op=ALU.add), r=["m1", "m2"], w=["mrgT"])
            for oc in range(16):
                wbt, wbb = wB32[oc % 2], wBb[oc % 2]
                nbt, nbb = "wB%d" % (oc % 2), "wBb%d" % (oc % 2)
                ld(nbt, wbt, wo_d[oc])
                E("act", lambda e, wbt=wbt, wbb=wbb: e.activation(out=wbb, in_=wbt, func=AF.Copy), r=[nbt], w=[nbb])
                for kc in range(16):
                    E("pe", lambda e, wbb=wbb, kc=kc, oc=oc: e.matmul(bank(2 + oc % 2, T3), lhsT=wbb[:, kc * 128:(kc + 1) * 128],
                                                                      rhs=mrgT[:, kc, :], start=(kc == 0), stop=(kc == 15)),
                      r=[nbb, "mrgT"], w=pb(2 + oc % 2))
                E("act", lambda e, oc=oc: e.activation(out=oTs[:, oc, :], in_=bank(2 + oc % 2, T3), func=AF.Copy),
                  r=pb(2 + oc % 2), w=["oTs"])
            for tt in range(T3 // 128):
                ld("xt3", xt3, xm[t0 + tt * 128:t0 + (tt + 1) * 128, :])
                for oc in range(16):
                    E("pe", lambda e, oc=oc, tt=tt: e.transpose(out=PS[:, 2048 + oc * 128:2048 + (oc + 1) * 128],
                                                                in_=oTs[:, oc, tt * 128:(tt + 1) * 128], identity=ident),
                      r=["oTs", "ident"], w=pb(4 + oc // 4))
                E("dve", lambda e: e.tensor_tensor(out=tmp3, in0=PS[:, 2048:4096], in1=g1bc, op=ALU.mult), r=pb(4, 5, 6, 7) + ["g1bc"], w=["tmp3"])
                E("pool", lambda e: e.tensor_tensor(out=tmp3, in0=tmp3, in1=xt3, op=ALU.add), r=["tmp3", "xt3"], w=["tmp3"])
                stq("S_x1", S_x1[t0 + tt * 128:t0 + (tt + 1) * 128, :], "tmp3", tmp3, eng="sp")
        P.barrier()
        A.off = mark3
        if stop_after == "P3":
            return _finish(nc, P, out_d, E, A, st)

        x1ts = [A.f32(2048), A.f32(2048)]
        h2f = A.f32(2048)
        h2bs = [A.bf16(2048), A.bf16(2048)]
        junk4 = A.bf16(2048)
        junkb = A.bf16(2048)
        h2T = A.bf16(2048).rearrange("p (k t) -> p k t", k=16)
        wq32 = [A.f32(2048), A.f32(2048)]
        wqb = [A.bf16(2048), A.bf16(2048)]
        qT = A.bf16(2048).rearrange("p (c t) -> p c t", c=16)
        k32 = A.f32(2048)
        keysb = A.bf16(2048).rearrange("p (c n) -> p c n", c=16)
        sc_ = A.f32(2048)
        wk = A.f32(256)
        sv = A.f32(256)
        si = A.u32(256)
        sif = A.f32(256)
        cs = A.f32(256)
        ci = A.f32(256)
        ts_ = A.f32(16)
        pos = A.u32(16)
        posf = A.f32(16)
        ohs = [A.f32(256), A.f32(256)]
        wv = A.f32(128)
        prodj = [A.f32(2048), A.f32(2048)]
        junka = A.bf16(2048)
        idxf = A.f32(128)
        idxus = [A.u32(128), A.u32(128)]
        gates = [A.f32(128), A.f32(128)]
        actv = A.f32(128)
        gact = A.f32(128)
        sm = A.f32(8)
        smf = A.f32(8)
        NB = 4
        UVg = [A.bf16(4096) for _ in range(NB)]
        dg = [A.bf16(128), A.bf16(128)]
        a2bc = A.f32(2048)
        sh2bc = A.f32(2048)
        g2bc = A.f32(2048)
        fgbc = A.f32(2048)
        x2 = A.f32(2048)
        ld("a2bc", a2bc, S_vec[0:16, :].rearrange("(o a) b -> o (a b)", o=1).partition_broadcast(128))
        ld("sh2bc", sh2bc, S_vec[16:32, :].rearrange("(o a) b -> o (a b)", o=1).partition_broadcast(128))
        ld("g2bc", g2bc, S_vec[48:64, :].rearrange("(o a) b -> o (a b)", o=1).partition_broadcast(128))
        ld("fgbc", fgbc, fg_d.partition_broadcast(128))
        ld("k32", k32, keys_d)
        E("act", lambda e: e.activation(out=keysb, in_=k32.rearrange("p (c n) -> p c n", c=16), func=AF.Copy), r=["k32"], w=["keysb"])

        def rstd_of(src_name, src, jnk, jn, smt, smn):
            E("act", lambda e: e.activation(out=jnk, in_=src, func=AF.Square, accum_out=smt[:, 0:1]), r=[src_name], w=[jn, smn])
            E("dve", lambda e: e.tensor_scalar(out=smt[:, 1:2], in0=smt[:, 0:1], scalar1=1.0 / D, scalar2=EPS, op0=ALU.mult, op1=ALU.add),
              r=[smn], w=[smn])
            E("act", lambda e: e.activation(out=smt[:, 2:3], in_=smt[:, 1:2], func=AF.Sqrt), r=[smn], w=[smn])
            E("dve", lambda e: e.reciprocal(out=smt[:, 3:4], in_=smt[:, 2:3]), r=[smn], w=[smn])

        NT = Lm // 128

        def peer_setup(tl):
            par = tl % 2
            t0 = tl * 128
            x1t, h2b, idxu, gate = x1ts[par], h2bs[par], idxus[par], gates[par]
            xn, hbn, iun, gtn = "x1t%d" % par, "h2b%d" % par, "idxu%d" % par, "gate%d" % par
            ld(xn, x1t, S_x1[t0:t0 + 128, :])
            rstd_of(xn, x1t, junk4, "junk4", sm, "sm")
            E("dve", lambda e: e.scalar_tensor_tensor(out=h2f, in0=x1t, scalar=sm[:, 3:4], in1=a2bc, op0=ALU.mult, op1=ALU.mult),
              r=[xn, "sm", "a2bc"], w=["h2f"])
            E("pool", lambda e: e.tensor_tensor(out=h2f, in0=h2f, in1=sh2bc, op=ALU.add), r=["h2f", "sh2bc"], w=["h2f"])
            E("act", lambda e: e.activation(out=h2b, in_=h2f, func=AF.Copy), r=["h2f"], w=[hbn])
            for dc in range(16):
                E("pe", lambda e, dc=dc: e.transpose(out=PS[:, dc * 128:(dc + 1) * 128], in_=h2f[:, dc * 128:(dc + 1) * 128], identity=ident),
                  r=["h2f", "ident"], w=pb(dc // 4))
            E("act", lambda e: e.activation(out=h2T, in_=PS[:, 0:2048].rearrange("p (k t) -> p k t", k=16), func=AF.Copy),
              r=pb(0, 1, 2, 3), w=["h2T"])
            for c in range(16):
                w32, wb = wq32[c % 2], wqb[c % 2]
                n32, nb = "wq%d" % (c % 2), "wqb%d" % (c % 2)
                ld(n32, w32, wq_d[c])
                E("act", lambda e, w32=w32, wb=wb: e.activation(out=wb, in_=w32, func=AF.Copy), r=[n32], w=[nb])
                for kc in range(16):
                    E("pe", lambda e, wb=wb, kc=kc, c=c: e.matmul(PS[:, c * 128:(c + 1) * 128], lhsT=wb[:, kc * 128:(kc + 1) * 128],
                                                                  rhs=h2T[:, kc, :], start=(kc == 0), stop=(kc == 15)),
                      r=[nb, "h2T"], w=pb(c // 4))
            E("act", lambda e: e.activation(out=qT, in_=PS[:, 0:2048].rearrange("p (c t) -> p c t", c=16), func=AF.Copy),
              r=pb(0, 1, 2, 3), w=["qT"])
            for c in range(16):
                E("pe", lambda e, c=c: e.matmul(PS[:, c * 128:(c + 1) * 128], lhsT=qT[:, c, :], rhs=keysb[:, c, :], start=True, stop=True),
                  r=["qT", "keysb"], w=pb(c // 4))
            E("act", lambda e: e.activation(out=sc_, in_=PS[:, 0:2048], func=AF.Copy), r=pb(0, 1, 2, 3), w=["sc"])
            for c in range(16):
                cs_ = slice(c * 128, (c + 1) * 128)
                v0 = slice(c * 16, c * 16 + 8)
                v1 = slice(c * 16 + 8, c * 16 + 16)
                E("dve", lambda e, cs_=cs_, v0=v0: e.max(out=sv[:, v0], in_=sc_[:, cs_]), r=["sc"], w=["sv"])
                E("dve", lambda e, cs_=cs_, v0=v0: e.max_index(out=si[:, v0], in_max=sv[:, v0], in_values=sc_[:, cs_]), r=["sc", "sv"], w=["si"])
                E("dve", lambda e, cs_=cs_, v0=v0: e.match_replace(out=wk[:, 0:128], in_to_replace=sv[:, v0], in_values=sc_[:, cs_], imm_value=-1e30),
                  r=["sc", "sv"], w=["wk"])
                E("dve", lambda e, v1=v1: e.max(out=sv[:, v1], in_=wk[:, 0:128]), r=["wk"], w=["sv"])
                E("dve", lambda e, v1=v1: e.max_index(out=si[:, v1], in_max=sv[:, v1], in_values=wk[:, 0:128]), r=["wk", "sv"], w=["si"])
            E("dve", lambda e: e.tensor_copy(out=sif, in_=si), r=["si"], w=["sif"])
            for h in range(8):
                s1 = sv[:, (2 * h) * 16:(2 * h) * 16 + 16]
                s2 = sv[:, (2 * h + 1) * 16:(2 * h + 1) * 16 + 16]
                i1 = sif[:, (2 * h) * 16:(2 * h) * 16 + 16]
                i2 = sif[:, (2 * h + 1) * 16:(2 * h + 1) * 16 + 16]
                cs3 = cs.rearrange("p (a b) -> p a b", a=16)
                ci3 = ci.rearrange("p (a b) -> p a b", a=16)
                E("dve", lambda e, s1=s1, s2=s2: e.tensor_tensor(out=cs3, in0=s1.unsqueeze(2).to_broadcast([128, 16, 16]),
                                                                 in1=s2.unsqueeze(1).to_broadcast([128, 16, 16]), op=ALU.add),
                  r=["sv"], w=["cs"])
                E("dve", lambda e, i1=i1, i2=i2: e.scalar_tensor_tensor(out=ci3, in0=i1.unsqueeze(2).to_broadcast([128, 16, 16]), scalar=128.0,
                                                                        in1=i2.unsqueeze(1).to_broadcast([128, 16, 16]),
                                                                        op0=ALU.mult, op1=ALU.add), r=["sif"], w=["ci"])
                E("dve", lambda e: e.max(out=ts_[:, 0:8], in_=cs), r=["cs"], w=["ts"])
                E("dve", lambda e: e.max_index(out=pos[:, 0:8], in_max=ts_[:, 0:8], in_values=cs), r=["cs", "ts"], w=["pos"])
                E("dve", lambda e: e.match_replace(out=wk, in_to_replace=ts_[:, 0:8], in_values=cs, imm_value=-1e30), r=["cs", "ts"], w=["wk"])
                E("dve", lambda e: e.max(out=ts_[:, 8:16], in_=wk), r=["wk"], w=["ts"])
                E("dve", lambda e: e.max_index(out=pos[:, 8:16], in_max=ts_[:, 8:16], in_values=wk), r=["wk", "ts"], w=["pos"])
                E("dve", lambda e: e.tensor_copy(out=posf, in_=pos), r=["pos"], w=["posf"])
                for k in range(16):
                    oh = ohs[k % 2]
                    ohn = "oh%d" % (k % 2)
                    E("pool", lambda e, k=k, oh=oh: e.tensor_scalar(out=oh, in0=io256, scalar1=posf[:, k:k + 1], scalar2=None, op0=ALU.is_equal),
                      r=["io256", "posf"], w=[ohn])
                    E("dve", lambda e, k=k, h=h, oh=oh: e.scalar_tensor_tensor(out=oh, in0=oh, scalar=1.0, in1=ci, op0=ALU.mult, op1=ALU.mult,
                                                                               accum_out=idxf[:, h * 16 + k:h * 16 + k + 1]),
                      r=[ohn, "ci"], w=[ohn, "idxf"])
                E("dve", lambda e: e.tensor_single_scalar(out=sm[:, 4:5], in_=ts_[:, 0:1], scalar=-1.0, op=ALU.mult), r=["ts"], w=["sm4"])
                E("act", lambda e, h=h: e.activation(out=gate[:, h * 16:(h + 1) * 16], in_=ts_, func=AF.Exp, bias=sm[:, 4:5],
                                                     accum_out=sm[:, 5:6]), r=["ts", "sm4"], w=[gtn, "sm5"])
                E("dve", lambda e: e.reciprocal(out=sm[:, 6:7], in_=sm[:, 5:6]), r=["sm5"], w=["sm6"])
                E("dve", lambda e, h=h: e.tensor_scalar(out=gate[:, h * 16:(h + 1) * 16], in0=gate[:, h * 16:(h + 1) * 16],
                                                        scalar1=sm[:, 6:7], scalar2=None, op0=ALU.mult), r=[gtn, "sm6"], w=[gtn])
            E("dve", lambda e: e.tensor_copy(out=idxu, in_=idxf), r=["idxf"], w=[iun])

        def peer_slots(tl):
            par = tl % 2
            t0 = tl * 128
            x1t, h2b, idxu, gate = x1ts[par], h2bs[par], idxus[par], gates[par]
            xn, hbn, iun, gtn = "x1t%d" % par, "h2b%d" % par, "idxu%d" % par, "gate%d" % par
            for s in range(128):
                uv, d_ = UVg[s % NB], dg[s % 2]
                ug, vg = uv[:, 0:2048], uv[:, 2048:4096]
                un, dn = "UV%d" % (s % NB), "dg%d" % (s % 2)
                vn = un
                an, gn_ = "actv%d" % (s % 8), "gact%d" % (s % 8)
                E("pool", lambda e, uv=uv, s=s: e.indirect_dma_start(out=uv, out_offset=None, in_=S_uv,
                                                                     in_offset=bass.IndirectOffsetOnAxis(ap=idxu[:, s:s + 1], axis=0)),
                  r=[iun], w=[un], dma=True)
                wn_ = "wv%d" % (s % 8)
                if s % 2 == 0:
                    E("dve", lambda e, ug=ug, s=s: e.scalar_tensor_tensor(out=junkb, in0=ug, scalar=1.0, in1=h2b, op0=ALU.mult, op1=ALU.mult,
                                                                          accum_out=actv[:, s:s + 1]), r=[un, hbn], w=["junkb", an])
                else:
                    pj = prodj[(s // 2) % 2]
                    pjn = "prodj%d" % ((s // 2) % 2)
                    E("pool", lambda e, ug=ug, pj=pj: e.tensor_tensor(out=pj, in0=ug, in1=h2b, op=ALU.mult), r=[un, hbn], w=[pjn])
                    E("act", lambda e, pj=pj, s=s: e.activation(out=junka, in_=pj, func=AF.Copy, accum_out=actv[:, s:s + 1]),
                      r=[pjn], w=["junka", an])
                E("act", lambda e, s=s: e.activation(out=gact[:, s:s + 1], in_=actv[:, s:s + 1], func=AF.Gelu), r=[an], w=[gn_])
                E("act", lambda e, s=s: e.activation(out=wv[:, s:s + 1], in_=gact[:, s:s + 1], func=AF.Copy, scale=gate[:, s:s + 1]),
                  r=[gn_, gtn], w=[wn_])
                E("act", lambda e, d_=d_, s=s: e.activation(out=d_, in_=ident, func=AF.Copy, scale=wv[:, s:s + 1]),
                  r=["ident", wn_], w=[dn])
                for n in range(4):
                    E("pe", lambda e, d_=d_, vg=vg, n=n, s=s: e.matmul(bank(4 + n), lhsT=d_, rhs=vg[:, n * 512:(n + 1) * 512],
                                                                       start=(s == 0), stop=(s == 127)), r=[dn, vn], w=pb(4 + n))
            E("dve", lambda e: e.tensor_tensor(out=x2, in0=PS[:, 2048:4096], in1=g2bc, op=ALU.mult), r=pb(4, 5, 6, 7) + ["g2bc"], w=["x2"])
            E("pool", lambda e: e.tensor_tensor(out=x2, in0=x2, in1=x1t, op=ALU.add), r=["x2", xn], w=["x2"])
            rstd_of("x2", x2, junk4, "junk4", smf, "smf")
            E("dve", lambda e: e.scalar_tensor_tensor(out=x2, in0=x2, scalar=smf[:, 3:4], in1=fgbc, op0=ALU.mult, op1=ALU.mult),
              r=["x2", "smf", "fgbc"], w=["x2"])
            stq("out", out_d[t0:t0 + 128, :], "x2", x2, eng="sp")

        peer_setup(0)
        for tl in range(NT):
            if tl + 1 < NT:
                peer_setup(tl + 1)
            peer_slots(tl)
        return _finish(nc, P, out_d, E, A, st)


def _finish(nc, P, out_d, E, A, st):
    P.barrier()
    P.replay()
    return nc


def _wl(w, kc):
    K, N = w.shape
    return np.ascontiguousarray(w.reshape(kc, 128, N // 128, 128).transpose(2, 1, 0, 3).reshape(N // 128, 128, kc * 128))


def _col(v, n):
    return np.ascontiguousarray(v.reshape(n, 128).T)


def make_in_maps(inp, n_cores, Lh):
    f = lambda a: np.asarray(a, dtype=np.float32)
    x, c, ctx, c_ctx = f(inp["x"]), f(inp["c"]), f(inp["ctx"]), f(inp["c_ctx"])
    w_in = f(inp["w_in"])[0]
    OFF_DT = 6144
    w_in_sw = w_in.copy()
    w_in_sw[:, OFF_DT:OFF_DT + 64] = w_in[:, OFF_DT + 64:OFF_DT + 128]
    w_in_sw[:, OFF_DT + 64:OFF_DT + 128] = w_in[:, OFF_DT:OFF_DT + 64]
    w_in_l = [_wl(w_in, 16), _wl(w_in_sw, 16)]
    w_ada_l = _wl(f(inp["w_ada"])[0], 16)
    b_ada_l = _col(f(inp["b_ada"])[0], 96)
    n1g = _col(f(inp["norm1_g"])[0], 16)
    n2g = _col(f(inp["norm2_g"])[0], 16)
    cw = f(inp["ssd_conv_w"])[0]
    cwl = [np.ascontiguousarray(cw.T.reshape(48, 128, 5).transpose(1, 0, 2).reshape(128, 240)),
           np.ascontiguousarray(cw[::-1].T.reshape(48, 128, 5).transpose(1, 0, 2).reshape(128, 240))]
    cb = _col(f(inp["ssd_conv_b"])[0], 48)
    dtb = f(inp["ssd_dt_bias"])[0].reshape(128, 1)
    alog = f(inp["ssd_A_log"])[0].reshape(128, 1)
    dtb_l = [dtb, np.ascontiguousarray(np.concatenate([dtb[64:], dtb[:64]], 0))]
    alog_l = [alog, np.ascontiguousarray(np.concatenate([alog[64:], alog[:64]], 0))]
    ssdD = f(inp["ssd_D"])[0].reshape(1, 64)
    gn = _col(f(inp["ssd_norm_g"])[0], 32)
    wssd = _wl(f(inp["ssd_w_out"])[0], 32)
    sw = f(inp["sc_conv_w"])[0]
    swl = [np.ascontiguousarray(sw.T.reshape(16, 128, 3).transpose(1, 0, 2).reshape(128, 48)),
           np.ascontiguousarray(sw[::-1].T.reshape(16, 128, 3).transpose(1, 0, 2).reshape(128, 48))]
    wsc = _wl(f(inp["sc_w_out"])[0], 16)
    wo = _wl(f(inp["w_o"])[0], 16)
    wq = _wl(f(inp["peer_w_q"])[0], 16)
    keys = f(inp["peer_keys"])[0]
    keysT = np.ascontiguousarray(keys.reshape(16, 128, 128).transpose(2, 0, 1).reshape(128, 2048))
    pu = np.ascontiguousarray(f(inp["peer_u"])[0])
    pv = np.ascontiguousarray(f(inp["peer_v"])[0])
    fg = f(inp["final_g"]).reshape(1, D)
    maps = []
    for core in range(n_cores):
        b, half = core // 2, core % 2
        xb = x[b]
        if half == 0:
            xm_, xa_, xc_ = xb[:Lh], xb[Lh:2 * Lh], ctx[b]
        else:
            rv = xb[:2 * Lh][::-1]
            xm_, xa_, xc_ = rv[:Lh], rv[Lh:], ctx[b][::-1]
        cvec = np.stack([c[b], c_ctx], -1).reshape(16, 128, 2).transpose(1, 0, 2).reshape(128, 32)
        maps.append({
            "xm": np.ascontiguousarray(xm_), "xa": np.ascontiguousarray(xa_), "xc": np.ascontiguousarray(xc_),
            "cvec": np.ascontiguousarray(cvec), "w_ada": w_ada_l, "b_ada": b_ada_l, "n1g": n1g, "n2g": n2g,
            "w_in": w_in_l[half], "convw": cwl[half], "convb": cb, "dtb": dtb_l[half], "alog": alog_l[half],
            "ssdD": ssdD, "gnorm": gn, "wssd": wssd, "scw": swl[half], "wsc": wsc, "wo": wo, "wq": wq,
            "keysT": keysT, "peer_u": pu, "peer_v": pv, "final_g": fg,
        })
    return maps


def kernel(**inputs):
    B, S, _ = inputs["x"].shape
    Lh = S // 2
    n_cores = 2 * B
    nc = build_nc(Lh, Lh, inputs["ctx"].shape[1])
    maps = make_in_maps(inputs, n_cores, Lh)
    res = run_bass_kernel_spmd(nc, maps, core_ids=list(range(n_cores)))
    out = np.empty((B, S, D), np.float32)
    for core in range(n_cores):
        b, half = core // 2, core % 2
        o = res.results[core]["out"]
        if half == 0:
            out[b, :Lh] = o
        else:
            out[b, Lh:] = o[::-1]
    return out
```

```python
import numpy as np
from contextlib import ExitStack
import concourse.bass as bass
import concourse.mybir as mybir
from concourse.bass_utils import run_bass_kernel_spmd

F32 = mybir.dt.float32
BF16 = mybir.dt.bfloat16
U32 = mybir.dt.uint32
I32 = mybir.dt.int32
AF = mybir.ActivationFunctionType
ALU = mybir.AluOpType
AX = mybir.AxisListType

D = 2048
DC = 16
DSSD = 4096
NH = 64
EPS = 1e-6
NG_IN = 161
ENGS = ("pe", "act", "dve", "pool", "sp")
DMA_ENGS = ("sp", "pool")


class Buf:
    __slots__ = ("w", "r")

    def __init__(self):
        self.w = {}
        self.r = {}


class Prog:
    def __init__(self, nc, stack, n_slots=8):
        self.nc = nc
        self.stack = stack
        self.ops = {e: [] for e in ENGS}
        self.cnt = {}
        self.known = {e: {} for e in ENGS}
        self.sems = {}
        self.cur = {}
        self.epoch = 0
        for e in ("pe", "act", "dve", "pool"):
            self._new_compute_sem(e)
        self.slots = {}
        self.slot_next = {}
        for e in DMA_ENGS:
            self.slots[e] = []
            self.slot_next[e] = 0
            for i in range(n_slots):
                k = "d_%s_%d" % (e, i)
                self.sems[k] = stack.enter_context(nc.semaphore(k))
                self.cnt[k] = 0
                self.slots[e].append(k)
        self.bufs = {}
        self.n_ops = 0

    def _new_compute_sem(self, e):
        k = "c_%s_%d" % (e, self.epoch)
        self.sems[k] = self.stack.enter_context(self.nc.semaphore(k))
        self.cnt[k] = 0
        self.cur[e] = k

    def _b(self, name):
        b = self.bufs.get(name)
        if b is None:
            b = Buf()
            self.bufs[name] = b
        return b

    def emit(self, eng, fn, r=(), w=(), pw=(), dma=False):
        deps = {}

        def add(d):
            for k, v in d.items():
                if deps.get(k, 0) < v:
                    deps[k] = v

        for n in r:
            add(self._b(n).w)
        for n in w:
            b = self._b(n)
            add(b.w)
            add(b.r)
        for n in pw:
            add(self._b(n).r)
        if dma:
            sl = self.slots[eng]
            k = sl[self.slot_next[eng] % len(sl)]
            self.slot_next[eng] += 1
            if self.cnt[k] > 0:
                add({k: self.cnt[k]})
            self.cnt[k] += 16
            inc = 16
        else:
            k = self.cur[eng]
            self.cnt[k] += 1
            inc = 1
        sig = (k, self.cnt[k])
        waits = []
        kn = self.known[eng]
        for dk, dv in deps.items():
            if (not dma) and dk == k and eng == "pe":
                continue
            if kn.get(dk, 0) >= dv:
                continue
            kn[dk] = dv
            waits.append((dk, dv))
        self.ops[eng].append((waits, fn, sig[0], inc))
        for n in r:
            b = self._b(n)
            if b.r.get(sig[0], 0) < sig[1]:
                b.r[sig[0]] = sig[1]
        for n in w:
            b = self._b(n)
            b.w = {sig[0]: sig[1]}
            b.r = {}
        for n in pw:
            b = self._b(n)
            b.w[sig[0]] = sig[1]
            b.r = {}
        self.n_ops += 1

    def barrier(self):
        allc = [(k, v) for k, v in self.cnt.items() if v > 0]
        for e in ENGS:
            kn = self.known[e]
            waits = []
            for k, v in allc:
                if kn.get(k, 0) < v:
                    kn[k] = v
                    waits.append((k, v))
            if waits:
                self.ops[e].append((waits, None, None, 0))
        self.bufs = {}
        self.epoch += 1
        for e in ("pe", "act", "dve", "pool"):
            if self.cnt[self.cur[e]] > 20000:
                self._new_compute_sem(e)

    def replay(self):
        nc = self.nc
        with nc.Block() as block:
            def run(e):
                def body(engine):
                    for waits, fn, sk, inc in self.ops[e]:
                        for k, v in waits:
                            engine.wait_ge(self.sems[k], v)
                        if fn is not None:
                            fn(engine).then_inc(self.sems[sk], inc)
                return body
            block.tensor(run("pe"))
            block.scalar(run("act"))
            block.vector(run("dve"))
            block.gpsimd(run("pool"))
            block.sync(run("sp"))


class Alloc:
    def __init__(self, big, nwords):
        self.big = big
        self.cap = nwords
        self.off = 0

    def f32(self, n):
        ap = self.big[:, self.off:self.off + n]
        self.off += n
        assert self.off <= self.cap, ("SBUF overflow", self.off, self.cap)
        return ap

    def bf16(self, n):
        return self.f32((n + 1) // 2).bitcast(BF16)

    def u32(self, n):
        return self.f32(n).bitcast(U32)


def build_nc(Lm, La, Lc, TB=2048, T3=512, CSP=2048, dbg=(), stop_after=None):
    nc = bass.Bass("TRN2", target_bir_lowering=False)
    T3 = min(T3, Lm)
    Ltot = Lm + La
    NM, NA, NC_ = Lm // 128, La // 128, Lc // 128
    NCH = NM + NA + NC_

    def din(name, shape, dt=F32):
        return nc.dram_tensor(name, list(shape), dt, kind="ExternalInput").ap()

    def dscr(name, shape, dt=F32):
        kind = "ExternalOutput" if name in dbg else "Internal"
        return nc.dram_tensor(name, list(shape), dt, kind=kind).ap()

    xm = din("xm", [Lm, D])
    xa = din("xa", [La, D])
    xc = din("xc", [Lc, D])
    cvec = din("cvec", [128, 32])
    w_ada = din("w_ada", [96, 128, 2048])
    b_ada = din("b_ada", [128, 96])
    n1g_d = din("n1g", [128, 16])
    n2g_d = din("n2g", [128, 16])
    w_in = din("w_in", [NG_IN, 128, 2048])
    convw_d = din("convw", [128, 48 * 5])
    convb_d = din("convb", [128, 48])
    dtb_d = din("dtb", [128, 1])
    alog_d = din("alog", [128, 1])
    ssdD_d = din("ssdD", [1, 64])
    gn_d = din("gnorm", [128, 32])
    wssd_d = din("wssd", [16, 128, 4096])
    scw_d = din("scw", [128, 48])
    wsc_d = din("wsc", [16, 128, 2048])
    wo_d = din("wo", [16, 128, 2048])
    wq_d = din("wq", [16, 128, 2048])
    keys_d = din("keysT", [128, 2048])
    pu_d = din("peer_u", [16384, D])
    pv_d = din("peer_v", [16384, D])
    fg_d = din("final_g", [1, D])
    out_d = nc.dram_tensor("out", [Lm, D], F32, kind="ExternalOutput").ap()

    S_a = dscr("S_a", [49 * 128, Ltot + 4])
    S_ac = dscr("S_ac", [49 * 128, Lc + 4])
    S_b = dscr("S_b", [112 * 128, Lm])
    S_xt = dscr("S_xt", [Ltot + Lc, DSSD])
    S_bt = dscr("S_bt", [Ltot + Lc, 1024])
    S_bT = dscr("S_bT", [1024, Lm])
    S_cT = dscr("S_cT", [1024, Lm])
    S_dtt = dscr("S_dtt", [NCH, 128, 128])
    S_act = dscr("S_act", [NCH, 128, 128])
    S_acc = dscr("S_acc", [NCH, 128 * 128])
    S_tot = dscr("S_tot", [NCH, 128])
    S_yb = dscr("S_yb", [Lm, DSSD])
    S_yn = dscr("S_yn", [DSSD, Lm], BF16)
    S_x1 = dscr("S_x1", [Lm, D])
    S_vec = dscr("S_vec", [64, 128])
    S_hst = dscr("S_hst", [2, 128, DSSD])
    S_uv = dscr("S_uv", [16384, 2 * D], BF16)

    with ExitStack() as st:
        NW = 52400
        big = st.enter_context(nc.sbuf_tensor("big", [128, NW], F32))
        PS = st.enter_context(nc.psum_tensor("PS", [128, 4096], F32))
        A = Alloc(big, NW)
        P = Prog(nc, st)
        E = P.emit

        def bank(b, n=512, o=0):
            return PS[:, b * 512 + o:b * 512 + o + n]

        def pb(*bs):
            return ["ps%d" % b for b in bs]

        def ld(name, out_ap, in_ap, r=(), eng="sp"):
            E(eng, lambda e: e.dma_start(out=out_ap, in_=in_ap), r=list(r), w=[name], dma=True)

        def stq(out_name, out_ap, in_name, in_ap, eng="pool"):
            E(eng, lambda e: e.dma_start(out=out_ap, in_=in_ap), r=[in_name], pw=[out_name], dma=True)

        iot = A.f32(128)
        ident = A.f32(128)
        maskU = A.f32(128)
        maskL = A.f32(128)
        io256 = A.f32(256)
        ones = A.f32(128)
        zt = A.f32(128)
        a1 = A.f32(16)
        sh1 = A.f32(16)
        a1c = A.f32(16)
        sh1c = A.f32(16)
        convw = A.f32(240)
        convb = A.f32(48)
        scw = A.f32(48)
        dtb = A.f32(1)
        acol_A = A.f32(1)
        gncol = A.f32(32)
        Dbc = A.f32(64)
        E("pool", lambda e: e.iota(iot, pattern=[[1, 128]], base=0, channel_multiplier=-1,
                                   allow_small_or_imprecise_dtypes=True), w=["iot"])
        E("pool", lambda e: e.iota(io256, pattern=[[1, 256]], base=0, channel_multiplier=0,
                                   allow_small_or_imprecise_dtypes=True), w=["io256"])
        E("dve", lambda e: e.tensor_single_scalar(out=ident, in_=iot, scalar=0.0, op=ALU.is_equal), r=["iot"], w=["ident"])
        E("dve", lambda e: e.tensor_single_scalar(out=maskU, in_=iot, scalar=0.0, op=ALU.is_ge), r=["iot"], w=["maskU"])
        E("dve", lambda e: e.tensor_single_scalar(out=maskL, in_=iot, scalar=0.0, op=ALU.is_le), r=["iot"], w=["maskL"])
        E("pool", lambda e: e.memset(ones, 1.0), w=["ones"])
        E("pool", lambda e: e.memset(zt, 0.0), w=["zt"])
        ld("convw", convw, convw_d)
        ld("convb", convb, convb_d)
        ld("scw", scw, scw_d)
        ld("dtb", dtb, dtb_d)
        ld("gncol", gncol, gn_d)
        ld("Dbc", Dbc, ssdD_d.partition_broadcast(128))
        alog = A.f32(1)
        ld("alog", alog, alog_d)
        E("act", lambda e: e.activation(out=acol_A, in_=alog, func=AF.Exp), r=["alog"], w=["acolA"])
        E("dve", lambda e: e.tensor_single_scalar(out=acol_A, in_=acol_A, scalar=-1.0, op=ALU.mult), r=["acolA"], w=["acolA"])
        E("sp", lambda e: e.dma_start(out=S_a.rearrange("(g p) c -> p g c", p=128)[:, :, 0:2],
                                      in_=zt[:, 0:98].rearrange("p (g c) -> p g c", c=2)), r=["zt"], pw=["S_a"], dma=True)
        E("sp", lambda e: e.dma_start(out=S_a.rearrange("(g p) c -> p g c", p=128)[:, :, Ltot + 2:Ltot + 4],
                                      in_=zt[:, 0:98].rearrange("p (g c) -> p g c", c=2)), r=["zt"], pw=["S_a"], dma=True)
        E("sp", lambda e: e.dma_start(out=S_ac.rearrange("(g p) c -> p g c", p=128)[:, :, 0:2],
                                      in_=zt[:, 0:98].rearrange("p (g c) -> p g c", c=2)), r=["zt"], pw=["S_ac"], dma=True)
        E("sp", lambda e: e.dma_start(out=S_ac.rearrange("(g p) c -> p g c", p=128)[:, :, Lc + 2:Lc + 4],
                                      in_=zt[:, 0:98].rearrange("p (g c) -> p g c", c=2)), r=["zt"], pw=["S_ac"], dma=True)
        base_off = A.off

        cv = A.f32(32)
        scv = A.f32(32)
        wts = [A.f32(2048), A.f32(2048)]
        modv = A.f32(192)
        bl = A.f32(96)
        n1g = A.f32(16)
        n2g = A.f32(16)
        V4 = A.f32(64)
        V4T = A.f32(128)
        ld("cv", cv, cvec)
        ld("bl", bl, b_ada)
        ld("n1g", n1g, n1g_d)
        ld("n2g", n2g, n2g_d)
        E("act", lambda e: e.activation(out=scv, in_=cv, func=AF.Silu), r=["cv"], w=["scv"])
        for j in range(96):
            wt = wts[j % 2]
            wn = "wt%d" % (j % 2)
            ld(wn, wt, w_ada[j])
            for kc in range(16):
                E("pe", lambda e, wt=wt, kc=kc, j=j: e.matmul(bank(0, 2, 2 * j), lhsT=wt[:, kc * 128:(kc + 1) * 128],
                                                             rhs=scv[:, 2 * kc:2 * kc + 2], start=(kc == 0), stop=(kc == 15)),
                  r=[wn, "scv"], w=pb(0))
        m3 = modv.rearrange("p (j n) -> p j n", n=2)
        E("dve", lambda e: e.tensor_tensor(out=m3, in0=bank(0, 192).rearrange("p (j n) -> p j n", n=2),
                                           in1=bl.unsqueeze(2).to_broadcast([128, 96, 2]), op=ALU.add),
          r=pb(0) + ["bl"], w=["modv"])

        def mcol(ch, n):
            return m3[:, ch * 16:(ch + 1) * 16, n]

        E("dve", lambda e: e.scalar_tensor_tensor(out=a1, in0=mcol(1, 0), scalar=1.0, in1=n1g, op0=ALU.add, op1=ALU.mult),
          r=["modv", "n1g"], w=["a1"])
        E("dve", lambda e: e.scalar_tensor_tensor(out=a1c, in0=mcol(1, 1), scalar=1.0, in1=n1g, op0=ALU.add, op1=ALU.mult),
          r=["modv", "n1g"], w=["a1c"])
        E("dve", lambda e: e.tensor_copy(out=sh1, in_=mcol(0, 0)), r=["modv"], w=["sh1"])
        E("dve", lambda e: e.tensor_copy(out=sh1c, in_=mcol(0, 1)), r=["modv"], w=["sh1c"])
        E("dve", lambda e: e.scalar_tensor_tensor(out=V4[:, 0:16], in0=mcol(4, 0), scalar=1.0, in1=n2g, op0=ALU.add, op1=ALU.mult),
          r=["modv", "n2g"], w=["V4"])
        E("dve", lambda e: e.tensor_copy(out=V4[:, 16:32], in_=mcol(3, 0)), r=["modv"], w=["V4"])
        E("dve", lambda e: e.tensor_copy(out=V4[:, 32:48], in_=mcol(2, 0)), r=["modv"], w=["V4"])
        E("dve", lambda e: e.tensor_copy(out=V4[:, 48:64], in_=mcol(5, 0)), r=["modv"], w=["V4"])
        E("pe", lambda e: e.transpose(out=bank(1, 128)[0:64, :], in_=V4, identity=ident), r=["V4", "ident"], w=pb(1))
        E("dve", lambda e: e.tensor_copy(out=V4T[0:64, :], in_=bank(1, 128)[0:64, :]), r=pb(1), w=["V4T"])
        E("sp", lambda e: e.dma_start(out=S_vec, in_=V4T[0:64, :]), r=["V4T"], w=["S_vec"], dma=True)
        P.barrier()
        A.off = base_off
        if stop_after == "P0":
            return _finish(nc, P, out_d, E, A, st)

        def inproj(tag, xd, L, acol, shcol, groups, dest, precast=False):
            T = min(L, TB)
            mark = A.off
            xts = [A.f32(2048), A.f32(2048)]
            junk = A.bf16(2048)
            xs = A.f32(2048)
            hT = A.bf16(16 * T).rearrange("p (k t) -> p k t", k=16)
            w32s = [A.f32(2048), A.f32(2048)]
            wbs = [A.bf16(2048), A.bf16(2048)]
            stg = [A.f32(T), A.f32(T)]
            ss = A.f32(4)
            NS = min(512, T)
            if precast:
                pc32 = [A.f32(2048), A.f32(2048)]
                pcb = [A.bf16(2048), A.bf16(2048)]
            pci = [0]

            def precast_step():
                i = pci[0]
                if (not precast) or i >= 256:
                    return
                pci[0] += 1
                tbl, dst, dn = (pu_d, S_uv[:, 0:D], "S_uv") if i < 128 else (pv_d, S_uv[:, D:2 * D], "S_uv")
                r0 = (i % 128) * 128
                a32, ab = pc32[i % 2], pcb[i % 2]
                n32_, nb_ = "pc32_%d" % (i % 2), "pcb_%d" % (i % 2)
                ld(n32_, a32, tbl[r0:r0 + 128, :])
                E("pool", lambda e: e.tensor_copy(out=ab, in_=a32), r=[n32_], w=[nb_])
                stq(dn, dst[r0:r0 + 128, :], nb_, ab)
            for blk in range(L // T):
                t0 = blk * T
                for tt in range(T // 128):
                    xt = xts[tt % 2]
                    xn = "xt%d" % (tt % 2)
                    ld(xn, xt, xd[t0 + tt * 128:t0 + (tt + 1) * 128, :])
                    E("act", lambda e, xt=xt: e.activation(out=junk, in_=xt, func=AF.Square, accum_out=ss[:, 0:1]),
                      r=[xn], w=["junk", "ss"])
                    E("dve", lambda e: e.tensor_scalar(out=ss[:, 1:2], in0=ss[:, 0:1], scalar1=1.0 / D, scalar2=EPS,
                                                       op0=ALU.mult, op1=ALU.add), r=["ss"], w=["ss1"])
                    E("act", lambda e: e.activation(out=ss[:, 2:3], in_=ss[:, 1:2], func=AF.Sqrt), r=["ss1"], w=["ss2"])
                    E("dve", lambda e: e.reciprocal(out=ss[:, 3:4], in_=ss[:, 2:3]), r=["ss2"], w=["ss3"])
                    E("act", lambda e, xt=xt: e.activation(out=xs, in_=xt, func=AF.Copy, scale=ss[:, 3:4]),
                      r=[xn, "ss3"], w=["xs"])
                    for dc in range(16):
                        E("pe", lambda e, dc=dc: e.transpose(out=bank(dc // 4, 128, (dc % 4) * 128),
                                                             in_=xs[:, dc * 128:(dc + 1) * 128], identity=ident),
                          r=["xs", "ident"], w=pb(dc // 4))
                    for dc in range(16):
                        E("dve", lambda e, dc=dc, tt=tt: e.tensor_scalar(
                            out=hT[:, dc, tt * 128:(tt + 1) * 128], in0=bank(dc // 4, 128, (dc % 4) * 128),
                            scalar1=acol[:, dc:dc + 1], scalar2=shcol[:, dc:dc + 1], op0=ALU.mult, op1=ALU.add),
                          r=pb(dc // 4) + [tag + "a", tag + "s"], w=["hT"])
                for gi, g in enumerate(groups):
                    w32 = w32s[gi % 2]
                    wb = wbs[gi % 2]
                    sg = stg[gi % 2]
                    n32, nb, nsg = "w32_%d" % (gi % 2), "wb_%d" % (gi % 2), "stg_%d" % (gi % 2)
                    ld(n32, w32, w_in[g])
                    E("act", lambda e, wb=wb, w32=w32: e.activation(out=wb, in_=w32, func=AF.Copy), r=[n32], w=[nb])
                    pbase = 4 * (gi % 2)
                    nsl = T // NS
                    for ns in range(nsl):
                        for kc in range(16):
                            E("pe", lambda e, wb=wb, ns=ns, kc=kc, pbase=pbase: e.matmul(
                                bank(pbase + ns, NS), lhsT=wb[:, kc * 128:(kc + 1) * 128],
                                rhs=hT[:, kc, ns * NS:(ns + 1) * NS], start=(kc == 0), stop=(kc == 15)),
                              r=[nb, "hT"], w=pb(pbase + ns))
                    E("dve", lambda e, sg=sg, pbase=pbase: e.tensor_copy(out=sg, in_=PS[:, pbase * 512:pbase * 512 + T]),
                      r=pb(*range(pbase, pbase + nsl)), w=[nsg])
                    dname, dap = dest(g, t0, T)
                    stq(dname, dap, nsg, sg)
                    precast_step()
            while precast and pci[0] < 256:
                precast_step()
            P.barrier()
            A.off = mark

        def dest_lat(off):
            def f(g, t0, T):
                if g < 49:
                    return "S_a", S_a[g * 128:(g + 1) * 128, 2 + off + t0:2 + off + t0 + T]
                return "S_b", S_b[(g - 49) * 128:(g - 48) * 128, t0:t0 + T]
            return f

        def dest_ctx(g, t0, T):
            return "S_ac", S_ac[g * 128:(g + 1) * 128, 2 + t0:2 + t0 + T]

        for nm, src in (("ca", "a1c"), ("cs", "sh1c"), ("la", "a1"), ("ls", "sh1")):
            pass
        inproj("c", xc, Lc, a1c, sh1c, list(range(40)) + [48], dest_ctx)
        inproj("l", xa, La, a1, sh1, list(range(49)), dest_lat(Lm))
        inproj("l", xm, Lm, a1, sh1, list(range(NG_IN)), dest_lat(0), precast=True)
        P.barrier()
        if stop_after == "P1":
            return _finish(nc, P, out_d, E, A, st)

        def conv_set(src, base, L, row0, ccs, feat):
            mark = A.off
            SPN = min(CSP, L)
            pres = [A.f32(SPN + 4), A.f32(SPN + 4)]
            acc = A.f32(SPN)
            ress = [A.f32(SPN), A.f32(SPN)]
            tms = [A.f32(SPN), A.f32(SPN)]
            it = 0
            for cc in ccs:
                for s0 in range(0, L, SPN):
                    n = SPN
                    pre, res, tm = pres[it % 2], ress[it % 2], tms[it % 2]
                    npre, nres, ntm = "pre%d" % (it % 2), "res%d" % (it % 2), "tm%d" % (it % 2)
                    it += 1
                    ld(npre, pre, src[cc * 128:(cc + 1) * 128, base + s0:base + s0 + n + 4])
                    E("dve", lambda e, pre=pre, cc=cc: e.tensor_scalar(out=acc, in0=pre[:, 0:n], scalar1=convw[:, cc * 5:cc * 5 + 1],
                                                                       scalar2=None, op0=ALU.mult), r=[npre, "convw"], w=["acc"])
                    for k in range(1, 5):
                        E("dve", lambda e, pre=pre, cc=cc, k=k: e.scalar_tensor_tensor(
                            out=acc, in0=pre[:, k:k + n], scalar=convw[:, cc * 5 + k:cc * 5 + k + 1], in1=acc,
                            op0=ALU.mult, op1=ALU.add), r=[npre, "convw", "acc"], w=["acc"])
                    E("act", lambda e, res=res, cc=cc: e.activation(out=res, in_=acc, func=AF.Silu, bias=convb[:, cc:cc + 1]),
                      r=["acc", "convb"], w=[nres])
                    if cc < 40:
                        J = n // 128
                        for j in range(J):
                            E("pe", lambda e, res=res, j=j: e.transpose(out=PS[:, j * 128:(j + 1) * 128],
                                                                        in_=res[:, j * 128:(j + 1) * 128], identity=ident),
                              r=[nres, "ident"], w=pb(j // 4))
                        E("act", lambda e, tm=tm: e.activation(out=tm, in_=PS[:, 0:n], func=AF.Copy),
                          r=pb(*range((J + 3) // 4)), w=[ntm])
                        if cc < 32:
                            dst = S_xt[row0 + s0:row0 + s0 + n, cc * 128:(cc + 1) * 128]
                            dn = "S_xt"
                        else:
                            dst = S_bt[row0 + s0:row0 + s0 + n, (cc - 32) * 128:(cc - 31) * 128]
                            dn = "S_bt"
                        stq(dn, dst.rearrange("(j p) c -> p j c", p=128), ntm, tm.rearrange("p (j c) -> p j c", c=128))
                    if feat and cc >= 32:
                        if cc < 40:
                            stq("S_bT", S_bT[(cc - 32) * 128:(cc - 31) * 128, s0:s0 + n], nres, res)
                        else:
                            stq("S_cT", S_cT[(cc - 40) * 128:(cc - 39) * 128, s0:s0 + n], nres, res)
            P.barrier()
            A.off = mark

        conv_set(S_ac, 0, Lc, Ltot, list(range(40)), False)
        conv_set(S_a, Lm, La, Lm, list(range(40)), False)
        conv_set(S_a, 0, Lm, 0, list(range(48)), True)

        def dt_set(src, base, L, cid0):
            mark = A.off
            SPN = min(2048, L)
            pd = A.f32(SPN)
            xb = A.f32(SPN)
            t1 = A.f32(SPN)
            dtv = A.f32(SPN)
            av = A.f32(SPN)
            ac = A.f32(SPN)
            tmpb = A.f32(128)
            totc = A.f32(16)
            tmA = A.f32(SPN)
            tmD = A.f32(SPN)
            totT = A.f32(128)
            for s0 in range(0, L, SPN):
                n = SPN
                ncn = n // 128
                ld("pd", pd, src[48 * 128:49 * 128, 2 + base + s0:2 + base + s0 + n])
                E("act", lambda e: e.activation(out=xb, in_=pd, func=AF.Identity, bias=dtb[:, 0:1]), r=["pd", "dtb"], w=["xb"])
                E("dve", lambda e: e.scalar_tensor_tensor(out=t1, in0=xb, scalar=-1.0, in1=xb, op0=ALU.mult, op1=ALU.max), r=["xb"], w=["t1"])
                E("act", lambda e: e.activation(out=t1, in_=t1, func=AF.Exp, scale=-1.0), r=["t1"], w=["t1"])
                E("act", lambda e: e.activation(out=t1, in_=t1, func=AF.Ln, bias=1.0), r=["t1"], w=["t1"])
                E("dve", lambda e: e.scalar_tensor_tensor(out=dtv, in0=xb, scalar=0.0, in1=t1, op0=ALU.max, op1=ALU.add),
                  r=["xb", "t1"], w=["dtv"])
                E("dve", lambda e: e.tensor_scalar(out=av, in0=dtv, scalar1=acol_A[:, 0:1], scalar2=None, op0=ALU.mult),
                  r=["dtv", "acolA"], w=["av"])
                E("dve", lambda e: e.tensor_reduce(out=totc[:, 0:ncn], in_=av.rearrange("p (c t) -> p c t", t=128),
                                                   axis=AX.X, op=ALU.add), r=["av"], w=["totc"])
                for c in range(ncn):
                    sl = slice(c * 128, (c + 1) * 128)
                    E("dve", lambda e, sl=sl: e.tensor_tensor_scan(out=ac[:, sl], data0=ones, data1=av[:, sl], initial=0.0,
                                                                   op0=ALU.mult, op1=ALU.add), r=["ones", "av", "ac"], w=["ac"])
                    E("dve", lambda e, sl=sl, c=c: e.tensor_scalar(out=tmpb[64:128, :], in0=ac[64:128, sl], scalar1=-1.0,
                                                                   scalar2=totc[64:128, c:c + 1], op0=ALU.mult, op1=ALU.add),
                      r=["ac", "totc"], w=["tmpb"])
                    E("dve", lambda e, sl=sl: e.tensor_tensor(out=ac[64:128, sl], in0=tmpb[64:128, :], in1=av[64:128, sl], op=ALU.add),
                      r=["tmpb", "av", "ac"], w=["ac"])
                stq("S_acc", S_acc[cid0 + s0 // 128:cid0 + s0 // 128 + ncn, :].rearrange("c (p t) -> p c t", p=128),
                    "ac", ac.rearrange("p (c t) -> p c t", t=128), eng="sp")
                for c in range(ncn):
                    E("pe", lambda e, c=c: e.transpose(out=PS[:, c * 128:(c + 1) * 128], in_=ac[:, c * 128:(c + 1) * 128], identity=ident),
                      r=["ac", "ident"], w=pb(c // 4))
                    E("pe", lambda e, c=c: e.transpose(out=PS[:, 2048 + c * 128:2048 + (c + 1) * 128], in_=dtv[:, c * 128:(c + 1) * 128],
                                                       identity=ident), r=["dtv", "ident"], w=pb(4 + c // 4))
                nb4 = (ncn + 3) // 4
                E("act", lambda e: e.activation(out=tmA, in_=PS[:, 0:n], func=AF.Copy), r=pb(*range(nb4)), w=["tmA"])
                E("act", lambda e: e.activation(out=tmD, in_=PS[:, 2048:2048 + n], func=AF.Copy), r=pb(*range(4, 4 + nb4)), w=["tmD"])
                c0 = cid0 + s0 // 128
                stq("S_act", S_act[c0:c0 + ncn].rearrange("c p k -> p c k"), "tmA", tmA.rearrange("p (c k) -> p c k", k=128), eng="sp")
                stq("S_dtt", S_dtt[c0:c0 + ncn].rearrange("c p k -> p c k"), "tmD", tmD.rearrange("p (c k) -> p c k", k=128), eng="sp")
                E("pe", lambda e: e.transpose(out=bank(3, 128)[0:ncn, :], in_=totc[:, 0:ncn], identity=ident),
                  r=["totc", "ident"], w=pb(3))
                E("dve", lambda e: e.tensor_copy(out=totT[0:ncn, :], in_=bank(3, 128)[0:ncn, :]), r=pb(3), w=["totT"])
                stq("S_tot", S_tot[c0:c0 + ncn, :], "totT", totT[0:ncn, :], eng="sp")
            P.barrier()
            A.off = mark

        dt_set(S_a, 0, Ltot, 0)
        dt_set(S_ac, 0, Lc, NM + NA)
        P.barrier()
        if stop_after == "P1b":
            return _finish(nc, P, out_d, E, A, st)

        mark2 = A.off
        hTst = [A.f32(DSSD), A.f32(DSSD)]
        xtoks = [A.f32(DSSD), A.f32(DSSD)]
        btoks = [A.f32(1024), A.f32(1024)]
        btokb = A.bf16(1024)
        bTfs = [A.f32(1024), A.f32(1024)]
        cTfs = [A.f32(1024), A.f32(1024)]
        bTb = A.bf16(1024).rearrange("p (g t) -> p g t", g=8)
        cTb = A.bf16(1024).rearrange("p (g t) -> p g t", g=8)
        dtcs = [A.f32(64), A.f32(64)]
        acls = [A.f32(64), A.f32(64)]
        totbs = [A.f32(128), A.f32(128)]
        chunk_ctr = [0]
        decend = A.f32(64)
        wcol = A.f32(64)
        ein = A.f32(64)
        declast = A.f32(64)
        xdt = A.bf16(DSSD)
        xdd = A.bf16(DSSD)
        bcgs = [A.f32(1024), A.f32(1024)]
        segs = [A.f32(1024), A.f32(1024)]
        Egs = [A.f32(1024), A.f32(1024)]
        wTs = [A.bf16(1024).rearrange("p (r t) -> p r t", r=8), A.bf16(1024).rearrange("p (r t) -> p r t", r=8)]
        cbms = [A.f32(128), A.f32(128)]
        hTbf = A.bf16(DSSD)
        yacc = A.f32(DSSD)
        tmps = [A.f32(512), A.f32(512)]
        zTs = [A.f32(1024), A.f32(1024)]
        sz = A.f32(1024)
        ynT = A.bf16(DSSD).rearrange("p (f t) -> p f t", f=32)
        ssq = A.f32(8)

        E("dve", lambda e: e.memset(hTst[0], 0.0), w=["hT0"])
        E("dve", lambda e: e.memset(hTst[1], 0.0), w=["hT1"])

        def ssd_chunk(cid, row0, dr, need_y, fcol0, final):
            hT = hTst[dr]
            hn = "hT%d" % dr
            mask = maskU if dr == 0 else maskL
            mname = "maskU" if dr == 0 else "maskL"
            cp = chunk_ctr[0] % 2
            chunk_ctr[0] += 1
            xtok, btok, bTf, cTf, dtc, acl, totb = xtoks[cp], btoks[cp], bTfs[cp], cTfs[cp], dtcs[cp], acls[cp], totbs[cp]
            nxt, nbtk, nbTf, ncTf, ndtc, nacl, ntotb = ["%s%d" % (n_, cp) for n_ in ("xtok", "btok", "bTf", "cTf", "dtc", "acl", "totb")]
            ld(nxt, xtok, S_xt[row0:row0 + 128, :])
            ld(nbtk, btok, S_bt[row0:row0 + 128, :])
            ld(ndtc, dtc, S_dtt[cid][:, dr * 64:(dr + 1) * 64])
            ld(nacl, acl, S_act[cid][:, dr * 64:(dr + 1) * 64])
            ld(ntotb, totb, S_tot[cid:cid + 1, :].partition_broadcast(128))
            if need_y:
                ld(nbTf, bTf.rearrange("p (g t) -> p g t", g=8),
                   S_bT[:, fcol0:fcol0 + 128].rearrange("(g p) t -> p g t", p=128))
                ld(ncTf, cTf.rearrange("p (g t) -> p g t", g=8),
                   S_cT[:, fcol0:fcol0 + 128].rearrange("(g p) t -> p g t", p=128))
            td = totb[:, dr * 64:(dr + 1) * 64]
            E("dve", lambda e: e.tensor_tensor(out=decend, in0=td, in1=acl, op=ALU.subtract), r=[ntotb, nacl], w=["decend"])
            E("act", lambda e: e.activation(out=decend, in_=decend, func=AF.Exp), r=["decend"], w=["decend"])
            E("dve", lambda e: e.tensor_tensor(out=wcol, in0=dtc, in1=decend, op=ALU.mult), r=[ndtc, "decend"], w=["wcol"])
            E("act", lambda e: e.activation(out=declast, in_=td, func=AF.Exp), r=[ntotb], w=["declast"])
            x3 = xtok.rearrange("p (h d) -> p h d", d=64)
            E("dve", lambda e: e.tensor_tensor(out=xdd.rearrange("p (h d) -> p h d", d=64), in0=x3,
                                               in1=wcol.unsqueeze(2).to_broadcast([128, 64, 64]), op=ALU.mult),
              r=[nxt, "wcol"], w=["xdd"])
            E("act", lambda e: e.activation(out=btokb, in_=btok, func=AF.Copy), r=[nbtk], w=["btokb"])
            if need_y:
                E("act", lambda e: e.activation(out=bTb, in_=bTf.rearrange("p (g t) -> p g t", g=8), func=AF.Copy), r=[nbTf], w=["bTb"])
                E("act", lambda e: e.activation(out=cTb, in_=cTf.rearrange("p (g t) -> p g t", g=8), func=AF.Copy), r=[ncTf], w=["cTb"])
                E("act", lambda e: e.activation(out=ein, in_=acl, func=AF.Exp), r=[nacl], w=["ein"])
                E("pool", lambda e: e.tensor_tensor(out=xdt.rearrange("p (h d) -> p h d", d=64), in0=x3,
                                                    in1=dtc.unsqueeze(2).to_broadcast([128, 64, 64]), op=ALU.mult),
                  r=[nxt, ndtc], w=["xdt"])
                E("act", lambda e: e.activation(out=hTbf, in_=hT, func=AF.Copy), r=[hn], w=["hTbf"])
                if final:
                    ld("yacc", yacc, S_yb[row0:row0 + 128, :], r=["S_yb"])
                else:
                    E("dve", lambda e: e.tensor_tensor(out=yacc.rearrange("p (h d) -> p h d", d=64), in0=x3,
                                                       in1=Dbc.unsqueeze(2).to_broadcast([128, 64, 64]), op=ALU.mult),
                      r=[nxt, "Dbc"], w=["yacc"])

            def stage_a(g):
                gp = g % 2
                pb0 = 4 * gp
                seg, Eg, cbm, bcg = segs[gp], Egs[gp], cbms[gp], bcgs[gp]
                sgn, egn, cbn, bn = "seg%d" % gp, "Eg%d" % gp, "cbm%d" % gp, "bcg%d" % gp
                o0 = (dr * 64 + g * 8) * 128
                ld(bn, bcg, S_acc[cid:cid + 1, o0:o0 + 1024].partition_broadcast(128))
                E("pe", lambda e: e.matmul(bank(pb0, 128), lhsT=bTb[:, g, :], rhs=cTb[:, g, :], start=True, stop=True),
                  r=["bTb", "cTb"], w=pb(pb0))
                E("dve", lambda e: e.tensor_tensor(out=cbm, in0=bank(pb0, 128), in1=mask, op=ALU.mult), r=pb(pb0) + [mname], w=[cbn])
                for r_ in range(8):
                    hh = g * 8 + r_
                    E("dve", lambda e, r_=r_, hh=hh: e.tensor_scalar(
                        out=seg[:, r_ * 128:(r_ + 1) * 128], in0=bcg[:, r_ * 128:(r_ + 1) * 128],
                        scalar1=acl[:, hh:hh + 1], scalar2=0.0, op0=ALU.subtract, op1=ALU.min),
                      r=[bn, nacl], w=[sgn])
                E("act", lambda e: e.activation(out=Eg, in_=seg, func=AF.Exp), r=[sgn], w=[egn])

            def stage_b(g):
                gp = g % 2
                pb0 = 4 * gp
                gs = slice(g * 512, (g + 1) * 512)
                Eg, cbm, wT, tmp = Egs[gp], cbms[gp], wTs[gp], tmps[gp]
                egn, cbn, wtn, tn = "Eg%d" % gp, "cbm%d" % gp, "wT%d" % gp, "tmp%d" % gp
                if need_y:
                    E("dve", lambda e: e.tensor_tensor(out=wT, in0=Eg.rearrange("p (r t) -> p r t", r=8),
                                                       in1=cbm.unsqueeze(1).to_broadcast([128, 8, 128]), op=ALU.mult),
                      r=[egn, cbn], w=[wtn])
                    for r_ in range(8):
                        hh = g * 8 + r_
                        E("pe", lambda e, r_=r_, hh=hh: e.matmul(bank(pb0 + 1, 64, r_ * 64), lhsT=wT[:, r_, :],
                                                                 rhs=xdt[:, hh * 64:(hh + 1) * 64], start=True, stop=True),
                          r=[wtn, "xdt"], w=pb(pb0 + 1))
                    E("pe", lambda e: e.matmul(bank(pb0 + 2), lhsT=cTb[:, g, :], rhs=hTbf[:, gs], start=True, stop=True),
                      r=["cTb", "hTbf"], w=pb(pb0 + 2))
                    E("dve", lambda e: e.tensor_tensor(
                        out=tmp.rearrange("p (r d) -> p r d", d=64), in0=bank(pb0 + 2).rearrange("p (r d) -> p r d", d=64),
                        in1=ein[:, g * 8:(g + 1) * 8].unsqueeze(2).to_broadcast([128, 8, 64]), op=ALU.mult),
                      r=pb(pb0 + 2) + ["ein"], w=[tn])
                    E("pool", lambda e: e.tensor_tensor(out=yacc[:, gs], in0=yacc[:, gs], in1=tmp, op=ALU.add),
                      r=[tn, "yacc"], w=["yacc"])
                    E("dve", lambda e: e.tensor_tensor(out=yacc[:, gs], in0=yacc[:, gs], in1=bank(pb0 + 1), op=ALU.add),
                      r=pb(pb0 + 1) + ["yacc"], w=["yacc"])
                E("pe", lambda e: e.matmul(bank(pb0 + 3), lhsT=btokb[:, g * 128:(g + 1) * 128], rhs=xdd[:, gs], start=True, stop=True),
                  r=["btokb", "xdd"], w=pb(pb0 + 3))
                E("pool", lambda e: e.tensor_tensor(
                    out=hT[:, gs].rearrange("p (r d) -> p r d", d=64), in0=hT[:, gs].rearrange("p (r d) -> p r d", d=64),
                    in1=declast[:, g * 8:(g + 1) * 8].unsqueeze(2).to_broadcast([128, 8, 64]), op=ALU.mult),
                  r=[hn, "declast", "hTbf"], w=[hn])
                E("dve", lambda e: e.tensor_tensor(out=hT[:, gs], in0=hT[:, gs], in1=bank(pb0 + 3), op=ALU.add),
                  r=pb(pb0 + 3) + [hn], w=[hn])

            if need_y:
                stage_a(0)
            for g in range(8):
                if need_y and g + 1 < 8:
                    stage_a(g + 1)
                stage_b(g)
            if need_y and not final:
                stq("S_yb", S_yb[row0:row0 + 128, :], "yacc", yacc, eng="sp")
            if final:
                for q in range(4):
                    zT = zTs[q % 2]
                    zn = "zT%d" % (q % 2)
                    ld(zn, zT.rearrange("p (f t) -> p f t", f=8),
                       S_b[q * 1024:(q + 1) * 1024, fcol0:fcol0 + 128].rearrange("(f p) t -> p f t", p=128))
                    for f in range(8):
                        E("pe", lambda e, f=f, zT=zT: e.transpose(out=PS[:, 2048 + f * 128:2048 + (f + 1) * 128],
                                                                  in_=zT[:, f * 128:(f + 1) * 128], identity=ident),
                          r=[zn, "ident"], w=pb(4 + f // 4))
                    E("act", lambda e: e.activation(out=sz, in_=PS[:, 2048:3072], func=AF.Silu), r=pb(4, 5), w=["sz"])
                    qs = slice(q * 1024, (q + 1) * 1024)
                    E("dve", lambda e, qs=qs: e.tensor_tensor(out=yacc[:, qs], in0=yacc[:, qs], in1=sz, op=ALU.mult),
                      r=["sz", "yacc"], w=["yacc"])
                    E("act", lambda e, qs=qs, q=q: e.activation(out=sz, in_=yacc[:, qs], func=AF.Square, accum_out=ssq[:, q:q + 1]),
                      r=["yacc", "sz"], w=["sz", "ssq"])
                E("dve", lambda e: e.tensor_reduce(out=ssq[:, 4:5], in_=ssq[:, 0:4], axis=AX.X, op=ALU.add), r=["ssq"], w=["ssq"])
                E("dve", lambda e: e.tensor_scalar(out=ssq[:, 5:6], in0=ssq[:, 4:5], scalar1=1.0 / DSSD, scalar2=EPS,
                                                   op0=ALU.mult, op1=ALU.add), r=["ssq"], w=["ssq"])
                E("act", lambda e: e.activation(out=ssq[:, 6:7], in_=ssq[:, 5:6], func=AF.Sqrt), r=["ssq"], w=["ssq"])
                E("dve", lambda e: e.reciprocal(out=ssq[:, 7:8], in_=ssq[:, 6:7]), r=["ssq"], w=["ssq"])
                E("act", lambda e: e.activation(out=yacc, in_=yacc, func=AF.Copy, scale=ssq[:, 7:8]), r=["yacc", "ssq"], w=["yacc"])
                for half in range(2):
                    for f in range(16):
                        ff = half * 16 + f
                        E("pe", lambda e, f=f, ff=ff: e.transpose(out=PS[:, 2048 + f * 128:2048 + (f + 1) * 128],
                                                                  in_=yacc[:, ff * 128:(ff + 1) * 128], identity=ident),
                          r=["yacc", "ident"], w=pb(4 + f // 4))
                    E("act", lambda e, half=half: e.activation(out=ynT[:, half * 16:(half + 1) * 16, :],
                                                               in_=PS[:, 2048:4096].rearrange("p (f t) -> p f t", f=16), func=AF.Copy),
                      r=pb(4, 5, 6, 7), w=["ynT"])
                stq("S_yn", S_yn[:, fcol0:fcol0 + 128].rearrange("(f p) t -> p f t", p=128), "ynT", ynT, eng="sp")

        for c in range(NC_):
            ssd_chunk(NM + NA + c, Ltot + c * 128, 0, False, 0, False)
        for c in reversed(range(NC_)):
            ssd_chunk(NM + NA + c, Ltot + c * 128, 1, False, 0, False)
        for c in reversed(range(NA)):
            ssd_chunk(NM + c, Lm + c * 128, 1, False, 0, False)
        for c in reversed(range(NM)):
            ssd_chunk(c, c * 128, 1, True, c * 128, False)
        for c in range(NM):
            ssd_chunk(c, c * 128, 0, True, c * 128, True)
        if "S_hst" in dbg:
            stq("S_hst", S_hst[0], "hT0", hTst[0], eng="sp")
            stq("S_hst", S_hst[1], "hT1", hTst[1], eng="sp")
        P.barrier()
        A.off = mark2
        if stop_after == "P2":
            return _finish(nc, P, out_d, E, A, st)

        mark3 = A.off
        vT = A.bf16(16 * T3).rearrange("p (k t) -> p k t", k=16)
        ynB = A.bf16(32 * T3).rearrange("p (k t) -> p k t", k=32)
        mrgT = A.bf16(16 * T3).rearrange("p (k t) -> p k t", k=16)
        oTs = A.f32(16 * T3).rearrange("p (k t) -> p k t", k=16)
        wA32_1 = A.f32(4096)
        wA32 = [wA32_1, wA32_1]
        wAb = [A.bf16(4096), A.bf16(4096)]
        wB32 = [A.f32(2048), A.f32(2048)]
        wBb = [A.bf16(2048), A.bf16(2048)]
        tb_ = [A.f32(T3)] * 2
        tc_ = [A.f32(T3)] * 2
        tx_ = [A.f32(T3)] * 2
        uu = A.f32(T3)
        cu = A.f32(T3)
        gsl = [A.f32(T3), A.f32(T3)]
        gcl = [A.f32(T3), A.f32(T3)]
        m1 = A.f32(T3)
        m2 = A.f32(T3)
        xt3 = A.f32(2048)
        tmp3 = A.f32(2048)
        g1bc = A.f32(2048)
        ld("g1bc", g1bc, S_vec[32:48, :].rearrange("(o a) b -> o (a b)", o=1).partition_broadcast(128))
        R = T3 // 64
        for blk in range(Lm // T3):
            t0 = blk * T3
            ld("ynB", ynB, S_yn[:, t0:t0 + T3].rearrange("(f p) t -> p f t", p=128))
            for dc in range(16):
                tb, tc, tx = tb_[dc % 2], tc_[dc % 2], tx_[dc % 2]
                nbn, ncn2, nxn = "tb0", "tc0", "tx0"
                ld(nbn, tb, S_b[(32 + dc) * 128:(33 + dc) * 128, t0:t0 + T3])
                ld(ncn2, tc, S_b[(48 + dc) * 128:(49 + dc) * 128, t0:t0 + T3])
                ld(nxn, tx, S_b[(64 + dc) * 128:(65 + dc) * 128, t0:t0 + T3])
                E("dve", lambda e, tc=tc, tx=tx: e.tensor_tensor(out=uu, in0=tc, in1=tx, op=ALU.mult), r=[ncn2, nxn], w=["uu"])
                E("dve", lambda e, dc=dc: e.tensor_scalar(out=cu, in0=uu, scalar1=scw[:, dc * 3 + 1:dc * 3 + 2], scalar2=None, op0=ALU.mult),
                  r=["uu", "scw"], w=["cu"])
                u3 = uu.rearrange("p (r w) -> p r w", w=64)
                c3 = cu.rearrange("p (r w) -> p r w", w=64)
                E("dve", lambda e, dc=dc: e.scalar_tensor_tensor(out=c3[:, :, 1:64], in0=u3[:, :, 0:63], scalar=scw[:, dc * 3:dc * 3 + 1],
                                                                 in1=c3[:, :, 1:64], op0=ALU.mult, op1=ALU.add),
                  r=["uu", "scw", "cu"], w=["cu"])
                E("dve", lambda e, dc=dc: e.scalar_tensor_tensor(out=c3[:, :, 0:63], in0=u3[:, :, 1:64], scalar=scw[:, dc * 3 + 2:dc * 3 + 3],
                                                                 in1=c3[:, :, 0:63], op0=ALU.mult, op1=ALU.add),
                  r=["uu", "scw", "cu"], w=["cu"])
                E("dve", lambda e, dc=dc, tb=tb: e.tensor_tensor(out=vT[:, dc, :], in0=tb, in1=cu, op=ALU.mult), r=[nbn, "cu"], w=["vT"])
            for oc in range(16):
                wa, wab, wbt, wbb = wA32[oc % 2], wAb[oc % 2], wB32[oc % 2], wBb[oc % 2]
                na, nab, nbt, nbb = "wA0", "wAb%d" % (oc % 2), "wB%d" % (oc % 2), "wBb%d" % (oc % 2)
                ld(na, wa, wssd_d[oc])
                ld(nbt, wbt, wsc_d[oc])
                E("dve", lambda e, wa=wa, wab=wab: e.tensor_tensor(out=wab.rearrange("p (k m) -> p k m", k=32),
                                                                   in0=wa.rearrange("p (k m) -> p k m", k=32),
                                                                   in1=gncol.unsqueeze(2).to_broadcast([128, 32, 128]), op=ALU.mult),
                  r=[na, "gncol"], w=[nab])
                E("act", lambda e, wbt=wbt, wbb=wbb: e.activation(out=wbb, in_=wbt, func=AF.Copy), r=[nbt], w=[nbb])
                for kc in range(32):
                    E("pe", lambda e, wab=wab, kc=kc: e.matmul(bank(0, T3), lhsT=wab[:, kc * 128:(kc + 1) * 128], rhs=ynB[:, kc, :],
                                                               start=(kc == 0), stop=(kc == 31)), r=[nab, "ynB"], w=pb(0))
                for kc in range(16):
                    E("pe", lambda e, wbb=wbb, kc=kc: e.matmul(bank(1, T3), lhsT=wbb[:, kc * 128:(kc + 1) * 128], rhs=vT[:, kc, :],
                                                               start=(kc == 0), stop=(kc == 15)), r=[nbb, "vT"], w=pb(1))
                gsv, gcv = gsl[oc % 2], gcl[oc % 2]
                ngs, ngc = "gs%d" % (oc % 2), "gc%d" % (oc % 2)
                ld(ngs, gsv, S_b[(80 + oc) * 128:(81 + oc) * 128, t0:t0 + T3])
                ld(ngc, gcv, S_b[(96 + oc) * 128:(97 + oc) * 128, t0:t0 + T3])
                E("act", lambda e, gsv=gsv: e.activation(out=gsv, in_=gsv, func=AF.Sigmoid), r=[ngs], w=[ngs])
                E("act", lambda e, gcv=gcv: e.activation(out=gcv, in_=gcv, func=AF.Sigmoid), r=[ngc], w=[ngc])
                E("dve", lambda e, gsv=gsv: e.tensor_tensor(out=m1, in0=bank(0, T3), in1=gsv, op=ALU.mult), r=pb(0) + [ngs], w=["m1"])
                E("dve", lambda e, gcv=gcv: e.tensor_tensor(out=m2, in0=bank(1, T3), in1=gcv, op=ALU.mult), r=pb(1) + [ngc], w=["m2"])
                E("pool", lambda e, oc=oc: e.tensor_tensor(out=mrgT[:, oc, :], in0=m1, in1=m2, op=ALU.add), r=["m1", "m2"], w=["mrgT"])
            for oc in range(16):
                wbt, wbb = wB32[oc % 2], wBb[oc % 2]
                nbt, nbb = "wB%d" % (oc % 2), "wBb%d" % (oc % 2)
                ld(nbt, wbt, wo_d[oc])
                E("act", lambda e, wbt=wbt, wbb=wbb: e.activation(out=wbb, in_=wbt, func=AF.Copy), r=[nbt], w=[nbb])
                for kc in range(16):
                    E("pe", lambda e, wbb=wbb, kc=kc, oc=oc: e.matmul(bank(2 + oc % 2, T3), lhsT=wbb[:, kc * 128:(kc + 1) * 128],
                                                                      rhs=mrgT[:, kc, :], start=(kc == 0), stop=(kc == 15)),
                      r=[nbb, "mrgT"], w=pb(2 + oc % 2))
                E("act", lambda e, oc=oc: e.activation(out=oTs[:, oc, :], in_=bank(2 + oc % 2, T3), func=AF.Copy),
                  r=pb(2 + oc % 2), w=["oTs"])
            for tt in range(T3 // 128):
                ld("xt3", xt3, xm[t0 + tt * 128:t0 + (tt + 1) * 128, :])
                for oc in range(16):
                    E("pe", lambda e, oc=oc, tt=tt: e.transpose(out=PS[:, 2048 + oc * 128:2048 + (oc + 1) * 128],
                                                                in_=oTs[:, oc, tt * 128:(tt + 1) * 128], identity=ident),
                      r=["oTs", "ident"], w=pb(4 + oc // 4))
                E("dve", lambda e: e.tensor_tensor(out=tmp3, in0=PS[:, 2048:4096], in1=g1bc, op=ALU.mult), r=pb(4, 5, 6, 7) + ["g1bc"], w=["tmp3"])
                E("pool", lambda e: e.tensor_tensor(out=tmp3, in0=tmp3, in1=xt3, op=ALU.add), r=["tmp3", "xt3"], w=["tmp3"])
                stq("S_x1", S_x1[t0 + tt * 128:t0 + (tt + 1) * 128, :], "tmp3", tmp3, eng="sp")
        P.barrier()
        A.off = mark3
        if stop_after == "P3":
            return _finish(nc, P, out_d, E, A, st)

        x1ts = [A.f32(2048), A.f32(2048)]
        h2f = A.f32(2048)
        h2bs = [A.bf16(2048), A.bf16(2048)]
        junk4 = A.bf16(2048)
        junkb = A.bf16(2048)
        h2T = A.bf16(2048).rearrange("p (k t) -> p k t", k=16)
        wq32 = [A.f32(2048), A.f32(2048)]
        wqb = [A.bf16(2048), A.bf16(2048)]
        qT = A.bf16(2048).rearrange("p (c t) -> p c t", c=16)
        k32 = A.f32(2048)
        keysb = A.bf16(2048).rearrange("p (c n) -> p c n", c=16)
        sc_ = A.f32(2048)
        wk = A.f32(256)
        sv = A.f32(256)
        si = A.u32(256)
        sif = A.f32(256)
        cs = A.f32(256)
        ci = A.f32(256)
        ts_ = A.f32(16)
        pos = A.u32(16)
        posf = A.f32(16)
        ohs = [A.f32(256), A.f32(256)]
        wv = A.f32(128)
        prodj = [A.bf16(2048), A.bf16(2048)]
        junka = A.bf16(2048)
        idxf = A.f32(128)
        idxus = [A.u32(128), A.u32(128)]
        gates = [A.f32(128), A.f32(128)]
        actv = A.f32(128)
        gact = A.f32(128)
        sm = A.f32(8)
        smf = A.f32(8)
        NB = 4
        UVg = [A.bf16(4096) for _ in range(NB)]
        dg = [A.bf16(128), A.bf16(128)]
        a2bc = A.f32(2048)
        sh2bc = A.f32(2048)
        g2bc = A.f32(2048)
        fgbc = A.f32(2048)
        x2 = A.f32(2048)
        ld("a2bc", a2bc, S_vec[0:16, :].rearrange("(o a) b -> o (a b)", o=1).partition_broadcast(128))
        ld("sh2bc", sh2bc, S_vec[16:32, :].rearrange("(o a) b -> o (a b)", o=1).partition_broadcast(128))
        ld("g2bc", g2bc, S_vec[48:64, :].rearrange("(o a) b -> o (a b)", o=1).partition_broadcast(128))
        ld("fgbc", fgbc, fg_d.partition_broadcast(128))
        ld("k32", k32, keys_d)
        E("act", lambda e: e.activation(out=keysb, in_=k32.rearrange("p (c n) -> p c n", c=16), func=AF.Copy), r=["k32"], w=["keysb"])

        def rstd_of(src_name, src, jnk, jn, smt, smn):
            E("act", lambda e: e.activation(out=jnk, in_=src, func=AF.Square, accum_out=smt[:, 0:1]), r=[src_name], w=[jn, smn])
            E("dve", lambda e: e.tensor_scalar(out=smt[:, 1:2], in0=smt[:, 0:1], scalar1=1.0 / D, scalar2=EPS, op0=ALU.mult, op1=ALU.add),
              r=[smn], w=[smn])
            E("act", lambda e: e.activation(out=smt[:, 2:3], in_=smt[:, 1:2], func=AF.Sqrt), r=[smn], w=[smn])
            E("dve", lambda e: e.reciprocal(out=smt[:, 3:4], in_=smt[:, 2:3]), r=[smn], w=[smn])

        NT = Lm // 128

        def peer_setup(tl):
            par = tl % 2
            t0 = tl * 128
            x1t, h2b, idxu, gate = x1ts[par], h2bs[par], idxus[par], gates[par]
            xn, hbn, iun, gtn = "x1t%d" % par, "h2b%d" % par, "idxu%d" % par, "gate%d" % par
            ld(xn, x1t, S_x1[t0:t0 + 128, :])
            rstd_of(xn, x1t, junk4, "junk4", sm, "sm")
            E("dve", lambda e: e.scalar_tensor_tensor(out=h2f, in0=x1t, scalar=sm[:, 3:4], in1=a2bc, op0=ALU.mult, op1=ALU.mult),
              r=[xn, "sm", "a2bc"], w=["h2f"])
            E("pool", lambda e: e.tensor_tensor(out=h2f, in0=h2f, in1=sh2bc, op=ALU.add), r=["h2f", "sh2bc"], w=["h2f"])
            E("act", lambda e: e.activation(out=h2b, in_=h2f, func=AF.Copy), r=["h2f"], w=[hbn])
            for dc in range(16):
                E("pe", lambda e, dc=dc: e.transpose(out=PS[:, dc * 128:(dc + 1) * 128], in_=h2f[:, dc * 128:(dc + 1) * 128], identity=ident),
                  r=["h2f", "ident"], w=pb(dc // 4))
            E("act", lambda e: e.activation(out=h2T, in_=PS[:, 0:2048].rearrange("p (k t) -> p k t", k=16), func=AF.Copy),
              r=pb(0, 1, 2, 3), w=["h2T"])
            for c in range(16):
                w32, wb = wq32[c % 2], wqb[c % 2]
                n32, nb = "wq%d" % (c % 2), "wqb%d" % (c % 2)
                ld(n32, w32, wq_d[c])
                E("act", lambda e, w32=w32, wb=wb: e.activation(out=wb, in_=w32, func=AF.Copy), r=[n32], w=[nb])
                for kc in range(16):
                    E("pe", lambda e, wb=wb, kc=kc, c=c: e.matmul(PS[:, c * 128:(c + 1) * 128], lhsT=wb[:, kc * 128:(kc + 1) * 128],
                                                                  rhs=h2T[:, kc, :], start=(kc == 0), stop=(kc == 15)),
                      r=[nb, "h2T"], w=pb(c // 4))
            E("act", lambda e: e.activation(out=qT, in_=PS[:, 0:2048].rearrange("p (c t) -> p c t", c=16), func=AF.Copy),
              r=pb(0, 1, 2, 3), w=["qT"])
            for c in range(16):
                E("pe", lambda e, c=c: e.matmul(PS[:, c * 128:(c + 1) * 128], lhsT=qT[:, c, :], rhs=keysb[:, c, :], start=True, stop=True),
                  r=["qT", "keysb"], w=pb(c // 4))
            E("act", lambda e: e.activation(out=sc_, in_=PS[:, 0:2048], func=AF.Copy), r=pb(0, 1, 2, 3), w=["sc"])
            for c in range(16):
                cs_ = slice(c * 128, (c + 1) * 128)
                v0 = slice(c * 16, c * 16 + 8)
                v1 = slice(c * 16 + 8, c * 16 + 16)
                E("dve", lambda e, cs_=cs_, v0=v0: e.max(out=sv[:, v0], in_=sc_[:, cs_]), r=["sc"], w=["sv"])
                E("dve", lambda e, cs_=cs_, v0=v0: e.max_index(out=si[:, v0], in_max=sv[:, v0], in_values=sc_[:, cs_]), r=["sc", "sv"], w=["si"])
                E("dve", lambda e, cs_=cs_, v0=v0: e.match_replace(out=wk[:, 0:128], in_to_replace=sv[:, v0], in_values=sc_[:, cs_], imm_value=-1e30),
                  r=["sc", "sv"], w=["wk"])
                E("dve", lambda e, v1=v1: e.max(out=sv[:, v1], in_=wk[:, 0:128]), r=["wk"], w=["sv"])
                E("dve", lambda e, v1=v1: e.max_index(out=si[:, v1], in_max=sv[:, v1], in_values=wk[:, 0:128]), r=["wk", "sv"], w=["si"])
            E("dve", lambda e: e.tensor_copy(out=sif, in_=si), r=["si"], w=["sif"])
            for h in range(8):
                s1 = sv[:, (2 * h) * 16:(2 * h) * 16 + 16]
                s2 = sv[:, (2 * h + 1) * 16:(2 * h + 1) * 16 + 16]
                i1 = sif[:, (2 * h) * 16:(2 * h) * 16 + 16]
                i2 = sif[:, (2 * h + 1) * 16:(2 * h + 1) * 16 + 16]
                cs3 = cs.rearrange("p (a b) -> p a b", a=16)
                ci3 = ci.rearrange("p (a b) -> p a b", a=16)
                E("dve", lambda e, s1=s1, s2=s2: e.tensor_tensor(out=cs3, in0=s1.unsqueeze(2).to_broadcast([128, 16, 16]),
                                                                 in1=s2.unsqueeze(1).to_broadcast([128, 16, 16]), op=ALU.add),
                  r=["sv"], w=["cs"])
                E("dve", lambda e, i1=i1, i2=i2: e.scalar_tensor_tensor(out=ci3, in0=i1.unsqueeze(2).to_broadcast([128, 16, 16]), scalar=128.0,
                                                                        in1=i2.unsqueeze(1).to_broadcast([128, 16, 16]),
                                                                        op0=ALU.mult, op1=ALU.add), r=["sif"], w=["ci"])
                E("dve", lambda e: e.max(out=ts_[:, 0:8], in_=cs), r=["cs"], w=["ts"])
                E("dve", lambda e: e.max_index(out=pos[:, 0:8], in_max=ts_[:, 0:8], in_values=cs), r=["cs", "ts"], w=["pos"])
                E("dve", lambda e: e.match_replace(out=wk, in_to_replace=ts_[:, 0:8], in_values=cs, imm_value=-1e30), r=["cs", "ts"], w=["wk"])
                E("dve", lambda e: e.max(out=ts_[:, 8:16], in_=wk), r=["wk"], w=["ts"])
                E("dve", lambda e: e.max_index(out=pos[:, 8:16], in_max=ts_[:, 8:16], in_values=wk), r=["wk", "ts"], w=["pos"])
                E("dve", lambda e: e.tensor_copy(out=posf, in_=pos), r=["pos"], w=["posf"])
                for k in range(16):
                    oh = ohs[k % 2]
                    ohn = "oh%d" % (k % 2)
                    E("dve", lambda e, k=k, oh=oh: e.tensor_scalar(out=oh, in0=io256, scalar1=posf[:, k:k + 1], scalar2=None, op0=ALU.is_equal),
                      r=["io256", "posf"], w=[ohn])
                    E("dve", lambda e, k=k, h=h, oh=oh: e.scalar_tensor_tensor(out=oh, in0=oh, scalar=1.0, in1=ci, op0=ALU.mult, op1=ALU.mult,
                                                                               accum_out=idxf[:, h * 16 + k:h * 16 + k + 1]),
                      r=[ohn, "ci"], w=[ohn, "idxf"])
                E("dve", lambda e: e.tensor_single_scalar(out=sm[:, 4:5], in_=ts_[:, 0:1], scalar=-1.0, op=ALU.mult), r=["ts"], w=["sm4"])
                E("act", lambda e, h=h: e.activation(out=gate[:, h * 16:(h + 1) * 16], in_=ts_, func=AF.Exp, bias=sm[:, 4:5],
                                                     accum_out=sm[:, 5:6]), r=["ts", "sm4"], w=[gtn, "sm5"])
                E("dve", lambda e: e.reciprocal(out=sm[:, 6:7], in_=sm[:, 5:6]), r=["sm5"], w=["sm6"])
                E("dve", lambda e, h=h: e.tensor_scalar(out=gate[:, h * 16:(h + 1) * 16], in0=gate[:, h * 16:(h + 1) * 16],
                                                        scalar1=sm[:, 6:7], scalar2=None, op0=ALU.mult), r=[gtn, "sm6"], w=[gtn])
            E("dve", lambda e: e.tensor_copy(out=idxu, in_=idxf), r=["idxf"], w=[iun])

        def peer_slots(tl):
            par = tl % 2
            t0 = tl * 128
            x1t, h2b, idxu, gate = x1ts[par], h2bs[par], idxus[par], gates[par]
            xn, hbn, iun, gtn = "x1t%d" % par, "h2b%d" % par, "idxu%d" % par, "gate%d" % par
            for s in range(128):
                uv, d_ = UVg[s % NB], dg[s % 2]
                ug, vg = uv[:, 0:2048], uv[:, 2048:4096]
                un, dn = "UV%d" % (s % NB), "dg%d" % (s % 2)
                vn = un
                an, gn_ = "actv%d" % (s % 8), "gact%d" % (s % 8)
                E("pool", lambda e, uv=uv, s=s: e.indirect_dma_start(out=uv, out_offset=None, in_=S_uv,
                                                                     in_offset=bass.IndirectOffsetOnAxis(ap=idxu[:, s:s + 1], axis=0)),
                  r=[iun], w=[un], dma=True)
                wn_ = "wv%d" % (s % 8)
                E("dve", lambda e, ug=ug, s=s: e.scalar_tensor_tensor(out=junkb, in0=ug, scalar=1.0, in1=h2b, op0=ALU.mult, op1=ALU.mult,
                                                                      accum_out=actv[:, s:s + 1]), r=[un, hbn], w=["junkb", an])
                E("act", lambda e, s=s: e.activation(out=gact[:, s:s + 1], in_=actv[:, s:s + 1], func=AF.Gelu), r=[an], w=[gn_])
                E("act", lambda e, s=s: e.activation(out=wv[:, s:s + 1], in_=gact[:, s:s + 1], func=AF.Copy, scale=gate[:, s:s + 1]),
                  r=[gn_, gtn], w=[wn_])
                E("act", lambda e, d_=d_, s=s: e.activation(out=d_, in_=ident, func=AF.Copy, scale=wv[:, s:s + 1]),
                  r=["ident", wn_], w=[dn])
                for n in range(4):
                    E("pe", lambda e, d_=d_, vg=vg, n=n, s=s: e.matmul(bank(4 + n), lhsT=d_, rhs=vg[:, n * 512:(n + 1) * 512],
                                                                       start=(s == 0), stop=(s == 127)), r=[dn, vn], w=pb(4 + n))
            E("dve", lambda e: e.tensor_tensor(out=x2, in0=PS[:, 2048:4096], in1=g2bc, op=ALU.mult), r=pb(4, 5, 6, 7) + ["g2bc"], w=["x2"])
            E("pool", lambda e: e.tensor_tensor(out=x2, in0=x2, in1=x1t, op=ALU.add), r=["x2", xn], w=["x2"])
            rstd_of("x2", x2, junk4, "junk4", smf, "smf")
            E("dve", lambda e: e.scalar_tensor_tensor(out=x2, in0=x2, scalar=smf[:, 3:4], in1=fgbc, op0=ALU.mult, op1=ALU.mult),
              r=["x2", "smf", "fgbc"], w=["x2"])
            stq("out", out_d[t0:t0 + 128, :], "x2", x2, eng="sp")

        peer_setup(0)
        for tl in range(NT):
            if tl + 1 < NT:
                peer_setup(tl + 1)
            peer_slots(tl)
        return _finish(nc, P, out_d, E, A, st)


def _finish(nc, P, out_d, E, A, st):
    P.barrier()
    P.replay()
    return nc


def _wl(w, kc):
    K, N = w.shape
    return np.ascontiguousarray(w.reshape(kc, 128, N // 128, 128).transpose(2, 1, 0, 3).reshape(N // 128, 128, kc * 128))


def _col(v, n):
    return np.ascontiguousarray(v.reshape(n, 128).T)


def make_in_maps(inp, n_cores, Lh):
    f = lambda a: np.asarray(a, dtype=np.float32)
    x, c, ctx, c_ctx = f(inp["x"]), f(inp["c"]), f(inp["ctx"]), f(inp["c_ctx"])
    w_in = f(inp["w_in"])[0]
    OFF_DT = 6144
    w_in_sw = w_in.copy()
    w_in_sw[:, OFF_DT:OFF_DT + 64] = w_in[:, OFF_DT + 64:OFF_DT + 128]
    w_in_sw[:, OFF_DT + 64:OFF_DT + 128] = w_in[:, OFF_DT:OFF_DT + 64]
    w_in_l = [_wl(w_in, 16), _wl(w_in_sw, 16)]
    w_ada_l = _wl(f(inp["w_ada"])[0], 16)
    b_ada_l = _col(f(inp["b_ada"])[0], 96)
    n1g = _col(f(inp["norm1_g"])[0], 16)
    n2g = _col(f(inp["norm2_g"])[0], 16)
    cw = f(inp["ssd_conv_w"])[0]
    cwl = [np.ascontiguousarray(cw.T.reshape(48, 128, 5).transpose(1, 0, 2).reshape(128, 240)),
           np.ascontiguousarray(cw[::-1].T.reshape(48, 128, 5).transpose(1, 0, 2).reshape(128, 240))]
    cb = _col(f(inp["ssd_conv_b"])[0], 48)
    dtb = f(inp["ssd_dt_bias"])[0].reshape(128, 1)
    alog = f(inp["ssd_A_log"])[0].reshape(128, 1)
    dtb_l = [dtb, np.ascontiguousarray(np.concatenate([dtb[64:], dtb[:64]], 0))]
    alog_l = [alog, np.ascontiguousarray(np.concatenate([alog[64:], alog[:64]], 0))]
    ssdD = f(inp["ssd_D"])[0].reshape(1, 64)
    gn = _col(f(inp["ssd_norm_g"])[0], 32)
    wssd = _wl(f(inp["ssd_w_out"])[0], 32)
    sw = f(inp["sc_conv_w"])[0]
    swl = [np.ascontiguousarray(sw.T.reshape(16, 128, 3).transpose(1, 0, 2).reshape(128, 48)),
           np.ascontiguousarray(sw[::-1].T.reshape(16, 128, 3).transpose(1, 0, 2).reshape(128, 48))]
    wsc = _wl(f(inp["sc_w_out"])[0], 16)
    wo = _wl(f(inp["w_o"])[0], 16)
    wq = _wl(f(inp["peer_w_q"])[0], 16)
    keys = f(inp["peer_keys"])[0]
    keysT = np.ascontiguousarray(keys.reshape(16, 128, 128).transpose(2, 0, 1).reshape(128, 2048))
    pu = np.ascontiguousarray(f(inp["peer_u"])[0])
    pv = np.ascontiguousarray(f(inp["peer_v"])[0])
    fg = f(inp["final_g"]).reshape(1, D)
    maps = []
    for core in range(n_cores):
        b, half = core // 2, core % 2
        xb = x[b]
        if half == 0:
            xm_, xa_, xc_ = xb[:Lh], xb[Lh:2 * Lh], ctx[b]
        else:
            rv = xb[:2 * Lh][::-1]
            xm_, xa_, xc_ = rv[:Lh], rv[Lh:], ctx[b][::-1]
        cvec = np.stack([c[b], c_ctx], -1).reshape(16, 128, 2).transpose(1, 0, 2).reshape(128, 32)
        maps.append({
            "xm": np.ascontiguousarray(xm_), "xa": np.ascontiguousarray(xa_), "xc": np.ascontiguousarray(xc_),
            "cvec": np.ascontiguousarray(cvec), "w_ada": w_ada_l, "b_ada": b_ada_l, "n1g": n1g, "n2g": n2g,
            "w_in": w_in_l[half], "convw": cwl[half], "convb": cb, "dtb": dtb_l[half], "alog": alog_l[half],
            "ssdD": ssdD, "gnorm": gn, "wssd": wssd, "scw": swl[half], "wsc": wsc, "wo": wo, "wq": wq,
            "keysT": keysT, "peer_u": pu, "peer_v": pv, "final_g": fg,
        })
    return maps


def kernel(**inputs):
    B, S, _ = inputs["x"].shape
    Lh = S // 2
    n_cores = 2 * B
    nc = build_nc(Lh, Lh, inputs["ctx"].shape[1])
    maps = make_in_maps(inputs, n_cores, Lh)
    res = run_bass_kernel_spmd(nc, maps, core_ids=list(range(n_cores)))
    out = np.empty((B, S, D), np.float32)
    for core in range(n_cores):
        b, half = core // 2, core % 2
        o = res.results[core]["out"]
        if half == 0:
            out[b, :Lh] = o
        else:
            out[b, Lh:] = o[::-1]
    return out
```

```python
import numpy as np
from contextlib import ExitStack
import concourse.bass as bass
import concourse.mybir as mybir
from concourse.bass_utils import run_bass_kernel_spmd

F32 = mybir.dt.float32
BF16 = mybir.dt.bfloat16
U32 = mybir.dt.uint32
I32 = mybir.dt.int32
AF = mybir.ActivationFunctionType
ALU = mybir.AluOpType
AX = mybir.AxisListType

D = 2048
DC = 16
DSSD = 4096
NH = 64
EPS = 1e-6
NG_IN = 161
ENGS = ("pe", "act", "dve", "pool", "sp")
DMA_ENGS = ("sp", "pool")


class Buf:
    __slots__ = ("w", "r")

    def __init__(self):
        self.w = {}
        self.r = {}


class Prog:
    def __init__(self, nc, stack, n_slots=8):
        self.nc = nc
        self.stack = stack
        self.ops = {e: [] for e in ENGS}
        self.cnt = {}
        self.known = {e: {} for e in ENGS}
        self.sems = {}
        self.cur = {}
        self.epoch = 0
        for e in ("pe", "act", "dve", "pool"):
            self._new_compute_sem(e)
        self.slots = {}
        self.slot_next = {}
        for e in DMA_ENGS:
            self.slots[e] = []
            self.slot_next[e] = 0
            for i in range(n_slots):
                k = "d_%s_%d" % (e, i)
                self.sems[k] = stack.enter_context(nc.semaphore(k))
                self.cnt[k] = 0
                self.slots[e].append(k)
        self.bufs = {}
        self.n_ops = 0

    def _new_compute_sem(self, e):
        k = "c_%s_%d" % (e, self.epoch)
        self.sems[k] = self.stack.enter_context(self.nc.semaphore(k))
        self.cnt[k] = 0
        self.cur[e] = k

    def _b(self, name):
        b = self.bufs.get(name)
        if b is None:
            b = Buf()
            self.bufs[name] = b
        return b

    def emit(self, eng, fn, r=(), w=(), pw=(), dma=False):
        deps = {}

        def add(d):
            for k, v in d.items():
                if deps.get(k, 0) < v:
                    deps[k] = v

        for n in r:
            add(self._b(n).w)
        for n in w:
            b = self._b(n)
            add(b.w)
            add(b.r)
        for n in pw:
            add(self._b(n).r)
        if dma:
            sl = self.slots[eng]
            k = sl[self.slot_next[eng] % len(sl)]
            self.slot_next[eng] += 1
            if self.cnt[k] > 0:
                add({k: self.cnt[k]})
            self.cnt[k] += 16
            inc = 16
        else:
            k = self.cur[eng]
            self.cnt[k] += 1
            inc = 1
        sig = (k, self.cnt[k])
        waits = []
        kn = self.known[eng]
        for dk, dv in deps.items():
            if (not dma) and dk == k and eng == "pe":
                continue
            if kn.get(dk, 0) >= dv:
                continue
            kn[dk] = dv
            waits.append((dk, dv))
        self.ops[eng].append((waits, fn, sig[0], inc))
        for n in r:
            b = self._b(n)
            if b.r.get(sig[0], 0) < sig[1]:
                b.r[sig[0]] = sig[1]
        for n in w:
            b = self._b(n)
            b.w = {sig[0]: sig[1]}
            b.r = {}
        for n in pw:
            b = self._b(n)
            b.w[sig[0]] = sig[1]
            b.r = {}
        self.n_ops += 1

    def barrier(self):
        allc = [(k, v) for k, v in self.cnt.items() if v > 0]
        for e in ENGS:
            kn = self.known[e]
            waits = []
            for k, v in allc:
                if kn.get(k, 0) < v:
                    kn[k] = v
                    waits.append((k, v))
            if waits:
                self.ops[e].append((waits, None, None, 0))
        self.bufs = {}
        self.epoch += 1
        for e in ("pe", "act", "dve", "pool"):
            if self.cnt[self.cur[e]] > 20000:
                self._new_compute_sem(e)

    def replay(self):
        nc = self.nc
        with nc.Block() as block:
            def run(e):
                def body(engine):
                    for waits, fn, sk, inc in self.ops[e]:
                        for k, v in waits:
                            engine.wait_ge(self.sems[k], v)
                        if fn is not None:
                            fn(engine).then_inc(self.sems[sk], inc)
                return body
            block.tensor(run("pe"))
            block.scalar(run("act"))
            block.vector(run("dve"))
            block.gpsimd(run("pool"))
            block.sync(run("sp"))


class Alloc:
    def __init__(self, big, nwords):
        self.big = big
        self.cap = nwords
        self.off = 0

    def f32(self, n):
        ap = self.big[:, self.off:self.off + n]
        self.off += n
        assert self.off <= self.cap, ("SBUF overflow", self.off, self.cap)
        return ap

    def bf16(self, n):
        return self.f32((n + 1) // 2).bitcast(BF16)

    def u32(self, n):
        return self.f32(n).bitcast(U32)


def build_nc(Lm, La, Lc, TB=2048, T3=512, CSP=2048, dbg=(), stop_after=None):
    nc = bass.Bass("TRN2", target_bir_lowering=False)
    T3 = min(T3, Lm)
    Ltot = Lm + La
    NM, NA, NC_ = Lm // 128, La // 128, Lc // 128
    NCH = NM + NA + NC_

    def din(name, shape, dt=F32):
        return nc.dram_tensor(name, list(shape), dt, kind="ExternalInput").ap()

    def dscr(name, shape, dt=F32):
        kind = "ExternalOutput" if name in dbg else "Internal"
        return nc.dram_tensor(name, list(shape), dt, kind=kind).ap()

    xm = din("xm", [Lm, D])
    xa = din("xa", [La, D])
    xc = din("xc", [Lc, D])
    cvec = din("cvec", [128, 32])
    w_ada = din("w_ada", [96, 128, 2048])
    b_ada = din("b_ada", [128, 96])
    n1g_d = din("n1g", [128, 16])
    n2g_d = din("n2g", [128, 16])
    w_in = din("w_in", [NG_IN, 128, 2048])
    convw_d = din("convw", [128, 48 * 5])
    convb_d = din("convb", [128, 48])
    dtb_d = din("dtb", [128, 1])
    alog_d = din("alog", [128, 1])
    ssdD_d = din("ssdD", [1, 64])
    gn_d = din("gnorm", [128, 32])
    wssd_d = din("wssd", [16, 128, 4096])
    scw_d = din("scw", [128, 48])
    wsc_d = din("wsc", [16, 128, 2048])
    wo_d = din("wo", [16, 128, 2048])
    wq_d = din("wq", [16, 128, 2048])
    keys_d = din("keysT", [128, 2048])
    pu_d = din("peer_u", [16384, D])
    pv_d = din("peer_v", [16384, D])
    fg_d = din("final_g", [1, D])
    out_d = nc.dram_tensor("out", [Lm, D], F32, kind="ExternalOutput").ap()

    S_a = dscr("S_a", [49 * 128, Ltot + 4])
    S_ac = dscr("S_ac", [49 * 128, Lc + 4])
    S_b = dscr("S_b", [112 * 128, Lm])
    S_xt = dscr("S_xt", [Ltot + Lc, DSSD])
    S_bt = dscr("S_bt", [Ltot + Lc, 1024])
    S_bT = dscr("S_bT", [1024, Lm])
    S_cT = dscr("S_cT", [1024, Lm])
    S_dtt = dscr("S_dtt", [NCH, 128, 128])
    S_act = dscr("S_act", [NCH, 128, 128])
    S_acc = dscr("S_acc", [NCH, 128 * 128])
    S_tot = dscr("S_tot", [NCH, 128])
    S_yb = dscr("S_yb", [Lm, DSSD])
    S_yn = dscr("S_yn", [DSSD, Lm], BF16)
    S_x1 = dscr("S_x1", [Lm, D])
    S_vec = dscr("S_vec", [64, 128])
    S_hst = dscr("S_hst", [2, 128, DSSD])
    S_uv = dscr("S_uv", [16384, 2 * D], BF16)

    with ExitStack() as st:
        NW = 52400
        big = st.enter_context(nc.sbuf_tensor("big", [128, NW], F32))
        PS = st.enter_context(nc.psum_tensor("PS", [128, 4096], F32))
        A = Alloc(big, NW)
        P = Prog(nc, st)
        E = P.emit

        def bank(b, n=512, o=0):
            return PS[:, b * 512 + o:b * 512 + o + n]

        def pb(*bs):
            return ["ps%d" % b for b in bs]

        def ld(name, out_ap, in_ap, r=(), eng="sp"):
            E(eng, lambda e: e.dma_start(out=out_ap, in_=in_ap), r=list(r), w=[name], dma=True)

        def stq(out_name, out_ap, in_name, in_ap, eng="pool"):
            E(eng, lambda e: e.dma_start(out=out_ap, in_=in_ap), r=[in_name], pw=[out_name], dma=True)

        iot = A.f32(128)
        ident = A.f32(128)
        maskU = A.f32(128)
        maskL = A.f32(128)
        io256 = A.f32(256)
        ones = A.f32(128)
        zt = A.f32(128)
        a1 = A.f32(16)
        sh1 = A.f32(16)
        a1c = A.f32(16)
        sh1c = A.f32(16)
        convw = A.f32(240)
        convb = A.f32(48)
        scw = A.f32(48)
        dtb = A.f32(1)
        acol_A = A.f32(1)
        gncol = A.f32(32)
        Dbc = A.f32(64)
        E("pool", lambda e: e.iota(iot, pattern=[[1, 128]], base=0, channel_multiplier=-1,
                                   allow_small_or_imprecise_dtypes=True), w=["iot"])
        E("pool", lambda e: e.iota(io256, pattern=[[1, 256]], base=0, channel_multiplier=0,
                                   allow_small_or_imprecise_dtypes=True), w=["io256"])
        E("dve", lambda e: e.tensor_single_scalar(out=ident, in_=iot, scalar=0.0, op=ALU.is_equal), r=["iot"], w=["ident"])
        E("dve", lambda e: e.tensor_single_scalar(out=maskU, in_=iot, scalar=0.0, op=ALU.is_ge), r=["iot"], w=["maskU"])
        E("dve", lambda e: e.tensor_single_scalar(out=maskL, in_=iot, scalar=0.0, op=ALU.is_le), r=["iot"], w=["maskL"])
        E("pool", lambda e: e.memset(ones, 1.0), w=["ones"])
        E("pool", lambda e: e.memset(zt, 0.0), w=["zt"])
        ld("convw", convw, convw_d)
        ld("convb", convb, convb_d)
        ld("scw", scw, scw_d)
        ld("dtb", dtb, dtb_d)
        ld("gncol", gncol, gn_d)
        ld("Dbc", Dbc, ssdD_d.partition_broadcast(128))
        alog = A.f32(1)
        ld("alog", alog, alog_d)
        E("act", lambda e: e.activation(out=acol_A, in_=alog, func=AF.Exp), r=["alog"], w=["acolA"])
        E("dve", lambda e: e.tensor_single_scalar(out=acol_A, in_=acol_A, scalar=-1.0, op=ALU.mult), r=["acolA"], w=["acolA"])
        E("sp", lambda e: e.dma_start(out=S_a.rearrange("(g p) c -> p g c", p=128)[:, :, 0:2],
                                      in_=zt[:, 0:98].rearrange("p (g c) -> p g c", c=2)), r=["zt"], pw=["S_a"], dma=True)
        E("sp", lambda e: e.dma_start(out=S_a.rearrange("(g p) c -> p g c", p=128)[:, :, Ltot + 2:Ltot + 4],
                                      in_=zt[:, 0:98].rearrange("p (g c) -> p g c", c=2)), r=["zt"], pw=["S_a"], dma=True)
        E("sp", lambda e: e.dma_start(out=S_ac.rearrange("(g p) c -> p g c", p=128)[:, :, 0:2],
                                      in_=zt[:, 0:98].rearrange("p (g c) -> p g c", c=2)), r=["zt"], pw=["S_ac"], dma=True)
        E("sp", lambda e: e.dma_start(out=S_ac.rearrange("(g p) c -> p g c", p=128)[:, :, Lc + 2:Lc + 4],
                                      in_=zt[:, 0:98].rearrange("p (g c) -> p g c", c=2)), r=["zt"], pw=["S_ac"], dma=True)
        base_off = A.off

        cv = A.f32(32)
        scv = A.f32(32)
        wts = [A.f32(2048), A.f32(2048)]
        modv = A.f32(192)
        bl = A.f32(96)
        n1g = A.f32(16)
        n2g = A.f32(16)
        V4 = A.f32(64)
        V4T = A.f32(128)
        ld("cv", cv, cvec)
        ld("bl", bl, b_ada)
        ld("n1g", n1g, n1g_d)
        ld("n2g", n2g, n2g_d)
        E("act", lambda e: e.activation(out=scv, in_=cv, func=AF.Silu), r=["cv"], w=["scv"])
        for j in range(96):
            wt = wts[j % 2]
            wn = "wt%d" % (j % 2)
            ld(wn, wt, w_ada[j])
            for kc in range(16):
                E("pe", lambda e, wt=wt, kc=kc, j=j: e.matmul(bank(0, 2, 2 * j), lhsT=wt[:, kc * 128:(kc + 1) * 128],
                                                             rhs=scv[:, 2 * kc:2 * kc + 2], start=(kc == 0), stop=(kc == 15)),
                  r=[wn, "scv"], w=pb(0))
        m3 = modv.rearrange("p (j n) -> p j n", n=2)
        E("dve", lambda e: e.tensor_tensor(out=m3, in0=bank(0, 192).rearrange("p (j n) -> p j n", n=2),
                                           in1=bl.unsqueeze(2).to_broadcast([128, 96, 2]), op=ALU.add),
          r=pb(0) + ["bl"], w=["modv"])

        def mcol(ch, n):
            return m3[:, ch * 16:(ch + 1) * 16, n]

        E("dve", lambda e: e.scalar_tensor_tensor(out=a1, in0=mcol(1, 0), scalar=1.0, in1=n1g, op0=ALU.add, op1=ALU.mult),
          r=["modv", "n1g"], w=["a1"])
        E("dve", lambda e: e.scalar_tensor_tensor(out=a1c, in0=mcol(1, 1), scalar=1.0, in1=n1g, op0=ALU.add, op1=ALU.mult),
          r=["modv", "n1g"], w=["a1c"])
        E("dve", lambda e: e.tensor_copy(out=sh1, in_=mcol(0, 0)), r=["modv"], w=["sh1"])
        E("dve", lambda e: e.tensor_copy(out=sh1c, in_=mcol(0, 1)), r=["modv"], w=["sh1c"])
        E("dve", lambda e: e.scalar_tensor_tensor(out=V4[:, 0:16], in0=mcol(4, 0), scalar=1.0, in1=n2g, op0=ALU.add, op1=ALU.mult),
          r=["modv", "n2g"], w=["V4"])
        E("dve", lambda e: e.tensor_copy(out=V4[:, 16:32], in_=mcol(3, 0)), r=["modv"], w=["V4"])
        E("dve", lambda e: e.tensor_copy(out=V4[:, 32:48], in_=mcol(2, 0)), r=["modv"], w=["V4"])
        E("dve", lambda e: e.tensor_copy(out=V4[:, 48:64], in_=mcol(5, 0)), r=["modv"], w=["V4"])
        E("pe", lambda e: e.transpose(out=bank(1, 128)[0:64, :], in_=V4, identity=ident), r=["V4", "ident"], w=pb(1))
        E("dve", lambda e: e.tensor_copy(out=V4T[0:64, :], in_=bank(1, 128)[0:64, :]), r=pb(1), w=["V4T"])
        E("sp", lambda e: e.dma_start(out=S_vec, in_=V4T[0:64, :]), r=["V4T"], w=["S_vec"], dma=True)
        P.barrier()
        A.off = base_off
        if stop_after == "P0":
            return _finish(nc, P, out_d, E, A, st)

        def inproj(tag, xd, L, acol, shcol, groups, dest, precast=False):
            T = min(L, TB)
            mark = A.off
            xts = [A.f32(2048), A.f32(2048)]
            junk = A.bf16(2048)
            xs = A.f32(2048)
            hT = A.bf16(16 * T).rearrange("p (k t) -> p k t", k=16)
            w32s = [A.f32(2048), A.f32(2048)]
            wbs = [A.bf16(2048), A.bf16(2048)]
            stg = [A.f32(T), A.f32(T)]
            ss = A.f32(4)
            NS = min(512, T)
            if precast:
                pc32 = [A.f32(2048), A.f32(2048)]
                pcb = [A.bf16(2048), A.bf16(2048)]
            pci = [0]

            def precast_step():
                i = pci[0]
                if (not precast) or i >= 256:
                    return
                pci[0] += 1
                tbl, dst, dn = (pu_d, S_uv[:, 0:D], "S_uv") if i < 128 else (pv_d, S_uv[:, D:2 * D], "S_uv")
                r0 = (i % 128) * 128
                a32, ab = pc32[i % 2], pcb[i % 2]
                n32_, nb_ = "pc32_%d" % (i % 2), "pcb_%d" % (i % 2)
                ld(n32_, a32, tbl[r0:r0 + 128, :])
                E("pool", lambda e: e.tensor_copy(out=ab, in_=a32), r=[n32_], w=[nb_])
                stq(dn, dst[r0:r0 + 128, :], nb_, ab)
            for blk in range(L // T):
                t0 = blk * T
                for tt in range(T // 128):
                    xt = xts[tt % 2]
                    xn = "xt%d" % (tt % 2)
                    ld(xn, xt, xd[t0 + tt * 128:t0 + (tt + 1) * 128, :])
                    E("act", lambda e, xt=xt: e.activation(out=junk, in_=xt, func=AF.Square, accum_out=ss[:, 0:1]),
                      r=[xn], w=["junk", "ss"])
                    E("dve", lambda e: e.tensor_scalar(out=ss[:, 1:2], in0=ss[:, 0:1], scalar1=1.0 / D, scalar2=EPS,
                                                       op0=ALU.mult, op1=ALU.add), r=["ss"], w=["ss1"])
                    E("act", lambda e: e.activation(out=ss[:, 2:3], in_=ss[:, 1:2], func=AF.Sqrt), r=["ss1"], w=["ss2"])
                    E("dve", lambda e: e.reciprocal(out=ss[:, 3:4], in_=ss[:, 2:3]), r=["ss2"], w=["ss3"])
                    E("act", lambda e, xt=xt: e.activation(out=xs, in_=xt, func=AF.Copy, scale=ss[:, 3:4]),
                      r=[xn, "ss3"], w=["xs"])
                    for dc in range(16):
                        E("pe", lambda e, dc=dc: e.transpose(out=bank(dc // 4, 128, (dc % 4) * 128),
                                                             in_=xs[:, dc * 128:(dc + 1) * 128], identity=ident),
                          r=["xs", "ident"], w=pb(dc // 4))
                    for dc in range(16):
                        E("dve", lambda e, dc=dc, tt=tt: e.tensor_scalar(
                            out=hT[:, dc, tt * 128:(tt + 1) * 128], in0=bank(dc // 4, 128, (dc % 4) * 128),
                            scalar1=acol[:, dc:dc + 1], scalar2=shcol[:, dc:dc + 1], op0=ALU.mult, op1=ALU.add),
                          r=pb(dc // 4) + [tag + "a", tag + "s"], w=["hT"])
                for gi, g in enumerate(groups):
                    w32 = w32s[gi % 2]
                    wb = wbs[gi % 2]
                    sg = stg[gi % 2]
                    n32, nb, nsg = "w32_%d" % (gi % 2), "wb_%d" % (gi % 2), "stg_%d" % (gi % 2)
                    ld(n32, w32, w_in[g])
                    E("act", lambda e, wb=wb, w32=w32: e.activation(out=wb, in_=w32, func=AF.Copy), r=[n32], w=[nb])
                    pbase = 4 * (gi % 2)
                    nsl = T // NS
                    for ns in range(nsl):
                        for kc in range(16):
                            E("pe", lambda e, wb=wb, ns=ns, kc=kc, pbase=pbase: e.matmul(
                                bank(pbase + ns, NS), lhsT=wb[:, kc * 128:(kc + 1) * 128],
                                rhs=hT[:, kc, ns * NS:(ns + 1) * NS], start=(kc == 0), stop=(kc == 15)),
                              r=[nb, "hT"], w=pb(pbase + ns))
                    E("dve", lambda e, sg=sg, pbase=pbase: e.tensor_copy(out=sg, in_=PS[:, pbase * 512:pbase * 512 + T]),
                      r=pb(*range(pbase, pbase + nsl)), w=[nsg])
                    dname, dap = dest(g, t0, T)
                    stq(dname, dap, nsg, sg)
                    precast_step()
            while precast and pci[0] < 256:
                precast_step()
            P.barrier()
            A.off = mark

        def dest_lat(off):
            def f(g, t0, T):
                if g < 49:
                    return "S_a", S_a[g * 128:(g + 1) * 128, 2 + off + t0:2 + off + t0 + T]
                return "S_b", S_b[(g - 49) * 128:(g - 48) * 128, t0:t0 + T]
            return f

        def dest_ctx(g, t0, T):
            return "S_ac", S_ac[g * 128:(g + 1) * 128, 2 + t0:2 + t0 + T]

        for nm, src in (("ca", "a1c"), ("cs", "sh1c"), ("la", "a1"), ("ls", "sh1")):
            pass
        inproj("c", xc, Lc, a1c, sh1c, list(range(40)) + [48], dest_ctx)
        inproj("l", xa, La, a1, sh1, list(range(49)), dest_lat(Lm))
        inproj("l", xm, Lm, a1, sh1, list(range(NG_IN)), dest_lat(0), precast=True)
        P.barrier()
        if stop_after == "P1":
            return _finish(nc, P, out_d, E, A, st)

        def conv_set(src, base, L, row0, ccs, feat):
            mark = A.off
            SPN = min(CSP, L)
            pres = [A.f32(SPN + 4), A.f32(SPN + 4)]
            acc = A.f32(SPN)
            ress = [A.f32(SPN), A.f32(SPN)]
            tms = [A.f32(SPN), A.f32(SPN)]
            it = 0
            for cc in ccs:
                for s0 in range(0, L, SPN):
                    n = SPN
                    pre, res, tm = pres[it % 2], ress[it % 2], tms[it % 2]
                    npre, nres, ntm = "pre%d" % (it % 2), "res%d" % (it % 2), "tm%d" % (it % 2)
                    it += 1
                    ld(npre, pre, src[cc * 128:(cc + 1) * 128, base + s0:base + s0 + n + 4])
                    E("dve", lambda e, pre=pre, cc=cc: e.tensor_scalar(out=acc, in0=pre[:, 0:n], scalar1=convw[:, cc * 5:cc * 5 + 1],
                                                                       scalar2=None, op0=ALU.mult), r=[npre, "convw"], w=["acc"])
                    for k in range(1, 5):
                        E("dve", lambda e, pre=pre, cc=cc, k=k: e.scalar_tensor_tensor(
                            out=acc, in0=pre[:, k:k + n], scalar=convw[:, cc * 5 + k:cc * 5 + k + 1], in1=acc,
                            op0=ALU.mult, op1=ALU.add), r=[npre, "convw", "acc"], w=["acc"])
                    E("act", lambda e, res=res, cc=cc: e.activation(out=res, in_=acc, func=AF.Silu, bias=convb[:, cc:cc + 1]),
                      r=["acc", "convb"], w=[nres])
                    if cc < 40:
                        J = n // 128
                        for j in range(J):
                            E("pe", lambda e, res=res, j=j: e.transpose(out=PS[:, j * 128:(j + 1) * 128],
                                                                        in_=res[:, j * 128:(j + 1) * 128], identity=ident),
                              r=[nres, "ident"], w=pb(j // 4))
                        E("act", lambda e, tm=tm: e.activation(out=tm, in_=PS[:, 0:n], func=AF.Copy),
                          r=pb(*range((J + 3) // 4)), w=[ntm])
                        if cc < 32:
                            dst = S_xt[row0 + s0:row0 + s0 + n, cc * 128:(cc + 1) * 128]
                            dn = "S_xt"
                        else:
                            dst = S_bt[row0 + s0:row0 + s0 + n, (cc - 32) * 128:(cc - 31) * 128]
                            dn = "S_bt"
                        stq(dn, dst.rearrange("(j p) c -> p j c", p=128), ntm, tm.rearrange("p (j c) -> p j c", c=128))
                    if feat and cc >= 32:
                        if cc < 40:
                            stq("S_bT", S_bT[(cc - 32) * 128:(cc - 31) * 128, s0:s0 + n], nres, res)
                        else:
                            stq("S_cT", S_cT[(cc - 40) * 128:(cc - 39) * 128, s0:s0 + n], nres, res)
            P.barrier()
            A.off = mark

        conv_set(S_ac, 0, Lc, Ltot, list(range(40)), False)
        conv_set(S_a, Lm, La, Lm, list(range(40)), False)
        conv_set(S_a, 0, Lm, 0, list(range(48)), True)

        def dt_set(src, base, L, cid0):
            mark = A.off
            SPN = min(2048, L)
            pd = A.f32(SPN)
            xb = A.f32(SPN)
            t1 = A.f32(SPN)
            dtv = A.f32(SPN)
            av = A.f32(SPN)
            ac = A.f32(SPN)
            tmpb = A.f32(128)
            totc = A.f32(16)
            tmA = A.f32(SPN)
            tmD = A.f32(SPN)
            totT = A.f32(128)
            for s0 in range(0, L, SPN):
                n = SPN
                ncn = n // 128
                ld("pd", pd, src[48 * 128:49 * 128, 2 + base + s0:2 + base + s0 + n])
                E("act", lambda e: e.activation(out=xb, in_=pd, func=AF.Identity, bias=dtb[:, 0:1]), r=["pd", "dtb"], w=["xb"])
                E("dve", lambda e: e.scalar_tensor_tensor(out=t1, in0=xb, scalar=-1.0, in1=xb, op0=ALU.mult, op1=ALU.max), r=["xb"], w=["t1"])
                E("act", lambda e: e.activation(out=t1, in_=t1, func=AF.Exp, scale=-1.0), r=["t1"], w=["t1"])
                E("act", lambda e: e.activation(out=t1, in_=t1, func=AF.Ln, bias=1.0), r=["t1"], w=["t1"])
                E("dve", lambda e: e.scalar_tensor_tensor(out=dtv, in0=xb, scalar=0.0, in1=t1, op0=ALU.max, op1=ALU.add),
                  r=["xb", "t1"], w=["dtv"])
                E("dve", lambda e: e.tensor_scalar(out=av, in0=dtv, scalar1=acol_A[:, 0:1], scalar2=None, op0=ALU.mult),
                  r=["dtv", "acolA"], w=["av"])
                E("dve", lambda e: e.tensor_reduce(out=totc[:, 0:ncn], in_=av.rearrange("p (c t) -> p c t", t=128),
                                                   axis=AX.X, op=ALU.add), r=["av"], w=["totc"])
                for c in range(ncn):
                    sl = slice(c * 128, (c + 1) * 128)
                    E("dve", lambda e, sl=sl: e.tensor_tensor_scan(out=ac[:, sl], data0=ones, data1=av[:, sl], initial=0.0,
                                                                   op0=ALU.mult, op1=ALU.add), r=["ones", "av", "ac"], w=["ac"])
                    E("dve", lambda e, sl=sl, c=c: e.tensor_scalar(out=tmpb[64:128, :], in0=ac[64:128, sl], scalar1=-1.0,
                                                                   scalar2=totc[64:128, c:c + 1], op0=ALU.mult, op1=ALU.add),
                      r=["ac", "totc"], w=["tmpb"])
                    E("dve", lambda e, sl=sl: e.tensor_tensor(out=ac[64:128, sl], in0=tmpb[64:128, :], in1=av[64:128, sl], op=ALU.add),
                      r=["tmpb", "av", "ac"], w=["ac"])
                stq("S_acc", S_acc[cid0 + s0 // 128:cid0 + s0 // 128 + ncn, :].rearrange("c (p t) -> p c t", p=128),
                    "ac", ac.rearrange("p (c t) -> p c t", t=128), eng="sp")
                for c in range(ncn):
                    E("pe", lambda e, c=c: e.transpose(out=PS[:, c * 128:(c + 1) * 128], in_=ac[:, c * 128:(c + 1) * 128], identity=ident),
                      r=["ac", "ident"], w=pb(c // 4))
                    E("pe", lambda e, c=c: e.transpose(out=PS[:, 2048 + c * 128:2048 + (c + 1) * 128], in_=dtv[:, c * 128:(c + 1) * 128],
                                                       identity=ident), r=["dtv", "ident"], w=pb(4 + c // 4))
                nb4 = (ncn + 3) // 4
                E("act", lambda e: e.activation(out=tmA, in_=PS[:, 0:n], func=AF.Copy), r=pb(*range(nb4)), w=["tmA"])
                E("act", lambda e: e.activation(out=tmD, in_=PS[:, 2048:2048 + n], func=AF.Copy), r=pb(*range(4, 4 + nb4)), w=["tmD"])
                c0 = cid0 + s0 // 128
                stq("S_act", S_act[c0:c0 + ncn].rearrange("c p k -> p c k"), "tmA", tmA.rearrange("p (c k) -> p c k", k=128), eng="sp")
                stq("S_dtt", S_dtt[c0:c0 + ncn].rearrange("c p k -> p c k"), "tmD", tmD.rearrange("p (c k) -> p c k", k=128), eng="sp")
                E("pe", lambda e: e.transpose(out=bank(3, 128)[0:ncn, :], in_=totc[:, 0:ncn], identity=ident),
                  r=["totc", "ident"], w=pb(3))
                E("dve", lambda e: e.tensor_copy(out=totT[0:ncn, :], in_=bank(3, 128)[0:ncn, :]), r=pb(3), w=["totT"])
                stq("S_tot", S_tot[c0:c0 + ncn, :], "totT", totT[0:ncn, :], eng="sp")
            P.barrier()
            A.off = mark

        dt_set(S_a, 0, Ltot, 0)
        dt_set(S_ac, 0, Lc, NM + NA)
        P.barrier()
        if stop_after == "P1b":
            return _finish(nc, P, out_d, E, A, st)

        mark2 = A.off
        hTst = [A.f32(DSSD), A.f32(DSSD)]
        xtoks = [A.f32(DSSD), A.f32(DSSD)]
        btoks = [A.f32(1024), A.f32(1024)]
        btokb = A.bf16(1024)
        bTfs = [A.f32(1024), A.f32(1024)]
        cTfs = [A.f32(1024), A.f32(1024)]
        bTb = A.bf16(1024).rearrange("p (g t) -> p g t", g=8)
        cTb = A.bf16(1024).rearrange("p (g t) -> p g t", g=8)
        dtcs = [A.f32(64), A.f32(64)]
        acls = [A.f32(64), A.f32(64)]
        totbs = [A.f32(128), A.f32(128)]
        chunk_ctr = [0]
        decend = A.f32(64)
        wcol = A.f32(64)
        ein = A.f32(64)
        declast = A.f32(64)
        xdt = A.bf16(DSSD)
        xdd = A.bf16(DSSD)
        bcgs = [A.f32(1024), A.f32(1024)]
        segs = [A.f32(1024), A.f32(1024)]
        Egs = [A.f32(1024), A.f32(1024)]
        wTs = [A.bf16(1024).rearrange("p (r t) -> p r t", r=8), A.bf16(1024).rearrange("p (r t) -> p r t", r=8)]
        cbms = [A.f32(128), A.f32(128)]
        hTbf = A.bf16(DSSD)
        yacc = A.f32(DSSD)
        tmps = [A.f32(512), A.f32(512)]
        zTs = [A.f32(1024), A.f32(1024)]
        sz = A.f32(1024)
        ynT = A.bf16(DSSD).rearrange("p (f t) -> p f t", f=32)
        ssq = A.f32(8)

        E("dve", lambda e: e.memset(hTst[0], 0.0), w=["hT0"])
        E("dve", lambda e: e.memset(hTst[1], 0.0), w=["hT1"])

        def ssd_chunk(cid, row0, dr, need_y, fcol0, final):
            hT = hTst[dr]
            hn = "hT%d" % dr
            mask = maskU if dr == 0 else maskL
            mname = "maskU" if dr == 0 else "maskL"
            cp = chunk_ctr[0] % 2
            chunk_ctr[0] += 1
            xtok, btok, bTf, cTf, dtc, acl, totb = xtoks[cp], btoks[cp], bTfs[cp], cTfs[cp], dtcs[cp], acls[cp], totbs[cp]
            nxt, nbtk, nbTf, ncTf, ndtc, nacl, ntotb = ["%s%d" % (n_, cp) for n_ in ("xtok", "btok", "bTf", "cTf", "dtc", "acl", "totb")]
            ld(nxt, xtok, S_xt[row0:row0 + 128, :])
            ld(nbtk, btok, S_bt[row0:row0 + 128, :])
            ld(ndtc, dtc, S_dtt[cid][:, dr * 64:(dr + 1) * 64])
            ld(nacl, acl, S_act[cid][:, dr * 64:(dr + 1) * 64])
            ld(ntotb, totb, S_tot[cid:cid + 1, :].partition_broadcast(128))
            if need_y:
                ld(nbTf, bTf.rearrange("p (g t) -> p g t", g=8),
                   S_bT[:, fcol0:fcol0 + 128].rearrange("(g p) t -> p g t", p=128))
                ld(ncTf, cTf.rearrange("p (g t) -> p g t", g=8),
                   S_cT[:, fcol0:fcol0 + 128].rearrange("(g p) t -> p g t", p=128))
            td = totb[:, dr * 64:(dr + 1) * 64]
            E("dve", lambda e: e.tensor_tensor(out=decend, in0=td, in1=acl, op=ALU.subtract), r=[ntotb, nacl], w=["decend"])
            E("act", lambda e: e.activation(out=decend, in_=decend, func=AF.Exp), r=["decend"], w=["decend"])
            E("dve", lambda e: e.tensor_tensor(out=wcol, in0=dtc, in1=decend, op=ALU.mult), r=[ndtc, "decend"], w=["wcol"])
            E("act", lambda e: e.activation(out=declast, in_=td, func=AF.Exp), r=[ntotb], w=["declast"])
            x3 = xtok.rearrange("p (h d) -> p h d", d=64)
            E("dve", lambda e: e.tensor_tensor(out=xdd.rearrange("p (h d) -> p h d", d=64), in0=x3,
                                               in1=wcol.unsqueeze(2).to_broadcast([128, 64, 64]), op=ALU.mult),
              r=[nxt, "wcol"], w=["xdd"])
            E("act", lambda e: e.activation(out=btokb, in_=btok, func=AF.Copy), r=[nbtk], w=["btokb"])
            if need_y:
                E("act", lambda e: e.activation(out=bTb, in_=bTf.rearrange("p (g t) -> p g t", g=8), func=AF.Copy), r=[nbTf], w=["bTb"])
                E("act", lambda e: e.activation(out=cTb, in_=cTf.rearrange("p (g t) -> p g t", g=8), func=AF.Copy), r=[ncTf], w=["cTb"])
                E("act", lambda e: e.activation(out=ein, in_=acl, func=AF.Exp), r=[nacl], w=["ein"])
                E("pool", lambda e: e.tensor_tensor(out=xdt.rearrange("p (h d) -> p h d", d=64), in0=x3,
                                                    in1=dtc.unsqueeze(2).to_broadcast([128, 64, 64]), op=ALU.mult),
                  r=[nxt, ndtc], w=["xdt"])
                E("act", lambda e: e.activation(out=hTbf, in_=hT, func=AF.Copy), r=[hn], w=["hTbf"])
                if final:
                    ld("yacc", yacc, S_yb[row0:row0 + 128, :], r=["S_yb"])
                else:
                    E("dve", lambda e: e.tensor_tensor(out=yacc.rearrange("p (h d) -> p h d", d=64), in0=x3,
                                                       in1=Dbc.unsqueeze(2).to_broadcast([128, 64, 64]), op=ALU.mult),
                      r=[nxt, "Dbc"], w=["yacc"])

            def stage_a(g):
                gp = g % 2
                pb0 = 4 * gp
                seg, Eg, cbm, bcg = segs[gp], Egs[gp], cbms[gp], bcgs[gp]
                sgn, egn, cbn, bn = "seg%d" % gp, "Eg%d" % gp, "cbm%d" % gp, "bcg%d" % gp
                o0 = (dr * 64 + g * 8) * 128
                ld(bn, bcg, S_acc[cid:cid + 1, o0:o0 + 1024].partition_broadcast(128))
                E("pe", lambda e: e.matmul(bank(pb0, 128), lhsT=bTb[:, g, :], rhs=cTb[:, g, :], start=True, stop=True),
                  r=["bTb", "cTb"], w=pb(pb0))
                E("dve", lambda e: e.tensor_tensor(out=cbm, in0=bank(pb0, 128), in1=mask, op=ALU.mult), r=pb(pb0) + [mname], w=[cbn])
                for r_ in range(8):
                    hh = g * 8 + r_
                    E("dve", lambda e, r_=r_, hh=hh: e.tensor_scalar(
                        out=seg[:, r_ * 128:(r_ + 1) * 128], in0=bcg[:, r_ * 128:(r_ + 1) * 128],
                        scalar1=acl[:, hh:hh + 1], scalar2=0.0, op0=ALU.subtract, op1=ALU.min),
                      r=[bn, nacl], w=[sgn])
                E("act", lambda e: e.activation(out=Eg, in_=seg, func=AF.Exp), r=[sgn], w=[egn])

            def stage_b(g):
                gp = g % 2
                pb0 = 4 * gp
                gs = slice(g * 512, (g + 1) * 512)
                Eg, cbm, wT, tmp = Egs[gp], cbms[gp], wTs[gp], tmps[gp]
                egn, cbn, wtn, tn = "Eg%d" % gp, "cbm%d" % gp, "wT%d" % gp, "tmp%d" % gp
                if need_y:
                    E("dve", lambda e: e.tensor_tensor(out=wT, in0=Eg.rearrange("p (r t) -> p r t", r=8),
                                                       in1=cbm.unsqueeze(1).to_broadcast([128, 8, 128]), op=ALU.mult),
                      r=[egn, cbn], w=[wtn])
                    for r_ in range(8):
                        hh = g * 8 + r_
                        E("pe", lambda e, r_=r_, hh=hh: e.matmul(bank(pb0 + 1, 64, r_ * 64), lhsT=wT[:, r_, :],
                                                                 rhs=xdt[:, hh * 64:(hh + 1) * 64], start=True, stop=True),
                          r=[wtn, "xdt"], w=pb(pb0 + 1))
                    E("pe", lambda e: e.matmul(bank(pb0 + 2), lhsT=cTb[:, g, :], rhs=hTbf[:, gs], start=True, stop=True),
                      r=["cTb", "hTbf"], w=pb(pb0 + 2))
                    E("dve", lambda e: e.tensor_tensor(
                        out=tmp.rearrange("p (r d) -> p r d", d=64), in0=bank(pb0 + 2).rearrange("p (r d) -> p r d", d=64),
                        in1=ein[:, g * 8:(g + 1) * 8].unsqueeze(2).to_broadcast([128, 8, 64]), op=ALU.mult),
                      r=pb(pb0 + 2) + ["ein"], w=[tn])
                    E("pool", lambda e: e.tensor_tensor(out=yacc[:, gs], in0=yacc[:, gs], in1=tmp, op=ALU.add),
                      r=[tn, "yacc"], w=["yacc"])
                    E("dve", lambda e: e.tensor_tensor(out=yacc[:, gs], in0=yacc[:, gs], in1=bank(pb0 + 1), op=ALU.add),
                      r=pb(pb0 + 1) + ["yacc"], w=["yacc"])
                E("pe", lambda e: e.matmul(bank(pb0 + 3), lhsT=btokb[:, g * 128:(g + 1) * 128], rhs=xdd[:, gs], start=True, stop=True),
                  r=["btokb", "xdd"], w=pb(pb0 + 3))
                E("pool", lambda e: e.tensor_tensor(
                    out=hT[:, gs].rearrange("p (r d) -> p r d", d=64), in0=hT[:, gs].rearrange("p (r d) -> p r d", d=64),
                    in1=declast[:, g * 8:(g + 1) * 8].unsqueeze(2).to_broadcast([128, 8, 64]), op=ALU.mult),
                  r=[hn, "declast", "hTbf"], w=[hn])
                E("dve", lambda e: e.tensor_tensor(out=hT[:, gs], in0=hT[:, gs], in1=bank(pb0 + 3), op=ALU.add),
                  r=pb(pb0 + 3) + [hn], w=[hn])

            if need_y:
                stage_a(0)
            for g in range(8):
                if need_y and g + 1 < 8:
                    stage_a(g + 1)
                stage_b(g)
            if need_y and not final:
                stq("S_yb", S_yb[row0:row0 + 128, :], "yacc", yacc, eng="sp")
            if final:
                for q in range(4):
                    zT = zTs[q % 2]
                    zn = "zT%d" % (q % 2)
                    ld(zn, zT.rearrange("p (f t) -> p f t", f=8),
                       S_b[q * 1024:(q + 1) * 1024, fcol0:fcol0 + 128].rearrange("(f p) t -> p f t", p=128))
                    for f in range(8):
                        E("pe", lambda e, f=f, zT=zT: e.transpose(out=PS[:, 2048 + f * 128:2048 + (f + 1) * 128],
                                                                  in_=zT[:, f * 128:(f + 1) * 128], identity=ident),
                          r=[zn, "ident"], w=pb(4 + f // 4))
                    E("act", lambda e: e.activation(out=sz, in_=PS[:, 2048:3072], func=AF.Silu), r=pb(4, 5), w=["sz"])
                    qs = slice(q * 1024, (q + 1) * 1024)
                    E("dve", lambda e, qs=qs: e.tensor_tensor(out=yacc[:, qs], in0=yacc[:, qs], in1=sz, op=ALU.mult),
                      r=["sz", "yacc"], w=["yacc"])
                    E("act", lambda e, qs=qs, q=q: e.activation(out=sz, in_=yacc[:, qs], func=AF.Square, accum_out=ssq[:, q:q + 1]),
                      r=["yacc", "sz"], w=["sz", "ssq"])
                E("dve", lambda e: e.tensor_reduce(out=ssq[:, 4:5], in_=ssq[:, 0:4], axis=AX.X, op=ALU.add), r=["ssq"], w=["ssq"])
                E("dve", lambda e: e.tensor_scalar(out=ssq[:, 5:6], in0=ssq[:, 4:5], scalar1=1.0 / DSSD, scalar2=EPS,
                                                   op0=ALU.mult, op1=ALU.add), r=["ssq"], w=["ssq"])
                E("act", lambda e: e.activation(out=ssq[:, 6:7], in_=ssq[:, 5:6], func=AF.Sqrt), r=["ssq"], w=["ssq"])
                E("dve", lambda e: e.reciprocal(out=ssq[:, 7:8], in_=ssq[:, 6:7]), r=["ssq"], w=["ssq"])
                E("act", lambda e: e.activation(out=yacc, in_=yacc, func=AF.Copy, scale=ssq[:, 7:8]), r=["yacc", "ssq"], w=["yacc"])
                for half in range(2):
                    for f in range(16):
                        ff = half * 16 + f
                        E("pe", lambda e, f=f, ff=ff: e.transpose(out=PS[:, 2048 + f * 128:2048 + (f + 1) * 128],
                                                                  in_=yacc[:, ff * 128:(ff + 1) * 128], identity=ident),
                          r=["yacc", "ident"], w=pb(4 + f // 4))
                    E("act", lambda e, half=half: e.activation(out=ynT[:, half * 16:(half + 1) * 16, :],
                                                               in_=PS[:, 2048:4096].rearrange("p (f t) -> p f t", f=16), func=AF.Copy),
                      r=pb(4, 5, 6, 7), w=["ynT"])
                stq("S_yn", S_yn[:, fcol0:fcol0 + 128].rearrange("(f p) t -> p f t", p=128), "ynT", ynT, eng="sp")

        for c in range(NC_):
            ssd_chunk(NM + NA + c, Ltot + c * 128, 0, False, 0, False)
        for c in reversed(range(NC_)):
            ssd_chunk(NM + NA + c, Ltot + c * 128, 1, False, 0, False)
        for c in reversed(range(NA)):
            ssd_chunk(NM + c, Lm + c * 128, 1, False, 0, False)
        for c in reversed(range(NM)):
            ssd_chunk(c, c * 128, 1, True, c * 128, False)
        for c in range(NM):
            ssd_chunk(c, c * 128, 0, True, c * 128, True)
        if "S_hst" in dbg:
            stq("S_hst", S_hst[0], "hT0", hTst[0], eng="sp")
            stq("S_hst", S_hst[1], "hT1", hTst[1], eng="sp")
        P.barrier()
        A.off = mark2
        if stop_after == "P2":
            return _finish(nc, P, out_d, E, A, st)

        mark3 = A.off
        vT = A.bf16(16 * T3).rearrange("p (k t) -> p k t", k=16)
        ynB = A.bf16(32 * T3).rearrange("p (k t) -> p k t", k=32)
        mrgT = A.bf16(16 * T3).rearrange("p (k t) -> p k t", k=16)
        oTs = A.f32(16 * T3).rearrange("p (k t) -> p k t", k=16)
        wA32_1 = A.f32(4096)
        wA32 = [wA32_1, wA32_1]
        wAb = [A.bf16(4096), A.bf16(4096)]
        wB32 = [A.f32(2048), A.f32(2048)]
        wBb = [A.bf16(2048), A.bf16(2048)]
        tb_ = [A.f32(T3)] * 2
        tc_ = [A.f32(T3)] * 2
        tx_ = [A.f32(T3)] * 2
        uu = A.f32(T3)
        cu = A.f32(T3)
        gsl = [A.f32(T3), A.f32(T3)]
        gcl = [A.f32(T3), A.f32(T3)]
        m1 = A.f32(T3)
        m2 = A.f32(T3)
        xt3 = A.f32(2048)
        tmp3 = A.f32(2048)
        g1bc = A.f32(2048)
        ld("g1bc", g1bc, S_vec[32:48, :].rearrange("(o a) b -> o (a b)", o=1).partition_broadcast(128))
        R = T3 // 64
        for blk in range(Lm // T3):
            t0 = blk * T3
            ld("ynB", ynB, S_yn[:, t0:t0 + T3].rearrange("(f p) t -> p f t", p=128))
            for dc in range(16):
                tb, tc, tx = tb_[dc % 2], tc_[dc % 2], tx_[dc % 2]
                nbn, ncn2, nxn = "tb0", "tc0", "tx0"
                ld(nbn, tb, S_b[(32 + dc) * 128:(33 + dc) * 128, t0:t0 + T3])
                ld(ncn2, tc, S_b[(48 + dc) * 128:(49 + dc) * 128, t0:t0 + T3])
                ld(nxn, tx, S_b[(64 + dc) * 128:(65 + dc) * 128, t0:t0 + T3])
                E("dve", lambda e, tc=tc, tx=tx: e.tensor_tensor(out=uu, in0=tc, in1=tx, op=ALU.mult), r=[ncn2, nxn], w=["uu"])
                E("dve", lambda e, dc=dc: e.tensor_scalar(out=cu, in0=uu, scalar1=scw[:, dc * 3 + 1:dc * 3 + 2], scalar2=None, op0=ALU.mult),
                  r=["uu", "scw"], w=["cu"])
                u3 = uu.rearrange("p (r w) -> p r w", w=64)
                c3 = cu.rearrange("p (r w) -> p r w", w=64)
                E("dve", lambda e, dc=dc: e.scalar_tensor_tensor(out=c3[:, :, 1:64], in0=u3[:, :, 0:63], scalar=scw[:, dc * 3:dc * 3 + 1],
                                                                 in1=c3[:, :, 1:64], op0=ALU.mult, op1=ALU.add),
                  r=["uu", "scw", "cu"], w=["cu"])
                E("dve", lambda e, dc=dc: e.scalar_tensor_tensor(out=c3[:, :, 0:63], in0=u3[:, :, 1:64], scalar=scw[:, dc * 3 + 2:dc * 3 + 3],
                                                                 in1=c3[:, :, 0:63], op0=ALU.mult, op1=ALU.add),
                  r=["uu", "scw", "cu"], w=["cu"])
                E("dve", lambda e, dc=dc, tb=tb: e.tensor_tensor(out=vT[:, dc, :], in0=tb, in1=cu, op=ALU.mult), r=[nbn, "cu"], w=["vT"])
            for oc in range(16):
                wa, wab, wbt, wbb = wA32[oc % 2], wAb[oc % 2], wB32[oc % 2], wBb[oc % 2]
                na, nab, nbt, nbb = "wA0", "wAb%d" % (oc % 2), "wB%d" % (oc % 2), "wBb%d" % (oc % 2)
                ld(na, wa, wssd_d[oc])
                ld(nbt, wbt, wsc_d[oc])
                E("dve", lambda e, wa=wa, wab=wab: e.tensor_tensor(out=wab.rearrange("p (k m) -> p k m", k=32),
                                                                   in0=wa.rearrange("p (k m) -> p k m", k=32),
                                                                   in1=gncol.unsqueeze(2).to_broadcast([128, 32, 128]), op=ALU.mult),
                  r=[na, "gncol"], w=[nab])
                E("act", lambda e, wbt=wbt, wbb=wbb: e.activation(out=wbb, in_=wbt, func=AF.Copy), r=[nbt], w=[nbb])
                for kc in range(32):
                    E("pe", lambda e, wab=wab, kc=kc: e.matmul(bank(0, T3), lhsT=wab[:, kc * 128:(kc + 1) * 128], rhs=ynB[:, kc, :],
                                                               start=(kc == 0), stop=(kc == 31)), r=[nab, "ynB"], w=pb(0))
                for kc in range(16):
                    E("pe", lambda e, wbb=wbb, kc=kc: e.matmul(bank(1, T3), lhsT=wbb[:, kc * 128:(kc + 1) * 128], rhs=vT[:, kc, :],
                                                               start=(kc == 0), stop=(kc == 15)), r=[nbb, "vT"], w=pb(1))
                gsv, gcv = gsl[oc % 2], gcl[oc % 2]
                ngs, ngc = "gs%d" % (oc % 2), "gc%d" % (oc % 2)
                ld(ngs, gsv, S_b[(80 + oc) * 128:(81 + oc) * 128, t0:t0 + T3])
                ld(ngc, gcv, S_b[(96 + oc) * 128:(97 + oc) * 128, t0:t0 + T3])
                E("act", lambda e, gsv=gsv: e.activation(out=gsv, in_=gsv, func=AF.Sigmoid), r=[ngs], w=[ngs])
                E("act", lambda e, gcv=gcv: e.activation(out=gcv, in_=gcv, func=AF.Sigmoid), r=[ngc], w=[ngc])
                E("dve", lambda e, gsv=gsv: e.tensor_tensor(out=m1, in0=bank(0, T3), in1=gsv, op=ALU.mult), r=pb(0) + [ngs], w=["m1"])
                E("dve", lambda e, gcv=gcv: e.tensor_tensor(out=m2, in0=bank(1, T3), in1=gcv, op=ALU.mult), r=pb(1) + [ngc], w=["m2"])
                E("pool", lambda e, oc=oc: e.tensor_tensor(out=mrgT[:, oc, :], in0=m1, in1=m2, op=ALU.add), r=["m1", "m2"], w=["mrgT"])
            for oc in range(16):
                wbt, wbb = wB32[oc % 2], wBb[oc % 2]
                nbt, nbb = "wB%d" % (oc % 2), "wBb%d" % (oc % 2)
                ld(nbt, wbt, wo_d[oc])
                E("act", lambda e, wbt=wbt, wbb=wbb: e.activation(out=wbb, in_=wbt, func=AF.Copy), r=[nbt], w=[nbb])
                for kc in range(16):
                    E("pe", lambda e, wbb=wbb, kc=kc, oc=oc: e.matmul(bank(2 + oc % 2, T3), lhsT=wbb[:, kc * 128:(kc + 1) * 128],
                                                                      rhs=mrgT[:, kc, :], start=(kc == 0), stop=(kc == 15)),
                      r=[nbb, "mrgT"], w=pb(2 + oc % 2))
                E("act", lambda e, oc=oc: e.activation(out=oTs[:, oc, :], in_=bank(2 + oc % 2, T3), func=AF.Copy),
                  r=pb(2 + oc % 2), w=["oTs"])
            for tt in range(T3 // 128):
                ld("xt3", xt3, xm[t0 + tt * 128:t0 + (tt + 1) * 128, :])
                for oc in range(16):
                    E("pe", lambda e, oc=oc, tt=tt: e.transpose(out=PS[:, 2048 + oc * 128:2048 + (oc + 1) * 128],
                                                                in_=oTs[:, oc, tt * 128:(tt + 1) * 128], identity=ident),
                      r=["oTs", "ident"], w=pb(4 + oc // 4))
                E("dve", lambda e: e.tensor_tensor(out=tmp3, in0=PS[:, 2048:4096], in1=g1bc, op=ALU.mult), r=pb(4, 5, 6, 7) + ["g1bc"], w=["tmp3"])
                E("pool", lambda e: e.tensor_tensor(out=tmp3, in0=tmp3, in1=xt3, op=ALU.add), r=["tmp3", "xt3"], w=["tmp3"])
                stq("S_x1", S_x1[t0 + tt * 128:t0 + (tt + 1) * 128, :], "tmp3", tmp3, eng="sp")
        P.barrier()
        A.off = mark3
        if stop_after == "P3":
            return _finish(nc, P, out_d, E, A, st)

        x1ts = [A.f32(2048), A.f32(2048)]
        h2f = A.f32(2048)
        h2bs = [A.bf16(2048), A.bf16(2048)]
        junk4 = A.bf16(2048)
        junkb = A.bf16(2048)
        h2T = A.bf16(2048).rearrange("p (k t) -> p k t", k=16)
        wq32 = [A.f32(2048), A.f32(2048)]
        wqb = [A.bf16(2048), A.bf16(2048)]
        qT = A.bf16(2048).rearrange("p (c t) -> p c t", c=16)
        k32 = A.f32(2048)
        keysb = A.bf16(2048).rearrange("p (c n) -> p c n", c=16)
        sc_ = A.f32(2048)
        wk = A.f32(256)
        sv = A.f32(256)
        si = A.u32(256)
        sif = A.f32(256)
        cs = A.f32(256)
        ci = A.f32(256)
        ts_ = A.f32(16)
        pos = A.u32(16)
        posf = A.f32(16)
        ohs = [A.f32(256), A.f32(256)]
        wv = A.f32(128)
        idxf = A.f32(128)
        idxus = [A.u32(128), A.u32(128)]
        gates = [A.f32(128), A.f32(128)]
        actv = A.f32(128)
        gact = A.f32(128)
        sm = A.f32(8)
        smf = A.f32(8)
        NB = 6
        UVg = [A.bf16(4096) for _ in range(NB)]
        dg = [A.bf16(128) for _ in range(4)]
        a2bc = A.f32(2048)
        sh2bc = A.f32(2048)
        g2bc = A.f32(2048)
        fgbc = A.f32(2048)
        x2 = A.f32(2048)
        ld("a2bc", a2bc, S_vec[0:16, :].rearrange("(o a) b -> o (a b)", o=1).partition_broadcast(128))
        ld("sh2bc", sh2bc, S_vec[16:32, :].rearrange("(o a) b -> o (a b)", o=1).partition_broadcast(128))
        ld("g2bc", g2bc, S_vec[48:64, :].rearrange("(o a) b -> o (a b)", o=1).partition_broadcast(128))
        ld("fgbc", fgbc, fg_d.partition_broadcast(128))
        ld("k32", k32, keys_d)
        E("act", lambda e: e.activation(out=keysb, in_=k32.rearrange("p (c n) -> p c n", c=16), func=AF.Copy), r=["k32"], w=["keysb"])

        def rstd_of(src_name, src, jnk, jn, smt, smn):
            E("act", lambda e: e.activation(out=jnk, in_=src, func=AF.Square, accum_out=smt[:, 0:1]), r=[src_name], w=[jn, smn])
            E("dve", lambda e: e.tensor_scalar(out=smt[:, 1:2], in0=smt[:, 0:1], scalar1=1.0 / D, scalar2=EPS, op0=ALU.mult, op1=ALU.add),
              r=[smn], w=[smn])
            E("act", lambda e: e.activation(out=smt[:, 2:3], in_=smt[:, 1:2], func=AF.Sqrt), r=[smn], w=[smn])
            E("dve", lambda e: e.reciprocal(out=smt[:, 3:4], in_=smt[:, 2:3]), r=[smn], w=[smn])

        NT = Lm // 128

        def peer_setup(tl):
            par = tl % 2
            t0 = tl * 128
            x1t, h2b, idxu, gate = x1ts[par], h2bs[par], idxus[par], gates[par]
            xn, hbn, iun, gtn = "x1t%d" % par, "h2b%d" % par, "idxu%d" % par, "gate%d" % par
            ld(xn, x1t, S_x1[t0:t0 + 128, :])
            rstd_of(xn, x1t, junk4, "junk4", sm, "sm")
            E("dve", lambda e: e.scalar_tensor_tensor(out=h2f, in0=x1t, scalar=sm[:, 3:4], in1=a2bc, op0=ALU.mult, op1=ALU.mult),
              r=[xn, "sm", "a2bc"], w=["h2f"])
            E("pool", lambda e: e.tensor_tensor(out=h2f, in0=h2f, in1=sh2bc, op=ALU.add), r=["h2f", "sh2bc"], w=["h2f"])
            E("act", lambda e: e.activation(out=h2b, in_=h2f, func=AF.Copy), r=["h2f"], w=[hbn])
            for dc in range(16):
                E("pe", lambda e, dc=dc: e.transpose(out=PS[:, dc * 128:(dc + 1) * 128], in_=h2f[:, dc * 128:(dc + 1) * 128], identity=ident),
                  r=["h2f", "ident"], w=pb(dc // 4))
            E("act", lambda e: e.activation(out=h2T, in_=PS[:, 0:2048].rearrange("p (k t) -> p k t", k=16), func=AF.Copy),
              r=pb(0, 1, 2, 3), w=["h2T"])
            for c in range(16):
                w32, wb = wq32[c % 2], wqb[c % 2]
                n32, nb = "wq%d" % (c % 2), "wqb%d" % (c % 2)
                ld(n32, w32, wq_d[c])
                E("act", lambda e, w32=w32, wb=wb: e.activation(out=wb, in_=w32, func=AF.Copy), r=[n32], w=[nb])
                for kc in range(16):
                    E("pe", lambda e, wb=wb, kc=kc, c=c: e.matmul(PS[:, c * 128:(c + 1) * 128], lhsT=wb[:, kc * 128:(kc + 1) * 128],
                                                                  rhs=h2T[:, kc, :], start=(kc == 0), stop=(kc == 15)),
                      r=[nb, "h2T"], w=pb(c // 4))
            E("act", lambda e: e.activation(out=qT, in_=PS[:, 0:2048].rearrange("p (c t) -> p c t", c=16), func=AF.Copy),
              r=pb(0, 1, 2, 3), w=["qT"])
            for c in range(16):
                E("pe", lambda e, c=c: e.matmul(PS[:, c * 128:(c + 1) * 128], lhsT=qT[:, c, :], rhs=keysb[:, c, :], start=True, stop=True),
                  r=["qT", "keysb"], w=pb(c // 4))
            E("act", lambda e: e.activation(out=sc_, in_=PS[:, 0:2048], func=AF.Copy), r=pb(0, 1, 2, 3), w=["sc"])
            for c in range(16):
                cs_ = slice(c * 128, (c + 1) * 128)
                v0 = slice(c * 16, c * 16 + 8)
                v1 = slice(c * 16 + 8, c * 16 + 16)
                E("dve", lambda e, cs_=cs_, v0=v0: e.max(out=sv[:, v0], in_=sc_[:, cs_]), r=["sc"], w=["sv"])
                E("dve", lambda e, cs_=cs_, v0=v0: e.max_index(out=si[:, v0], in_max=sv[:, v0], in_values=sc_[:, cs_]), r=["sc", "sv"], w=["si"])
                E("dve", lambda e, cs_=cs_, v0=v0: e.match_replace(out=wk[:, 0:128], in_to_replace=sv[:, v0], in_values=sc_[:, cs_], imm_value=-1e30),
                  r=["sc", "sv"], w=["wk"])
                E("dve", lambda e, v1=v1: e.max(out=sv[:, v1], in_=wk[:, 0:128]), r=["wk"], w=["sv"])
                E("dve", lambda e, v1=v1: e.max_index(out=si[:, v1], in_max=sv[:, v1], in_values=wk[:, 0:128]), r=["wk", "sv"], w=["si"])
            E("dve", lambda e: e.tensor_copy(out=sif, in_=si), r=["si"], w=["sif"])
            for h in range(8):
                s1 = sv[:, (2 * h) * 16:(2 * h) * 16 + 16]
                s2 = sv[:, (2 * h + 1) * 16:(2 * h + 1) * 16 + 16]
                i1 = sif[:, (2 * h) * 16:(2 * h) * 16 + 16]
                i2 = sif[:, (2 * h + 1) * 16:(2 * h + 1) * 16 + 16]
                cs3 = cs.rearrange("p (a b) -> p a b", a=16)
                ci3 = ci.rearrange("p (a b) -> p a b", a=16)
                E("dve", lambda e, s1=s1, s2=s2: e.tensor_tensor(out=cs3, in0=s1.unsqueeze(2).to_broadcast([128, 16, 16]),
                                                                 in1=s2.unsqueeze(1).to_broadcast([128, 16, 16]), op=ALU.add),
                  r=["sv"], w=["cs"])
                E("dve", lambda e, i1=i1, i2=i2: e.scalar_tensor_tensor(out=ci3, in0=i1.unsqueeze(2).to_broadcast([128, 16, 16]), scalar=128.0,
                                                                        in1=i2.unsqueeze(1).to_broadcast([128, 16, 16]),
                                                                        op0=ALU.mult, op1=ALU.add), r=["sif"], w=["ci"])
                E("dve", lambda e: e.max(out=ts_[:, 0:8], in_=cs), r=["cs"], w=["ts"])
                E("dve", lambda e: e.max_index(out=pos[:, 0:8], in_max=ts_[:, 0:8], in_values=cs), r=["cs", "ts"], w=["pos"])
                E("dve", lambda e: e.match_replace(out=wk, in_to_replace=ts_[:, 0:8], in_values=cs, imm_value=-1e30), r=["cs", "ts"], w=["wk"])
                E("dve", lambda e: e.max(out=ts_[:, 8:16], in_=wk), r=["wk"], w=["ts"])
                E("dve", lambda e: e.max_index(out=pos[:, 8:16], in_max=ts_[:, 8:16], in_values=wk), r=["wk", "ts"], w=["pos"])
                E("dve", lambda e: e.tensor_copy(out=posf, in_=pos), r=["pos"], w=["posf"])
                for k in range(16):
                    oh = ohs[k % 2]
                    ohn = "oh%d" % (k % 2)
                    E("dve", lambda e, k=k, oh=oh: e.tensor_scalar(out=oh, in0=io256, scalar1=posf[:, k:k + 1], scalar2=None, op0=ALU.is_equal),
                      r=["io256", "posf"], w=[ohn])
                    E("dve", lambda e, k=k, h=h, oh=oh: e.scalar_tensor_tensor(out=oh, in0=oh, scalar=1.0, in1=ci, op0=ALU.mult, op1=ALU.mult,
                                                                               accum_out=idxf[:, h * 16 + k:h * 16 + k + 1]),
                      r=[ohn, "ci"], w=[ohn, "idxf"])
                E("dve", lambda e: e.tensor_single_scalar(out=sm[:, 4:5], in_=ts_[:, 0:1], scalar=-1.0, op=ALU.mult), r=["ts"], w=["sm4"])
                E("act", lambda e, h=h: e.activation(out=gate[:, h * 16:(h + 1) * 16], in_=ts_, func=AF.Exp, bias=sm[:, 4:5],
                                                     accum_out=sm[:, 5:6]), r=["ts", "sm4"], w=[gtn, "sm5"])
                E("dve", lambda e: e.reciprocal(out=sm[:, 6:7], in_=sm[:, 5:6]), r=["sm5"], w=["sm6"])
                E("dve", lambda e, h=h: e.tensor_scalar(out=gate[:, h * 16:(h + 1) * 16], in0=gate[:, h * 16:(h + 1) * 16],
                                                        scalar1=sm[:, 6:7], scalar2=None, op0=ALU.mult), r=[gtn, "sm6"], w=[gtn])
            E("dve", lambda e: e.tensor_copy(out=idxu, in_=idxf), r=["idxf"], w=[iun])

        def peer_slots(tl):
            par = tl % 2
            t0 = tl * 128
            x1t, h2b, idxu, gate = x1ts[par], h2bs[par], idxus[par], gates[par]
            xn, hbn, iun, gtn = "x1t%d" % par, "h2b%d" % par, "idxu%d" % par, "gate%d" % par
            for s in range(128):
                uv, d_ = UVg[s % NB], dg[s % 4]
                ug, vg = uv[:, 0:2048], uv[:, 2048:4096]
                un, dn = "UV%d" % (s % NB), "dg%d" % (s % 4)
                vn = un
                an, gn_ = "actv%d" % (s % 8), "gact%d" % (s % 8)
                E("pool", lambda e, uv=uv, s=s: e.indirect_dma_start(out=uv, out_offset=None, in_=S_uv,
                                                                     in_offset=bass.IndirectOffsetOnAxis(ap=idxu[:, s:s + 1], axis=0)),
                  r=[iun], w=[un], dma=True)
                wn_ = "wv%d" % (s % 8)
                E("dve", lambda e, ug=ug, s=s: e.scalar_tensor_tensor(out=junkb, in0=ug, scalar=1.0, in1=h2b, op0=ALU.mult, op1=ALU.mult,
                                                                      accum_out=actv[:, s:s + 1]), r=[un, hbn], w=["junkb", an])
                E("act", lambda e, s=s: e.activation(out=gact[:, s:s + 1], in_=actv[:, s:s + 1], func=AF.Gelu), r=[an], w=[gn_])
                E("act", lambda e, s=s: e.activation(out=wv[:, s:s + 1], in_=gact[:, s:s + 1], func=AF.Copy, scale=gate[:, s:s + 1]),
                  r=[gn_, gtn], w=[wn_])
                E("act", lambda e, d_=d_, s=s: e.activation(out=d_, in_=ident, func=AF.Copy, scale=wv[:, s:s + 1]),
                  r=["ident", wn_], w=[dn])
                for n in range(4):
                    E("pe", lambda e, d_=d_, vg=vg, n=n, s=s: e.matmul(bank(4 + n), lhsT=d_, rhs=vg[:, n * 512:(n + 1) * 512],
                                                                       start=(s == 0), stop=(s == 127)), r=[dn, vn], w=pb(4 + n))
            E("dve", lambda e: e.tensor_tensor(out=x2, in0=PS[:, 2048:4096], in1=g2bc, op=ALU.mult), r=pb(4, 5, 6, 7) + ["g2bc"], w=["x2"])
            E("pool", lambda e: e.tensor_tensor(out=x2, in0=x2, in1=x1t, op=ALU.add), r=["x2", xn], w=["x2"])
            rstd_of("x2", x2, junk4, "junk4", smf, "smf")
            E("dve", lambda e: e.scalar_tensor_tensor(out=x2, in0=x2, scalar=smf[:, 3:4], in1=fgbc, op0=ALU.mult, op1=ALU.mult),
              r=["x2", "smf", "fgbc"], w=["x2"])
            stq("out", out_d[t0:t0 + 128, :], "x2", x2, eng="sp")

        peer_setup(0)
        for tl in range(NT):
            if tl + 1 < NT:
                peer_setup(tl + 1)
            peer_slots(tl)
        return _finish(nc, P, out_d, E, A, st)


def _finish(nc, P, out_d, E, A, st):
    P.barrier()
    P.replay()
    return nc


def _wl(w, kc):
    K, N = w.shape
    return np.ascontiguousarray(w.reshape(kc, 128, N // 128, 128).transpose(2, 1, 0, 3).reshape(N // 128, 128, kc * 128))


def _col(v, n):
    return np.ascontiguousarray(v.reshape(n, 128).T)


def make_in_maps(inp, n_cores, Lh):
    f = lambda a: np.asarray(a, dtype=np.float32)
    x, c, ctx, c_ctx = f(inp["x"]), f(inp["c"]), f(inp["ctx"]), f(inp["c_ctx"])
    w_in = f(inp["w_in"])[0]
    OFF_DT = 6144
    w_in_sw = w_in.copy()
    w_in_sw[:, OFF_DT:OFF_DT + 64] = w_in[:, OFF_DT + 64:OFF_DT + 128]
    w_in_sw[:, OFF_DT + 64:OFF_DT + 128] = w_in[:, OFF_DT:OFF_DT + 64]
    w_in_l = [_wl(w_in, 16), _wl(w_in_sw, 16)]
    w_ada_l = _wl(f(inp["w_ada"])[0], 16)
    b_ada_l = _col(f(inp["b_ada"])[0], 96)
    n1g = _col(f(inp["norm1_g"])[0], 16)
    n2g = _col(f(inp["norm2_g"])[0], 16)
    cw = f(inp["ssd_conv_w"])[0]
    cwl = [np.ascontiguousarray(cw.T.reshape(48, 128, 5).transpose(1, 0, 2).reshape(128, 240)),
           np.ascontiguousarray(cw[::-1].T.reshape(48, 128, 5).transpose(1, 0, 2).reshape(128, 240))]
    cb = _col(f(inp["ssd_conv_b"])[0], 48)
    dtb = f(inp["ssd_dt_bias"])[0].reshape(128, 1)
    alog = f(inp["ssd_A_log"])[0].reshape(128, 1)
    dtb_l = [dtb, np.ascontiguousarray(np.concatenate([dtb[64:], dtb[:64]], 0))]
    alog_l = [alog, np.ascontiguousarray(np.concatenate([alog[64:], alog[:64]], 0))]
    ssdD = f(inp["ssd_D"])[0].reshape(1, 64)
    gn = _col(f(inp["ssd_norm_g"])[0], 32)
    wssd = _wl(f(inp["ssd_w_out"])[0], 32)
    sw = f(inp["sc_conv_w"])[0]
    swl = [np.ascontiguousarray(sw.T.reshape(16, 128, 3).transpose(1, 0, 2).reshape(128, 48)),
           np.ascontiguousarray(sw[::-1].T.reshape(16, 128, 3).transpose(1, 0, 2).reshape(128, 48))]
    wsc = _wl(f(inp["sc_w_out"])[0], 16)
    wo = _wl(f(inp["w_o"])[0], 16)
    wq = _wl(f(inp["peer_w_q"])[0], 16)
    keys = f(inp["peer_keys"])[0]
    keysT = np.ascontiguousarray(keys.reshape(16, 128, 128).transpose(2, 0, 1).reshape(128, 2048))
    pu = np.ascontiguousarray(f(inp["peer_u"])[0])
    pv = np.ascontiguousarray(f(inp["peer_v"])[0])
    fg = f(inp["final_g"]).reshape(1, D)
    maps = []
    for core in range(n_cores):
        b, half = core // 2, core % 2
        xb = x[b]
        if half == 0:
            xm_, xa_, xc_ = xb[:Lh], xb[Lh:2 * Lh], ctx[b]
        else:
            rv = xb[:2 * Lh][::-1]
            xm_, xa_, xc_ = rv[:Lh], rv[Lh:], ctx[b][::-1]
        cvec = np.stack([c[b], c_ctx], -1).reshape(16, 128, 2).transpose(1, 0, 2).reshape(128, 32)
        maps.append({
            "xm": np.ascontiguousarray(xm_), "xa": np.ascontiguousarray(xa_), "xc": np.ascontiguousarray(xc_),
            "cvec": np.ascontiguousarray(cvec), "w_ada": w_ada_l, "b_ada": b_ada_l, "n1g": n1g, "n2g": n2g,
            "w_in": w_in_l[half], "convw": cwl[half], "convb": cb, "dtb": dtb_l[half], "alog": alog_l[half],
            "ssdD": ssdD, "gnorm": gn, "wssd": wssd, "scw": swl[half], "wsc": wsc, "wo": wo, "wq": wq,
            "keysT": keysT, "peer_u": pu, "peer_v": pv, "final_g": fg,
        })
    return maps


def kernel(**inputs):
    B, S, _ = inputs["x"].shape
    Lh = S // 2
    n_cores = 2 * B
    nc = build_nc(Lh, Lh, inputs["ctx"].shape[1])
    maps = make_in_maps(inputs, n_cores, Lh)
    res = run_bass_kernel_spmd(nc, maps, core_ids=list(range(n_cores)))
    out = np.empty((B, S, D), np.float32)
    for core in range(n_cores):
        b, half = core // 2, core % 2
        o = res.results[core]["out"]
        if half == 0:
            out[b, :Lh] = o
        else:
            out[b, Lh:] = o[::-1]
    return out
```
